# Optimizing a Trainium2 kernel written in Bass

```python
import math
import jax, jax.numpy as jnp
from jax import lax
import numpy as np

D_MODEL = 2048
BATCH = 4
SEQ = 2048
DEPTH = 4
DEC_BATCH = 8
DEC_SEQ = 1
PAST_LEN = 16384
PAGE_SIZE = 128

F32 = jnp.float32
EPS = 1e-6
N_MIXERS = 3
POOL_WINDOWS = (2, 4, 8, 16)
POOL_GROUP = D_MODEL // len(POOL_WINDOWS)
POOL_HIST = max(POOL_WINDOWS) - 1
DIL_PATTERNS = ((128, 1), (512, 4), (2048, 16))
DIL_GROUPS = len(DIL_PATTERNS)
DIL_HEADS = 8
DIL_HEAD_DIM = 128
DIL_WIDTH = DIL_GROUPS * DIL_HEADS * DIL_HEAD_DIM
DIL_BLOCK = 128
RET_HEADS = 8
RET_DK = D_MODEL // RET_HEADS
RET_DV = 2 * RET_DK
RET_QK = RET_HEADS * RET_DK
RET_V = RET_HEADS * RET_DV
RET_CHUNK = 128
PEER_HEADS = 8
PEER_NKEYS = 128
PEER_EXPERTS = PEER_NKEYS * PEER_NKEYS
PEER_KEY_DIM = 256
PEER_HALF = PEER_KEY_DIM // 2
PEER_TOPK = 16
PEER_BLOCK = 64
N_POOL_LAYERS = len(range(0, DEPTH, N_MIXERS))
N_DIL_LAYERS = len(range(1, DEPTH, N_MIXERS))
N_RET_LAYERS = len(range(2, DEPTH, N_MIXERS))

kernel_name = 'hybrid_pool_dilated_retention_peer_adaln_step'


def rmsnorm(x, g):
    xf = x.astype(F32)
    y = xf * lax.rsqrt(jnp.mean(xf * xf, axis=-1, keepdims=True) + EPS)
    return (y * g.astype(F32)).astype(x.dtype)


def adaln(c, w, b):
    m = (jax.nn.silu(c) @ w + b)[:, None, :]
    return jnp.split(m, 6, axis=-1)


def modulate(h, shift, scale):
    return h * (1.0 + scale) + shift


def alibi_slopes():
    n = DIL_GROUPS * DIL_HEADS
    return jnp.exp2(-8.0 * jnp.arange(1, n + 1, dtype=F32) / n).reshape(DIL_GROUPS, DIL_HEADS)


def pool_mixer(h, hist, pos0, w_pool, scale):
    n, l, _ = h.shape
    ext = h if hist is None else jnp.concatenate([hist.astype(h.dtype), h], axis=1)
    p = ext.shape[1] - l
    ef = ext.astype(F32)
    off = POOL_HIST + 1
    cs = jnp.concatenate([jnp.zeros((n, off, D_MODEL), F32), jnp.cumsum(ef, axis=1)], axis=1)
    pos = pos0 + jnp.arange(l)
    outs = []
    for gi, wdw in enumerate(POOL_WINDOWS):
        sl = slice(gi * POOL_GROUP, (gi + 1) * POOL_GROUP)
        win_sum = cs[:, off + p:off + p + l, sl] - cs[:, off + p - wdw:off + p - wdw + l, sl]
        cnt = jnp.minimum(wdw, pos + 1).astype(F32)[None, :, None]
        outs.append(win_sum / cnt - ef[:, p:, sl])
    d = jnp.stack(outs, axis=2)
    y = jnp.einsum('nlgc,gce->nlge', d, w_pool.astype(F32)).reshape(n, l, D_MODEL)
    y = y * scale.astype(F32)
    return y.astype(h.dtype), ext[:, -POOL_HIST:]


def band_attention(q, k, v, n_back, dil, slopes):
    n, ls, hh, hd = q.shape
    blk = DIL_BLOCK
    nb = -(-ls // blk)
    pad = nb * blk - ls

    def blocks(a):
        return jnp.pad(a, ((0, 0), (0, pad), (0, 0), (0, 0))).reshape(n, nb, blk, hh, hd)

    def with_prev(a):
        prev = jnp.pad(a, ((0, 0), (1, 0), (0, 0), (0, 0), (0, 0)))[:, :-1]
        return jnp.concatenate([prev, a], axis=2)

    qb = blocks(q)
    kk, vv = with_prev(blocks(k)), with_prev(blocks(v))
    s = jnp.einsum('nbqhd,nbkhd->nbhqk', qb, kk, preferred_element_type=F32) * (hd ** -0.5)
    qpos = jnp.arange(nb)[:, None] * blk + jnp.arange(blk)[None, :]
    kpos = jnp.arange(nb)[:, None] * blk - blk + jnp.arange(2 * blk)[None, :]
    dist = qpos[:, :, None] - kpos[:, None, :]
    valid = (dist >= 0) & (dist <= n_back) & (kpos[:, None, :] >= 0)
    s = s - slopes[None, :, None, None] * (dil * dist).astype(F32)[:, None]
    s = jnp.where(valid[None, :, None], s, -jnp.inf)
    m = jnp.max(s, axis=-1, keepdims=True)
    pr = jnp.exp(s - m)
    den = jnp.sum(pr, axis=-1)
    o = jnp.einsum('nbhqk,nbkhd->nbqhd', pr, vv.astype(F32)) / jnp.swapaxes(den, 2, 3)[..., None]
    lse = jnp.swapaxes(m[..., 0] + jnp.log(den), 2, 3)
    return o.reshape(n, nb * blk, hh, hd)[:, :ls], lse.reshape(n, nb * blk, hh)[:, :ls]


def dil_group_prompt(q, k, v, wdw, dil, slopes):
    n, t, hh, hd = q.shape
    ls = t // dil

    def sub(a):
        return a.reshape(n, ls, dil, hh, hd).transpose(0, 2, 1, 3, 4).reshape(n * dil, ls, hh, hd)

    o, lse = band_attention(sub(q), sub(k), sub(v), wdw // dil, dil, slopes)
    o = o.reshape(n, dil, ls, hh, hd).transpose(0, 2, 1, 3, 4).reshape(n, t, hh, hd)
    lse = lse.reshape(n, dil, ls, hh).transpose(0, 2, 1, 3).reshape(n, t, hh)
    nl = min(wdw, t)
    kv = jnp.stack([k[:, t - nl:], v[:, t - nl:]], axis=2)
    return o, lse, kv


def dil_group_sample(q, k, v, buf, wdw, dil, slopes):
    n, l, hh, hd = q.shape
    lb = buf.shape[1]
    ke = jnp.concatenate([buf[:, :, 0].astype(k.dtype), k], axis=1)
    ve = jnp.concatenate([buf[:, :, 1].astype(v.dtype), v], axis=1)
    steps = jnp.arange(wdw // dil + 1)
    idx = lb + jnp.arange(l)[:, None] - dil * steps[None, :]
    valid = idx >= 0
    idx = jnp.maximum(idx, 0)
    kg, vg = ke[:, idx], ve[:, idx]
    s = jnp.einsum('blhd,blkhd->bhlk', q, kg, preferred_element_type=F32) * (hd ** -0.5)
    s = s - slopes[None, :, None, None] * (dil * steps).astype(F32)
    s = jnp.where(valid[None, None], s, -jnp.inf)
    m = jnp.max(s, axis=-1, keepdims=True)
    pr = jnp.exp(s - m)
    den = jnp.sum(pr, axis=-1)
    o = jnp.einsum('bhlk,blkhd->blhd', pr, vg.astype(F32)) / jnp.swapaxes(den, 1, 2)[..., None]
    lse = jnp.swapaxes(m[..., 0] + jnp.log(den), 1, 2)
    nl = min(wdw, lb + l)
    kv = jnp.stack([ke[:, -nl:], ve[:, -nl:]], axis=2)
    return o, lse, kv


def dilated_mixer(h, bufs, w_in, w_out):
    n, l, _ = h.shape
    qkv = (h @ w_in).reshape(n, l, 3, DIL_GROUPS, DIL_HEADS, DIL_HEAD_DIM)
    q, k, v = qkv[:, :, 0], qkv[:, :, 1], qkv[:, :, 2]
    slopes = alibi_slopes()
    outs, lses, new = [], [], []
    for g, (wdw, dil) in enumerate(DIL_PATTERNS):
        if bufs is None:
            o, lse, kv = dil_group_prompt(q[:, :, g], k[:, :, g], v[:, :, g], wdw, dil, slopes[g])
        else:
            o, lse, kv = dil_group_sample(q[:, :, g], k[:, :, g], v[:, :, g], bufs[g], wdw, dil, slopes[g])
        outs.append(o)
        lses.append(lse)
        new.append(kv)
    wts = jax.nn.softmax(jnp.stack(lses), axis=0)
    o = jnp.sum(wts[..., None] * jnp.stack(outs), axis=0)
    y = o.reshape(n, l, DIL_HEADS * DIL_HEAD_DIM).astype(h.dtype) @ w_out
    return y, new


def retention_scan(q, k, v, s0):
    n, l, hh, _ = q.shape
    dv = v.shape[-1]
    c = math.gcd(l, RET_CHUNK)
    nc = l // c
    log_g = jnp.log1p(-jnp.exp2(-5.0 - jnp.arange(hh, dtype=F32)))
    pos = jnp.arange(c, dtype=F32)
    diff = pos[:, None] - pos[None, :]
    dmask = jnp.where(diff >= 0, jnp.exp(log_g[:, None, None] * jnp.maximum(diff, 0.0)), 0.0)
    q_dec = jnp.exp(log_g[None, :] * (pos[:, None] + 1.0))
    k_dec = jnp.exp(log_g[None, :] * (c - 1.0 - pos[:, None]))
    c_dec = jnp.exp(log_g * c)

    def chunks(a):
        return jnp.swapaxes(a.reshape(n, nc, c, hh, a.shape[-1]), 0, 1)

    def step(state, xs):
        qc, kc, vc = xs
        att = jnp.einsum('nchd,nmhd->nhcm', qc, kc) * dmask
        o = (jnp.einsum('nhcm,nmhe->nche', att, vc)
             + jnp.einsum('nchd,nhde->nche', qc * q_dec[None, :, :, None], state))
        state = (c_dec[None, :, None, None] * state
                 + jnp.einsum('nchd,nche->nhde', kc * k_dec[None, :, :, None], vc))
        return state, o

    state, o = lax.scan(step, s0, (chunks(q), chunks(k), chunks(v)))
    return jnp.swapaxes(o, 0, 1).reshape(n, l, hh, dv), state


def retention_mixer(h, s0, w_in, gn_g, w_out):
    n, l, _ = h.shape
    proj = h @ w_in
    q, k, v, gate = jnp.split(proj, [RET_QK, 2 * RET_QK, 2 * RET_QK + RET_V], axis=-1)
    q = q.reshape(n, l, RET_HEADS, RET_DK).astype(F32)
    k = k.reshape(n, l, RET_HEADS, RET_DK).astype(F32) * (RET_DK ** -0.5)
    v = v.reshape(n, l, RET_HEADS, RET_DV).astype(F32)
    if s0 is None:
        s0 = jnp.zeros((n, RET_HEADS, RET_DK, RET_DV), F32)
    o, state = retention_scan(q, k, v, s0.astype(F32))
    mu = jnp.mean(o, axis=-1, keepdims=True)
    var = jnp.mean(jnp.square(o - mu), axis=-1, keepdims=True)
    o = ((o - mu) * lax.rsqrt(var + EPS)).reshape(n, l, RET_V) * gn_g.astype(F32)
    y = (jax.nn.silu(gate.astype(F32)) * o).astype(h.dtype) @ w_out
    return y, state.astype(h.dtype)


def peer(h, w_q, sub_keys, u_tab, v_tab):
    n, l, d = h.shape
    t = n * l
    blk = min(PEER_BLOCK, t)
    nb = -(-t // blk)
    hb = jnp.pad(h.reshape(t, d), ((0, nb * blk - t), (0, 0))).reshape(nb, blk, d)

    def one(xb):
        q = (xb @ w_q).reshape(blk, PEER_HEADS, 2, PEER_HALF).astype(F32)
        sc = jnp.einsum('thpk,pmk->thpm', q, sub_keys.astype(F32))
        vals, idx = lax.top_k(sc, PEER_TOPK)
        cand = (vals[:, :, 0, :, None] + vals[:, :, 1, None, :]).reshape(blk, PEER_HEADS, -1)
        cid = (idx[:, :, 0, :, None] * PEER_NKEYS + idx[:, :, 1, None, :]).reshape(blk, PEER_HEADS, -1)
        top, sel = lax.top_k(cand, PEER_TOPK)
        eid = jnp.take_along_axis(cid, sel, axis=-1)
        g = jax.nn.softmax(top, axis=-1)
        u, v = u_tab[eid], v_tab[eid]
        a = jax.nn.gelu(jnp.einsum('td,thkd->thk', xb, u, preferred_element_type=F32), approximate=False)
        return jnp.einsum('thk,thkd->td', (g * a).astype(xb.dtype), v)

    out = lax.map(one, hb).reshape(nb * blk, d)[:t]
    return out.reshape(n, l, d)


def setup_inputs(seed: int = 0) -> dict:
    key = jax.random.key(seed)
    ks = iter(jax.random.split(key, 40))

    def nrm(shape, scale):
        return jax.random.normal(next(ks), shape, F32) * scale

    D = D_MODEL
    buf = [min(w, PAST_LEN) for w, _ in DIL_PATTERNS]
    kvs = (DIL_HEADS, DIL_HEAD_DIM)
    return {
        'x_prompt': nrm((BATCH, SEQ, D), 1.0),
        'x_sample': nrm((DEC_BATCH, DEC_SEQ, D), 1.0),
        'state_pool': nrm((N_POOL_LAYERS, DEC_BATCH, POOL_HIST, D), 1.0),
        'cache_dil_kv0': nrm((N_DIL_LAYERS, DEC_BATCH, buf[0], 2) + kvs, 1.0),
        'cache_dil_kv1': nrm((N_DIL_LAYERS, DEC_BATCH, buf[1], 2) + kvs, 1.0),
        'cache_dil_kv2': nrm((N_DIL_LAYERS, DEC_BATCH, buf[2], 2) + kvs, 1.0),
        'state_ret': nrm((N_RET_LAYERS, DEC_BATCH, RET_HEADS, RET_DK, RET_DV), 1.0),
        'c_prompt': nrm((BATCH, D), 1.0),
        'c_sample': nrm((DEC_BATCH, D), 1.0),
        'norm1_g': 1.0 + nrm((DEPTH, D), 0.1),
        'norm2_g': 1.0 + nrm((DEPTH, D), 0.1),
        'mod_w': nrm((DEPTH, D, 6 * D), 0.5 * D ** -0.5),
        'mod_b': nrm((DEPTH, 6 * D), 0.02),
        'pool_w': nrm((N_POOL_LAYERS, len(POOL_WINDOWS), POOL_GROUP, POOL_GROUP), POOL_GROUP ** -0.5),
        'pool_scale': 1.0 + nrm((N_POOL_LAYERS, D), 0.1),
        'dil_w_in': nrm((N_DIL_LAYERS, D, 3 * DIL_WIDTH), D ** -0.5),
        'dil_w_out': nrm((N_DIL_LAYERS, DIL_HEADS * DIL_HEAD_DIM, D), (DIL_HEADS * DIL_HEAD_DIM) ** -0.5),
        'ret_w_in': nrm((N_RET_LAYERS, D, 2 * RET_QK + 2 * RET_V), D ** -0.5),
        'ret_gn_g': 1.0 + nrm((N_RET_LAYERS, RET_V), 0.1),
        'ret_w_out': nrm((N_RET_LAYERS, RET_V, D), RET_V ** -0.5),
        'peer_w_q': nrm((DEPTH, D, PEER_HEADS * PEER_KEY_DIM), D ** -0.5),
        'peer_keys': nrm((DEPTH, 2, PEER_NKEYS, PEER_HALF), PEER_HALF ** -0.5),
        'peer_u': nrm((DEPTH, PEER_EXPERTS, D), D ** -0.5),
        'peer_v': nrm((DEPTH, PEER_EXPERTS, D), PEER_HEADS ** -0.5),
        'final_g': 1.0 + nrm((D,), 0.1),
    }


def reference(x_prompt, x_sample, state_pool, cache_dil_kv0, cache_dil_kv1, cache_dil_kv2, state_ret,
              c_prompt, c_sample, norm1_g, norm2_g, mod_w, mod_b, pool_w, pool_scale,
              dil_w_in, dil_w_out, ret_w_in, ret_gn_g, ret_w_out,
              peer_w_q, peer_keys, peer_u, peer_v, final_g):
    dil_caches = (cache_dil_kv0, cache_dil_kv1, cache_dil_kv2)
    xp, xs = x_prompt, x_sample
    pool_p, pool_s = [], []
    dil_p = [[] for _ in range(DIL_GROUPS)]
    dil_s = [[] for _ in range(DIL_GROUPS)]
    ret_p, ret_s = [], []
    for i in range(DEPTH):
        kind, j = i % N_MIXERS, i // N_MIXERS
        mp = adaln(c_prompt, mod_w[i], mod_b[i])
        ms = adaln(c_sample, mod_w[i], mod_b[i])
        hp = modulate(rmsnorm(xp, norm1_g[i]), mp[0], mp[1])
        hs = modulate(rmsnorm(xs, norm1_g[i]), ms[0], ms[1])
        if kind == 0:
            yp, st_p = pool_mixer(hp, None, 0, pool_w[j], pool_scale[j])
            ys, st_s = pool_mixer(hs, state_pool[j], PAST_LEN, pool_w[j], pool_scale[j])
            pool_p.append(st_p)
            pool_s.append(st_s)
        elif kind == 1:
            yp, kv_p = dilated_mixer(hp, None, dil_w_in[j], dil_w_out[j])
            ys, kv_s = dilated_mixer(hs, [cc[j] for cc in dil_caches], dil_w_in[j], dil_w_out[j])
            for g in range(DIL_GROUPS):
                dil_p[g].append(kv_p[g])
                dil_s[g].append(kv_s[g])
        else:
            yp, st_p = retention_mixer(hp, None, ret_w_in[j], ret_gn_g[j], ret_w_out[j])
            ys, st_s = retention_mixer(hs, state_ret[j], ret_w_in[j], ret_gn_g[j], ret_w_out[j])
            ret_p.append(st_p)
            ret_s.append(st_s)
        xp = xp + mp[2] * yp
        xs = xs + ms[2] * ys
        hp = modulate(rmsnorm(xp, norm2_g[i]), mp[3], mp[4])
        hs = modulate(rmsnorm(xs, norm2_g[i]), ms[3], ms[4])
        xp = xp + mp[5] * peer(hp, peer_w_q[i], peer_keys[i], peer_u[i], peer_v[i])
        xs = xs + ms[5] * peer(hs, peer_w_q[i], peer_keys[i], peer_u[i], peer_v[i])
    y_prompt = rmsnorm(xp, final_g)
    y_sample = rmsnorm(xs, final_g)
    return (y_prompt, y_sample,
            jnp.stack(pool_p), jnp.stack(pool_s),
            jnp.stack(dil_p[0]), jnp.stack(dil_s[0]),
            jnp.stack(dil_p[1]), jnp.stack(dil_s[1]),
            jnp.stack(dil_p[2]), jnp.stack(dil_s[2]),
            jnp.stack(ret_p), jnp.stack(ret_s))
```

```python
from contextlib import ExitStack
import math
import numpy as np
import concourse.bass as bass
import concourse.mybir as mybir
from concourse.ap import AP
from concourse.bass_utils import run_bass_kernel_spmd

F32 = mybir.dt.float32
BF16 = mybir.dt.bfloat16
U32 = mybir.dt.uint32
I32 = mybir.dt.int32
AF = mybir.ActivationFunctionType
ALU = mybir.AluOpType
AX = mybir.AxisListType

ENGS = ("pe", "dve", "act", "pool", "sp")
DMAQ = ("sp", "pool")
NDMA = 24
D = 2048
KT = 16
EPS = 1e-6


class Op:
    __slots__ = ("eng", "fn", "deps", "dma", "tok", "need", "pre")


class Prog:
    def __init__(self):
        self.nc = bass.Bass("TRN2", target_bir_lowering=False)
        self.es = ExitStack()
        self.ops = []
        self.lastw = {}
        self.readers = {}
        self.last_eng = {}
        self.recent_dma = {q: [] for q in DMAQ}
        self.uid = 0

    def sb(self, name, shape, dt=F32):
        return self.es.enter_context(self.nc.sbuf_tensor(name, list(shape), dt))

    def ps(self, name, shape, dt=F32):
        return self.es.enter_context(self.nc.psum_tensor(name, list(shape), dt))

    def dram(self, name, shape, dt=F32, kind="Internal"):
        return self.nc.dram_tensor(name, list(shape), dt, kind=kind).ap()

    def op(self, eng, fn, r=(), w=(), dma=False, join=False, extra=()):
        o = Op()
        o.eng, o.fn, o.dma, o.need, o.tok, o.pre = eng, fn, dma, False, None, None
        deps = set(extra)
        for x in r:
            deps.update(self.lastw.get(x, ()))
        for x in w:
            deps.update(self.lastw.get(x, ()))
            deps.update(self.readers.get(x, ()))
        if eng == "pe":
            deps = {d for d in deps if d.eng != "pe"}
        o.deps = deps
        for d in deps:
            d.need = True
        for x in r:
            lst = self.readers.setdefault(x, [])
            if not dma:
                lst[:] = [q for q in lst if q.dma or q.eng != eng]
            lst.append(o)
        for x in w:
            if join and x in self.lastw:
                self.lastw[x].append(o)
            else:
                self.lastw[x] = [o]
            self.readers[x] = []
        self.ops.append(o)
        if dma:
            self.recent_dma[eng].append(o)
            if len(self.recent_dma[eng]) > NDMA:
                self.recent_dma[eng].pop(0)
        else:
            self.last_eng[eng] = o
        return o

    def dma(self, out, in_, r=(), w=(), q="sp", join=False, **kw):
        kw.setdefault("allow_slow_non_contiguous", True)
        return self.op(q, lambda e: e.dma_start(out=out, in_=in_, **kw), r, w, dma=True, join=join)

    def barrier(self):
        deps = list(self.last_eng.values())
        for q in DMAQ:
            deps += self.recent_dma[q]
        for e in ENGS:
            self.op(e, lambda h: h.nop(), extra=deps)
        self.lastw = {}
        self.readers = {}

    def emit(self):
        nc = self.nc
        es = self.es
        sems = {e: es.enter_context(nc.semaphore("s_" + e)) for e in ENGS}
        dsem = {q: [es.enter_context(nc.semaphore("d_%s%d" % (q, i))) for i in range(NDMA)] for q in DMAQ}
        cnt = {e: 0 for e in ENGS}
        dcnt = {q: 0 for q in DMAQ}
        duse = {q: [0] * NDMA for q in DMAQ}
        for o in self.ops:
            if o.dma:
                i = dcnt[o.eng] % NDMA
                dcnt[o.eng] += 1
                prev = duse[o.eng][i]
                duse[o.eng][i] += 16
                o.pre = (dsem[o.eng][i], prev) if prev else None
                o.tok = (dsem[o.eng][i], prev + 16)
            elif o.need:
                cnt[o.eng] += 1
                o.tok = (sems[o.eng], cnt[o.eng])
        self.sem_counts = dict(cnt)
        per = {e: [o for o in self.ops if o.eng == e] for e in ENGS}
        block = es.enter_context(nc.Block())

        def run(engname, handle):
            known = {}
            for o in per[engname]:
                ws = []
                if o.pre is not None:
                    ws.append(o.pre)
                for d in o.deps:
                    ws.append(d.tok)
                for (s, v) in ws:
                    k = id(s)
                    if known.get(k, 0) >= v:
                        continue
                    known[k] = v
                    handle.wait_ge(s, v)
                inst = o.fn(handle)
                if o.tok is not None:
                    inst.then_inc(o.tok[0], 16 if o.dma else 1)
            if engname in dsem:
                for i in range(NDMA):
                    if duse[engname][i]:
                        handle.wait_ge(dsem[engname][i], duse[engname][i])

        @block.tensor
        def _(e):
            run("pe", e)

        @block.vector
        def _(e):
            run("dve", e)

        @block.scalar
        def _(e):
            run("act", e)

        @block.gpsimd
        def _(e):
            run("pool", e)

        @block.sync
        def _(e):
            run("sp", e)

        es.close()
        return nc


class Buf:
    def __init__(self, ap2d, name):
        self.ap = ap2d
        self.name = name
        self.t = ap2d.tensor
        self.off = ap2d.offset
        self.ps = ap2d.ap[0][0]
        self.n = ap2d.shape[1]

    def v(self, off=0, dims=None, p0=0, npart=128):
        if dims is None:
            dims = [(1, self.n - off)]
        return AP(self.t, self.off + p0 * self.ps + off, [[self.ps, npart]] + [[s, n] for (s, n) in dims])

    def __getitem__(self, k):
        return self.ap[k]


class Arena:
    def __init__(self, P, name, words):
        self.P = P
        self.t = P.sb(name, [128, words], F32)
        self.words = words
        self.off = 0
        self.gen = 0
        self.name = name

    def reset(self):
        self.off = 0
        self.gen += 1

    def f32(self, n, tag=""):
        assert self.off + n <= self.words, ("arena overflow", self.name, self.off, n, self.words)
        b = Buf(self.t[:, self.off:self.off + n], "%s_%d" % (self.name, self.off))
        self.off += n
        return b

    def bf16(self, n, tag=""):
        nw = (n + 1) // 2
        assert self.off + nw <= self.words, ("arena overflow", self.name, self.off, nw, self.words)
        b = Buf(self.t[:, self.off:self.off + nw].bitcast(BF16), "%s_%d" % (self.name, self.off))
        self.off += nw
        return b

    def u32(self, n, tag=""):
        assert self.off + n <= self.words
        b = Buf(self.t[:, self.off:self.off + n].bitcast(U32), "%s_%d" % (self.name, self.off))
        self.off += n
        return b


def blocks_of(T):
    bl = [(t0, min(512, T - t0)) for t0 in range(0, T, 512)]
    return bl + [(T, 1)]


class K:
    def __init__(self, T, dbg=()):
        self.T = T
        self.TT = T + 1
        self.P = Prog()
        self.dbg = set(dbg)
        P = self.P
        nc = P.nc
        self.inp = {}
        self.out = {}
        self.ar = Arena(P, "ar", 44 * 1024)
        self.cst = Arena(P, "cst", 3 * 1024)
        self.pb = [Buf(P.ps("pb%d" % i, [128, 512], F32)[:, :], "pb%d" % i) for i in range(8)]

    def din(self, name, shape, dt=F32):
        a = self.P.nc.dram_tensor(name, list(shape), dt, kind="ExternalInput").ap()
        self.inp[name] = a
        return a

    def dout(self, name, shape, dt=F32):
        a = self.P.nc.dram_tensor(name, list(shape), dt, kind="ExternalOutput").ap()
        self.out[name] = a
        return a

    def scratch(self, name, shape, dt=F32):
        if name in self.dbg:
            return self.dout(name, shape, dt)
        return self.P.dram(name, shape, dt)

    def mm(self, out, lhsT, rhs, start, stop, r, w):
        self.P.op("pe", lambda e: e.matmul(out, lhsT, rhs, start=start, stop=stop), r, w)

    def tr(self, out, in_, ident, r, w):
        self.P.op("pe", lambda e: e.transpose(out, in_, ident), r, w)

    def act(self, out, in_, func, r, w, bias=0.0, scale=1.0, accum_out=None):
        if accum_out is None:
            self.P.op("act", lambda e: e.activation(out, in_, func, bias=bias, scale=scale), r, w)
        else:
            self.P.op("act", lambda e: e.activation(out, in_, func, bias=bias, scale=scale, accum_out=accum_out), r, w)

    def tt(self, out, in0, in1, op, r, w, eng="dve"):
        self.P.op(eng, lambda e: e.tensor_tensor(out, in0, in1, op), r, w)

    def ts(self, out, in0, s1, op0, r, w, s2=None, op1=None, eng="dve"):
        if op1 is None:
            self.P.op(eng, lambda e: e.tensor_scalar(out, in0, s1, None, op0), r, w)
        else:
            self.P.op(eng, lambda e: e.tensor_scalar(out, in0, s1, s2, op0, op1), r, w)

    def stt(self, out, in0, scalar, in1, op0, op1, r, w):
        self.P.op("dve", lambda e: e.scalar_tensor_tensor(out, in0, scalar, in1, op0, op1), r, w)

    def cp(self, out, in_, r, w, eng="dve"):
        if eng == "act":
            self.P.op("act", lambda e: e.copy(out, in_), r, w)
        else:
            self.P.op(eng, lambda e: e.tensor_copy(out, in_), r, w)

    def memset(self, ap, val, w, eng="dve"):
        self.P.op(eng, lambda e: e.memset(ap, val), (), w)

    def stage_end(self):
        self.P.barrier()
        self.ar.reset()

    def setup_consts(self):
        P, c = self.P, self.cst
        self.identf = c.f32(128, "idf")
        self.identb = c.bf16(128, "idb")
        self.onesf = c.f32(128, "onf")
        self.onesb = c.bf16(128, "onb")
        self.iotaf = c.f32(128, "iof")
        tmp = self.ar.f32(128)
        tmpi = Buf(tmp.ap.bitcast(I32), tmp.name)
        P.op("pool", lambda e: e.iota(tmpi.ap, [[1, 128]], base=0, channel_multiplier=-1), (), [tmp.name])
        self.ts(self.identf.ap, tmpi.ap, 0.0, ALU.is_equal, [tmp.name], [self.identf.name])
        self.cp(self.identb.ap, self.identf.ap, [self.identf.name], [self.identb.name])
        self.memset(self.onesf.ap, 1.0, [self.onesf.name])
        self.memset(self.onesb.ap, 1.0, [self.onesb.name])
        tmp2 = self.ar.f32(128)
        tmp2i = Buf(tmp2.ap.bitcast(I32), tmp2.name)
        P.op("pool", lambda e: e.iota(tmp2i.ap, [[1, 128]], base=0, channel_multiplier=0), (), [tmp2.name])
        self.cp(self.iotaf.ap, tmp2i.ap, [tmp2.name], [self.iotaf.name])

    def load_vecT(self, dst, dname, src_ap, n):
        raw = self.ar.f32(128)
        pb = self.pb[7]
        self.P.dma(raw.v(0, [(1, 128)], 0, n), src_ap.rearrange("(k p) -> k p", p=128), (), [raw.name])
        self.tr(pb.v(0, [(1, n)]), raw.v(0, [(1, 128)], 0, n), self.identf.v(0, [(1, n)], 0, n), [raw.name, self.identf.name], [pb.name])
        self.cp(dst, pb.v(0, [(1, n)]), [pb.name], [dname])

    def st_loadx(self):
        T, P = self.T, self.P
        x_p = self.din("x_p", [T, D])
        x_s = self.din("x_s", [1, D])
        self.xT = self.scratch("xT", [KT, 128, self.TT])
        xtv = self.xT.rearrange("k p t -> p k t")
        for ti in range(T // 128):
            raw = self.ar.f32(D)
            xt = self.ar.f32(D)
            P.dma(raw.ap, x_p[ti * 128:(ti + 1) * 128, :], (), [raw.name])
            for q in range(4):
                pb = self.pb[q]
                for j in range(4):
                    k = q * 4 + j
                    self.tr(pb.v(j * 128, [(1, 128)]), raw.v(k * 128, [(1, 128)]), self.identf.ap, [raw.name, self.identf.name], [pb.name])
                self.cp(xt.v(q * 512, [(1, 512)]), pb.ap, [pb.name], [xt.name], eng="act" if q % 2 else "dve")
            P.dma(xtv[:, :, ti * 128:(ti + 1) * 128], xt.v(0, [(128, KT), (1, 128)]), [xt.name], ["xT"], q="pool", join=True)
            if ti % 2 == 1:
                self.ar.reset()
        self.stage_end()
        col = self.ar.f32(KT)
        self.load_vecT(col.ap, col.name, x_s[0, :], KT)
        P.dma(xtv[:, :, T:T + 1], col.v(0, [(1, KT), (1, 1)]), [col.name], ["xT"], q="pool", join=True)
        self.stage_end()

    def st_mod_setup(self):
        c_p = self.din("c_p", [D])
        c_s = self.din("c_s", [D])
        self.mod_w = self.din("mod_w", [4, D, 6 * D])
        self.mod_b = self.din("mod_b", [4, 6 * D])
        self.norm1_g = self.din("norm1_g", [4, D])
        self.norm2_g = self.din("norm2_g", [4, D])
        self.sc = self.cst.f32(KT * 2, "sc")
        self.modT = self.cst.f32(96 * 2, "modT")
        self.A1 = self.cst.f32(KT * 2, "A1")
        self.A2 = self.cst.f32(KT * 2, "A2")
        craw = self.ar.f32(KT * 2)
        self.load_vecT(craw.v(0, [(2, KT)]), craw.name, c_p, KT)
        self.load_vecT(craw.v(1, [(2, KT)]), craw.name, c_s, KT)
        self.act(self.sc.ap, craw.ap, AF.Silu, [craw.name], [self.sc.name])
        self.stage_end()

    def st_mod(self, i):
        P = self.P
        pb = self.pb[0]
        wv = self.mod_w[i].rearrange("(k p) f -> p k f", p=128)
        bufs = [self.ar.f32(KT * 512, "mw%d" % j) for j in range(2)]
        for fc in range(24):
            b = bufs[fc % 2]
            P.dma(b.v(0, [(512, KT), (1, 512)]), wv[:, :, fc * 512:(fc + 1) * 512], (), [b.name], q="sp" if fc % 2 == 0 else "pool")
            for ft in range(4):
                f = fc * 4 + ft
                for k in range(KT):
                    self.mm(pb.v(f * 2, [(1, 2)]), b.v(k * 512 + ft * 128, [(1, 128)]), self.sc.v(k * 2, [(1, 2)]),
                            k == 0, k == KT - 1, [b.name, self.sc.name], [pb.name])
        mb = self.ar.f32(96)
        self.load_vecT(mb.ap, mb.name, self.mod_b[i], 96)
        g1 = self.ar.f32(KT)
        g2 = self.ar.f32(KT)
        self.load_vecT(g1.ap, g1.name, self.norm1_g[i], KT)
        self.load_vecT(g2.ap, g2.name, self.norm2_g[i], KT)
        m = self.modT
        self.tt(m.v(0, [(2, 96), (1, 2)]), pb.v(0, [(2, 96), (1, 2)]), mb.v(0, [(1, 96), (0, 2)]), ALU.add,
                [pb.name, mb.name], [m.name])
        self.stt(self.A1.v(0, [(2, KT), (1, 2)]), m.v(1 * 32, [(2, KT), (1, 2)]), 1.0, g1.v(0, [(1, KT), (0, 2)]), ALU.add, ALU.mult,
                 [m.name, g1.name], [self.A1.name])
        self.stt(self.A2.v(0, [(2, KT), (1, 2)]), m.v(4 * 32, [(2, KT), (1, 2)]), 1.0, g2.v(0, [(1, KT), (0, 2)]), ALU.add, ALU.mult,
                 [m.name, g2.name], [self.A2.name])
        self.stage_end()

    def modv(self, idx, col):
        return self.modT.v(idx * 32 + col, [(2, KT)])

    def st_norm(self, A, shift_idx, hT, h32T=None, gvec=None):
        P, T = self.P, self.T
        xtv = self.xT.rearrange("k p t -> p k t")
        hv = hT.rearrange("k p t -> p k t") if hT is not None else None
        h32v = h32T.rearrange("k p t -> p k t") if h32T is not None else None
        for bi, (t0, w) in enumerate(blocks_of(T)):
            col = 1 if t0 == T else 0
            x = self.ar.f32(KT * 512, "x")
            sq = self.ar.f32(KT * 512, "sq")
            rs = self.ar.f32(512, "rs")
            hb = self.ar.bf16(KT * 512, "hb")
            pb = self.pb[bi % 2]
            xv = x.v(0, [(512, KT), (1, w)])
            sqv = sq.v(0, [(512, KT), (1, w)])
            P.dma(xv, xtv[:, :, t0:t0 + w], ["xT"], [x.name])
            self.act(sqv, xv, AF.Square, [x.name], [sq.name])
            for k in range(KT):
                self.mm(pb.v(0, [(1, w)]), self.onesf.ap, sq.v(k * 512, [(1, w)]), k == 0, k == KT - 1,
                        [sq.name, self.onesf.name], [pb.name])
            self.act(rs.v(0, [(1, w)]), pb.v(0, [(1, w)]), AF.Sqrt, [pb.name], [rs.name], bias=EPS, scale=1.0 / D)
            P.op("dve", lambda e, o=rs.v(0, [(1, w)]): e.reciprocal(o, o), [rs.name], [rs.name])
            self.tt(sqv, xv, rs.v(0, [(0, KT), (1, w)]), ALU.mult, [x.name, rs.name], [sq.name])
            if A is not None:
                self.tt(sqv, sqv, A.v(col, [(2, KT), (0, w)]), ALU.mult, [sq.name, A.name], [sq.name])
                self.tt(sqv, sqv, self.modT.v(shift_idx * 32 + col, [(2, KT), (0, w)]), ALU.add, [sq.name, self.modT.name], [sq.name])
            else:
                self.tt(sqv, sqv, gvec.v(0, [(1, KT), (0, w)]), ALU.mult, [sq.name, gvec.name], [sq.name])
            if hv is not None:
                self.cp(hb.v(0, [(512, KT), (1, w)]), sqv, [sq.name], [hb.name], eng="act")
                P.dma(hv[:, :, t0:t0 + w], hb.v(0, [(512, KT), (1, w)]), [hb.name], [hT.tensor.name], q="pool", join=True)
            if h32v is not None:
                P.dma(h32v[:, :, t0:t0 + w], sqv, [sq.name], [h32T.tensor.name], q="pool", join=True)
            if bi % 2 == 1:
                self.ar.reset()
        self.stage_end()

    def fm_to_rows(self, srcT, t0, n, dst_rows, r0=0):
        P = self.P
        src = self.ar.f32(KT * 128, "f2r")
        rows = self.ar.f32(D, "rows")
        P.dma(src.v(0, [(128, KT), (1, n)]), srcT.rearrange("k p t -> p k t")[:, :, t0:t0 + n], [srcT.tensor.name], [src.name])
        for q in range(4):
            pb = self.pb[4 + q]
            for j in range(4):
                k = q * 4 + j
                self.tr(pb.v(j * 128, [(1, 128)], 0, n), src.v(k * 128, [(1, n)]), self.identf.ap, [src.name, self.identf.name], [pb.name])
            self.cp(rows.v(q * 512, [(1, 512)], 0, n), pb.v(0, [(1, 512)], 0, n), [pb.name], [rows.name], eng="act" if q % 2 else "dve")
        P.dma(dst_rows, rows.v(0, [(1, D)], r0, n - r0), [rows.name], [dst_rows.tensor.name], q="pool", join=True)

    def st_final(self):
        T = self.T
        fg = self.din("final_g", [D])
        y_p = self.dout("y_p", [T, D])
        y_s = self.dout("y_s", [1, D])
        g = self.ar.f32(KT)
        self.load_vecT(g.ap, g.name, fg, KT)
        gk = self.cst.f32(KT, "fg")
        self.cp(gk.ap, g.ap, [g.name], [gk.name])
        self.stage_end()
        yT = self.scratch("yT", [KT, 128, self.TT])
        self.st_norm(None, 0, None, yT, gvec=gk)
        for ti in range(T // 128):
            self.fm_to_rows(yT, ti * 128, 128, y_p[ti * 128:(ti + 1) * 128, :])
            if ti % 2 == 1:
                self.ar.reset()
        self.stage_end()
        self.fm_to_rows(yT, T, 1, y_s[0:1, :])
        self.stage_end()

    def dump(self, name, view, shape, r):
        o = self.dout(name, shape)
        self.P.dma(o, view, r, [name], q="sp")

    def st_pool(self, j, h32T):
        P, T, TT = self.P, self.T, self.TT
        if j == 0:
            self.state_pool = self.din("state_pool", [2, 15, D])
            self.pool_w = self.din("pool_w", [2, 4, 512, 512])
            self.pool_scale = self.din("pool_scale", [2, D])
            self.pool_p = self.dout("pool_p", [2, 15, D])
            self.pool_s = self.dout("pool_s", [2, 15, D])
        self.fm_to_rows(h32T, T - 128, 128, self.pool_p[j], r0=113)
        P.dma(self.pool_s[j, 0:14, :], self.state_pool[j, 1:15, :], (), ["pool_s"], q="pool", join=True)
        self.stage_end()
        self.fm_to_rows(h32T, T, 1, self.pool_s[j, 14:15, :])
        self.stage_end()
        ps_ = self.ar.f32(KT)
        self.load_vecT(ps_.ap, ps_.name, self.pool_scale[j], KT)
        sg = self.cst_tmp("sg", KT * 2)
        self.tt(sg.v(0, [(2, KT), (1, 2)]), self.modT.v(2 * 32, [(2, KT), (1, 2)]), ps_.v(0, [(1, KT), (0, 2)]), ALU.mult,
                [self.modT.name, ps_.name], [sg.name])
        ext = self.cst_tmp("ext", KT * 16)
        hraw = self.ar.f32(D)
        P.dma(hraw.v(0, [(1, D)], 0, 15), self.state_pool[j], (), [hraw.name])
        for k in range(KT):
            pb = self.pb[4 + k % 4]
            self.tr(pb.v(0, [(1, 15)]), hraw.v(k * 128, [(1, 128)], 0, 15), self.identf.v(0, [(1, 15)], 0, 15),
                    [hraw.name, self.identf.name], [pb.name])
            self.cp(ext.v(k * 16, [(1, 15)]), pb.v(0, [(1, 15)]), [pb.name], [ext.name])
        P.dma(ext.v(15, [(16, KT), (1, 1)]), h32T.rearrange("k p t -> p k t")[:, :, T:T + 1], [h32T.tensor.name], [ext.name])
        self.stage_end()
        for g in range(4):
            w = 2 << g
            A = self.ar.f32(4 * T)
            B = self.ar.f32(4 * T)
            C = self.ar.f32(4 * T)
            dT = self.ar.bf16(4 * TT)
            X = self.ar.f32(4 * TT)
            Wf = self.ar.f32(4 * 512)
            Wb = self.ar.bf16(4 * 512)
            red = self.ar.f32(4)
            v3 = lambda b, o=0, n=T, s=T: b.v(o, [(s, 4), (1, n)])
            P.dma(v3(A), h32T[4 * g:4 * g + 4].rearrange("k p t -> p k t")[:, :, 0:T], [h32T.tensor.name], [A.name])
            P.dma(v3(X, 0, TT, TT), self.xT[4 * g:4 * g + 4].rearrange("k p t -> p k t"), ["xT"], [X.name], q="pool")
            P.dma(Wf.v(0, [(512, 4), (1, 512)]), self.pool_w[j, g].rearrange("(c p) e -> p c e", p=128), (), [Wf.name])
            self.cp(Wb.ap, Wf.ap, [Wf.name], [Wb.name], eng="act")
            cur = A
            for si in range(g + 1):
                st = 1 << si
                nxt = B if si % 2 == 0 else C
                self.cp(v3(nxt, 0, st), v3(cur, 0, st), [cur.name], [nxt.name], eng="act")
                self.tt(v3(nxt, st, T - st), v3(cur, st, T - st), v3(cur, 0, T - st), ALU.add, [cur.name], [nxt.name])
                cur = nxt
            self.stt(v3(dT, 0, T, TT), v3(cur), 1.0 / w, v3(A), ALU.mult, ALU.subtract, [cur.name, A.name], [dT.name])
            for t in range(w - 1):
                self.stt(v3(dT, t, 1, TT), v3(cur, t, 1), 1.0 / (t + 1), v3(A, t, 1), ALU.mult, ALU.subtract, [cur.name, A.name], [dT.name])
            P.op("dve", lambda e, o=red.ap, i=ext.v(4 * g * 16 + 16 - w, [(16, 4), (1, w)]): e.tensor_reduce(o, i, AX.X, ALU.add),
                 [ext.name], [red.name])
            self.stt(v3(dT, T, 1, TT), red.v(0, [(1, 4), (1, 1)]), 1.0 / w, ext.v(4 * g * 16 + 15, [(16, 4), (1, 1)]), ALU.mult, ALU.subtract,
                     [red.name, ext.name], [dT.name])
            nb = 0
            for et in range(4):
                k = 4 * g + et
                for (t0, wd) in blocks_of(T):
                    pb = self.pb[nb % 4]
                    nb += 1
                    col = 1 if t0 == T else 0
                    for c in range(4):
                        self.mm(pb.v(0, [(1, wd)]), Wb.v(c * 512 + et * 128, [(1, 128)]), dT.v(c * TT + t0, [(1, wd)]), c == 0, c == 3,
                                [Wb.name, dT.name], [pb.name])
                    xv = X.v(et * TT + t0, [(1, wd)])
                    self.stt(xv, pb.v(0, [(1, wd)]), sg.v(k * 2 + col, [(1, 1)]), xv, ALU.mult, ALU.add, [pb.name, sg.name, X.name], [X.name])
            P.dma(self.xT[4 * g:4 * g + 4].rearrange("k p t -> p k t"), v3(X, 0, TT, TT), [X.name], ["xT"], q="pool", join=True)
            self.ar.reset()
        self.stage_end()

    def cst_tmp(self, name, n):
        if not hasattr(self, "_ct"):
            self._ct = {}
        if name not in self._ct:
            self._ct[name] = self.cst.f32(n)
        return self._ct[name]

    def st_peer_route(self, i, hT):
        P, T, TT = self.P, self.T, self.TT
        if not hasattr(self, "peer_wq"):
            self.peer_wq = self.din("peer_w_q", [4, D, 2048])
            self.peer_keys = self.din("peer_keys", [4, 2, 128, 128])
            self.peer_u = self.din("peer_u", [4, 16384, D])
            self.peer_v = self.din("peer_v", [4, 16384, D])
            self.scd = self.scratch("scd", [T // 128 + 1, 128, 16, 128])
            self.Gd = self.scratch("Gd", [128, 128, T], BF16)
            self.Gsamp = self.cst.bf16(128)
        ntile = T // 128 + 1
        hs = self.ar.bf16(KT * TT)
        P.dma(hs.v(0, [(TT, KT), (1, TT)]), hT.rearrange("k p t -> p k t"), [hT.tensor.name], [hs.name])
        keysT = self.ar.f32(256)
        kraw = self.ar.f32(256)
        P.dma(kraw.v(0, [(128, 2), (1, 128)]), self.peer_keys[i].rearrange("a m k -> m a k"), (), [kraw.name])
        for p in range(2):
            pb = self.pb[6 + p]
            self.tr(pb.v(0, [(1, 128)]), kraw.v(p * 128, [(1, 128)]), self.identf.ap, [kraw.name, self.identf.name], [pb.name])
            self.cp(keysT.v(p * 128, [(1, 128)]), pb.v(0, [(1, 128)]), [pb.name], [keysT.name])
        wf = [self.ar.f32(KT * 128) for _ in range(2)]
        wb = [self.ar.bf16(KT * 128) for _ in range(2)]
        qf = [self.ar.f32(TT) for _ in range(2)]
        stg = [self.ar.f32(ntile * 128) for _ in range(2)]
        for s_ in stg:
            self.memset(s_.ap, 0.0, [s_.name], eng="pool")
        nb = 0
        for ft in range(16):
            s = ft % 2
            P.dma(wf[s].v(0, [(128, KT), (1, 128)]), self.peer_wq[i].rearrange("(k p) f -> p k f", p=128)[:, :, ft * 128:(ft + 1) * 128],
                  (), [wf[s].name])
            self.cp(wb[s].ap, wf[s].ap, [wf[s].name], [wb[s].name], eng="pool")
            for (t0, w) in blocks_of(T):
                pb = self.pb[nb % 3]
                nb += 1
                for k in range(KT):
                    self.mm(pb.v(0, [(1, w)]), wb[s].v(k * 128, [(1, 128)]), hs.v(k * TT + t0, [(1, w)]), k == 0, k == KT - 1,
                            [wb[s].name, hs.name], [pb.name])
                self.cp(qf[s].v(t0, [(1, w)]), pb.v(0, [(1, w)]), [pb.name], [qf[s].name], eng="act")
            for tt_ in range(ntile):
                m = 128 if tt_ < ntile - 1 else 1
                pb = self.pb[3 + tt_ % 3]
                self.mm(pb.v(0, [(1, 128)], 0, m), qf[s].v(tt_ * 128, [(1, m)]), keysT.v((ft % 2) * 128, [(1, 128)]), True, True,
                        [qf[s].name, keysT.name], [pb.name])
                self.cp(stg[s].v(tt_ * 128, [(1, 128)], 0, m), pb.v(0, [(1, 128)], 0, m), [pb.name], [stg[s].name])
            P.dma(self.scd[:, :, ft, :].rearrange("n p m -> p n m"), stg[s].v(0, [(128, ntile), (1, 128)]), [stg[s].name], ["scd"],
                  q="pool", join=True)
        self.stage_end()
        NEG = -1.0e30
        for tt_ in range(ntile):
            m = 128 if tt_ < ntile - 1 else 1
            sc = self.ar.f32(2048)
            scr = self.ar.f32(256)
            vals = self.ar.f32(256)
            idx = self.ar.u32(256)
            idxf = self.ar.f32(256)
            cand = self.ar.f32(2048)
            top = self.ar.f32(128)
            sel = self.ar.u32(128)
            selb = self.ar.u32(128)
            af = self.ar.f32(128)
            bf = self.ar.f32(128)
            gg = self.ar.f32(128)
            es = self.ar.f32(8)
            eq = self.ar.f32(2048)
            i1 = self.ar.f32(128)
            i2 = self.ar.f32(128)
            i1T = self.ar.f32(128)
            i2T = self.ar.f32(128)
            gT = self.ar.f32(128)
            oh1 = [self.ar.bf16(128) for _ in range(4)]
            oh2 = [self.ar.bf16(128) for _ in range(4)]
            Gs = self.ar.bf16(128 * 128)
            P.dma(sc.ap, self.scd[tt_].rearrange("p f m -> p (f m)"), ["scd"], [sc.name])
            V = lambda b, o, n: b.v(o, [(1, n)])
            for ft in range(16):
                s_in = V(sc, ft * 128, 128)
                P.op("dve", lambda e, o=V(vals, ft * 16, 8), i=s_in: e.max(out=o, in_=i), [sc.name], [vals.name])
                P.op("dve", lambda e, o=V(idx, ft * 16, 8), mx=V(vals, ft * 16, 8), i=s_in: e.max_index(o, mx, i), [sc.name, vals.name], [idx.name])
                P.op("dve", lambda e, o=V(scr, 0, 128), mx=V(vals, ft * 16, 8), i=s_in: e.match_replace(o, mx, i, NEG),
                     [sc.name, vals.name], [scr.name])
                P.op("dve", lambda e, o=V(vals, ft * 16 + 8, 8), i=V(scr, 0, 128): e.max(out=o, in_=i), [scr.name], [vals.name])
                P.op("dve", lambda e, o=V(idx, ft * 16 + 8, 8), mx=V(vals, ft * 16 + 8, 8), i=V(scr, 0, 128): e.max_index(o, mx, i),
                     [scr.name, vals.name], [idx.name])
            self.cp(idxf.ap, idx.ap, [idx.name], [idxf.name])
            self.tt(cand.v(0, [(256, 8), (16, 16), (1, 16)]), vals.v(0, [(32, 8), (1, 16), (0, 16)]), vals.v(16, [(32, 8), (0, 16), (1, 16)]),
                    ALU.add, [vals.name], [cand.name])
            for h in range(8):
                c_in = V(cand, h * 256, 256)
                P.op("dve", lambda e, o=V(top, h * 16, 8), i=c_in: e.max(out=o, in_=i), [cand.name], [top.name])
                P.op("dve", lambda e, o=V(sel, h * 16, 8), mx=V(top, h * 16, 8), i=c_in: e.max_index(o, mx, i), [cand.name, top.name], [sel.name])
                P.op("dve", lambda e, o=V(scr, 0, 256), mx=V(top, h * 16, 8), i=c_in: e.match_replace(o, mx, i, NEG),
                     [cand.name, top.name], [scr.name])
                P.op("dve", lambda e, o=V(top, h * 16 + 8, 8), i=V(scr, 0, 256): e.max(out=o, in_=i), [scr.name], [top.name])
                P.op("dve", lambda e, o=V(sel, h * 16 + 8, 8), mx=V(top, h * 16 + 8, 8), i=V(scr, 0, 256): e.max_index(o, mx, i),
                     [scr.name, top.name], [sel.name])
            self.tt(gg.v(0, [(16, 8), (1, 16)]), top.v(0, [(16, 8), (1, 16)]), top.v(0, [(16, 8), (0, 16)]), ALU.subtract, [top.name], [gg.name])
            self.act(gg.ap, gg.ap, AF.Exp, [gg.name], [gg.name])
            P.op("dve", lambda e, o=es.ap, i=gg.v(0, [(16, 8), (1, 16)]): e.tensor_reduce(o, i, AX.X, ALU.add), [gg.name], [es.name])
            P.op("dve", lambda e, o=es.ap: e.reciprocal(o, o), [es.name], [es.name])
            self.tt(gg.v(0, [(16, 8), (1, 16)]), gg.v(0, [(16, 8), (1, 16)]), es.v(0, [(1, 8), (0, 16)]), ALU.mult, [gg.name, es.name], [gg.name])
            self.ts(selb.ap, sel.ap, 4, ALU.logical_shift_right, [sel.name], [selb.name])
            self.cp(af.ap, selb.ap, [selb.name], [af.name])
            self.ts(selb.ap, sel.ap, 15, ALU.bitwise_and, [sel.name, af.name], [selb.name])
            self.cp(bf.ap, selb.ap, [selb.name], [bf.name])
            for (src, off, dst) in ((af, 0, i1), (bf, 16, i2)):
                self.tt(eq.v(0, [(256, 8), (16, 16), (1, 16)]), src.v(0, [(16, 8), (1, 16), (0, 16)]), self.iotaf.v(0, [(0, 8), (0, 16), (1, 16)]),
                        ALU.is_equal, [src.name, self.iotaf.name], [eq.name])
                self.tt(eq.v(0, [(256, 8), (16, 16), (1, 16)]), eq.v(0, [(256, 8), (16, 16), (1, 16)]), idxf.v(off, [(32, 8), (0, 16), (1, 16)]),
                        ALU.mult, [eq.name, idxf.name], [eq.name])
                P.op("dve", lambda e, o=dst.ap, i=eq.v(0, [(16, 128), (1, 16)]): e.tensor_reduce(o, i, AX.X, ALU.add), [eq.name], [dst.name])
            for (src, dst, bank) in ((i1, i1T, 5), (i2, i2T, 6), (gg, gT, 7)):
                pb = self.pb[bank]
                self.tr(pb.v(0, [(1, 128)]), src.ap, self.identf.ap, [src.name, self.identf.name], [pb.name])
                self.cp(dst.ap, pb.v(0, [(1, 128)]), [pb.name], [dst.name], eng="act")
            for t in range(m):
                s = t % 4
                self.ts(oh1[s].ap, self.iotaf.ap, i1T.v(t, [(1, 1)]), ALU.is_equal, [self.iotaf.name, i1T.name, gT.name], [oh1[s].name],
                        s2=gT.v(t, [(1, 1)]), op1=ALU.mult)
                self.ts(oh2[s].ap, self.iotaf.ap, i2T.v(t, [(1, 1)]), ALU.is_equal, [self.iotaf.name, i2T.name], [oh2[s].name])
                pb = self.pb[(t // 4) % 4]
                self.mm(pb.v((t % 4) * 128, [(1, 128)]), oh2[s].ap, oh1[s].ap, True, True, [oh1[s].name, oh2[s].name], [pb.name])
                if t % 4 == 3 or t == m - 1:
                    n4 = t % 4 + 1
                    tb = t - (t % 4)
                    self.cp(Gs.v(tb, [(1, n4), (128, 128)]), pb.v(0, [(128, n4), (1, 128)]), [pb.name], [Gs.name], eng="act")
            if m == 128:
                for cq in range(4):
                    P.dma(self.Gd[cq * 32:(cq + 1) * 32, :, tt_ * 128:(tt_ + 1) * 128].rearrange("c p t -> p c t"),
                          Gs.v(cq * 32 * 128, [(128, 32), (1, 128)]), [Gs.name], ["Gd"], q="pool" if cq % 2 else "sp", join=True)
            else:
                self.cp(self.Gsamp.ap, Gs.v(0, [(128, 128)]), [Gs.name], [self.Gsamp.name])
            self.ar.reset()
        self.stage_end()

    def st_peer_main(self, i, hT):
        P, T, TT = self.P, self.T, self.TT
        NCH = self.nchunks if hasattr(self, "nchunks") else 128
        groups = [(t0, min(1024, T - t0)) for t0 in range(0, T, 1024)]
        hv = hT.rearrange("k p t -> p k t")
        for gi, (t0, n) in enumerate(groups):
            last = gi == len(groups) - 1
            ne = n + 1 if last else n
            blks = [(b0, min(512, n - b0)) for b0 in range(0, n, 512)]
            acc = self.ar.f32(KT * ne)
            hg = self.ar.bf16(KT * ne)
            ufp = [self.ar.f32(D) for _ in range(2)]
            ubf = self.ar.bf16(D)
            uT = [self.ar.bf16(D) for _ in range(2)]
            vfp = [self.ar.f32(D) for _ in range(2)]
            vbf = [self.ar.bf16(D) for _ in range(4)]
            WT = [self.ar.bf16(ne) for _ in range(4)]
            actb = [self.ar.bf16(ne) for _ in range(2)]
            Gc = [self.ar.bf16(n) for _ in range(2)]
            P.dma(hg.v(0, [(ne, KT), (1, n)]), hv[:, :, t0:t0 + n], [hT.tensor.name], [hg.name])
            if last:
                P.dma(hg.v(n, [(ne, KT), (1, 1)]), hv[:, :, T:T + 1], [hT.tensor.name], [hg.name], join=True)
            pS = [self.pb[0], self.pb[1]]
            pSs = self.pb[2]
            pT = [self.pb[3], self.pb[4]]
            pV = [self.pb[5], self.pb[6]]
            pVs = self.pb[7]
            nv = 0
            for c in range(NCH):
                ci = c % 4
                s2 = c % 2
                P.dma(ufp[s2].ap, self.peer_u[i, c * 128:(c + 1) * 128, :], (), [ufp[s2].name])
                P.dma(vfp[s2].ap, self.peer_v[i, c * 128:(c + 1) * 128, :], (), [vfp[s2].name])
                P.dma(Gc[s2].ap, self.Gd[c, :, t0:t0 + n], ["Gd"], [Gc[s2].name], q="pool")
                self.cp(ubf.ap, ufp[s2].ap, [ufp[s2].name], [ubf.name], eng="pool")
                self.cp(vbf[ci].ap, vfp[s2].ap, [vfp[s2].name], [vbf[ci].name], eng="pool")
                for hb in range(2):
                    pt = pT[hb]
                    ptb = Buf(pt.ap.bitcast(BF16), pt.name)
                    for kk in range(8):
                        k = hb * 8 + kk
                        self.tr(ptb.v(kk * 128, [(1, 128)]), ubf.v(k * 128, [(1, 128)]), self.identb.ap, [ubf.name, self.identb.name], [pt.name])
                    self.cp(uT[s2].v(hb * 1024, [(1, 1024)]), ptb.v(0, [(1, 1024)]), [pt.name], [uT[s2].name], eng="act")
                for bi, (b0, wd) in enumerate(blks):
                    for k in range(KT):
                        self.mm(pS[bi].v(0, [(1, wd)]), uT[s2].v(k * 128, [(1, 128)]), hg.v(k * ne + b0, [(1, wd)]), k == 0, k == KT - 1,
                                [uT[s2].name, hg.name], [pS[bi].name])
                    self.act(actb[s2].v(b0, [(1, wd)]), pS[bi].v(0, [(1, wd)]), AF.Gelu, [pS[bi].name], [actb[s2].name])
                if last:
                    for k in range(KT):
                        self.mm(pSs.v(0, [(1, 1)]), uT[s2].v(k * 128, [(1, 128)]), hg.v(k * ne + n, [(1, 1)]), k == 0, k == KT - 1,
                                [uT[s2].name, hg.name], [pSs.name])
                    self.act(actb[s2].v(n, [(1, 1)]), pSs.v(0, [(1, 1)]), AF.Gelu, [pSs.name], [actb[s2].name])
                    self.tt(WT[ci].v(n, [(1, 1)]), actb[s2].v(n, [(1, 1)]), self.Gsamp.v(c, [(1, 1)]), ALU.mult,
                            [actb[s2].name, self.Gsamp.name], [WT[ci].name])
                self.tt(WT[ci].v(0, [(1, n)]), actb[s2].v(0, [(1, n)]), Gc[s2].ap, ALU.mult, [actb[s2].name, Gc[s2].name], [WT[ci].name])
                if ci == 3:
                    first = c == 3
                    for dt in range(KT):
                        for (b0, wd) in blks:
                            pv = pV[nv % 2]
                            nv += 1
                            for cj in range(4):
                                self.mm(pv.v(0, [(1, wd)]), vbf[cj].v(dt * 128, [(1, 128)]), WT[cj].v(b0, [(1, wd)]), cj == 0, cj == 3,
                                        [vbf[cj].name, WT[cj].name], [pv.name])
                            av = acc.v(dt * ne + b0, [(1, wd)])
                            if first:
                                self.cp(av, pv.v(0, [(1, wd)]), [pv.name], [acc.name])
                            else:
                                self.tt(av, pv.v(0, [(1, wd)]), av, ALU.add, [pv.name, acc.name], [acc.name])
                    if last:
                        for dt in range(KT):
                            for cj in range(4):
                                self.mm(pVs.v(dt, [(1, 1)]), vbf[cj].v(dt * 128, [(1, 128)]), WT[cj].v(n, [(1, 1)]), cj == 0, cj == 3,
                                        [vbf[cj].name, WT[cj].name], [pVs.name])
                        av = acc.v(n, [(ne, KT)])
                        if first:
                            self.cp(av, pVs.v(0, [(1, KT)]), [pVs.name], [acc.name])
                        else:
                            self.tt(av, pVs.v(0, [(1, KT)]), av, ALU.add, [pVs.name, acc.name], [acc.name])
            xtv = self.xT.rearrange("k p t -> p k t")
            for k in range(KT):
                xb = ufp[k % 2]
                P.dma(xb.v(0, [(1, n)]), xtv[:, k, t0:t0 + n], ["xT"], [xb.name])
                self.stt(xb.v(0, [(1, n)]), acc.v(k * ne, [(1, n)]), self.modT.v(5 * 32 + 2 * k, [(1, 1)]), xb.v(0, [(1, n)]), ALU.mult, ALU.add,
                         [acc.name, self.modT.name, xb.name], [xb.name])
                P.dma(xtv[:, k, t0:t0 + n], xb.v(0, [(1, n)]), [xb.name], ["xT"], q="pool", join=True)
            if last:
                xs = vfp[0]
                P.dma(xs.v(0, [(1, KT), (1, 1)]), xtv[:, :, T:T + 1], ["xT"], [xs.name])
                tmpg = vfp[1]
                self.tt(tmpg.v(0, [(1, KT)]), acc.v(n, [(ne, KT)]), self.modT.v(5 * 32 + 1, [(2, KT)]), ALU.mult, [acc.name, self.modT.name], [tmpg.name])
                self.tt(xs.v(0, [(1, KT)]), xs.v(0, [(1, KT)]), tmpg.v(0, [(1, KT)]), ALU.add, [tmpg.name, xs.name], [xs.name])
                P.dma(xtv[:, :, T:T + 1], xs.v(0, [(1, KT), (1, 1)]), [xs.name], ["xT"], q="pool", join=True)
            self.stage_end()

    def st_dil(self, hT):
        P, T, TT = self.P, self.T, self.TT
        DILS = (1, 4, 16)
        WIN = (128, 512, 2048)
        w_in = self.din("dil_w_in", [1, D, 9216])[0]
        w_out = self.din("dil_w_out", [1, 1024, D])[0]
        caches = [self.din("cache_dil_kv%d" % g, [1, WIN[g], 2, 8, 128])[0] for g in range(3)]
        kvp = [self.dout("kv%d_p" % g, [1, min(WIN[g], T), 2, 8, 128])[0] for g in range(3)]
        kvs = [self.dout("kv%d_s" % g, [1, WIN[g], 2, 8, 128])[0] for g in range(3)]
        winv = w_in.rearrange("(k p) f -> p k f", p=128)
        scale = 128 ** -0.5
        NEG = -1.0e30
        for g in range(3):
            Wg = WIN[g]
            nsp = 4 if Wg > 512 else 1
            for q_ in range(nsp):
                a = 1 + (Wg - 1) * q_ // nsp
                b = 1 + (Wg - 1) * (q_ + 1) // nsp
                P.dma(kvs[g][a - 1:b - 1].rearrange("w a h d -> w (a h d)"), caches[g][a:b].rearrange("w a h d -> w (a h d)"), (), ["kvs%d" % g],
                      q="pool", join=True)
        D2 = self.ar.f32(256)
        M2 = self.ar.f32(256)
        B2 = self.ar.f32(256)
        bS = self.ar.f32(1)
        bSh = self.ar.f32(1)
        ti = self.ar.f32(256)
        tii = Buf(ti.ap.bitcast(I32), ti.name)
        P.op("pool", lambda e: e.iota(tii.v(0, [(1, 128)]), [[1, 128]], base=128, channel_multiplier=-1), (), [ti.name])
        P.op("pool", lambda e: e.iota(tii.v(128, [(1, 128)]), [[1, 128]], base=0, channel_multiplier=-1), (), [ti.name])
        self.cp(D2.ap, tii.ap, [ti.name], [D2.name])
        self.ts(M2.v(0, [(1, 128)]), D2.v(0, [(1, 128)]), 128.0, ALU.is_gt, [D2.name], [M2.name], s2=NEG, op1=ALU.mult)
        self.ts(M2.v(128, [(1, 128)]), D2.v(128, [(1, 128)]), 0.0, ALU.is_lt, [D2.name], [M2.name], s2=NEG, op1=ALU.mult)
        P.op("pool", lambda e: e.iota(tii.v(0, [(1, 1)]), [[1, 1]], base=-128, channel_multiplier=1), (), [ti.name])
        self.cp(bS.ap, tii.v(0, [(1, 1)]), [ti.name], [bS.name])
        hs = self.ar.bf16(KT * TT)
        P.dma(hs.v(0, [(TT, KT), (1, TT)]), hT.rearrange("k p t -> p k t"), [hT.tensor.name], [hs.name])
        oT = self.ar.bf16(8 * TT)
        accO = self.ar.f32(T)
        accD = self.ar.f32(T)
        qT = self.ar.bf16(T)
        kT = self.ar.bf16(T)
        Vh = self.ar.bf16(16 * 128)
        wst = [self.ar.f32(D) for _ in range(2)]
        wbf = [self.ar.bf16(D) for _ in range(3)]
        PT = [self.ar.bf16(256) for _ in range(2)]
        tmpS = [self.ar.f32(256) for _ in range(2)]
        rows = [self.ar.f32(128) for _ in range(3)]
        vcol = self.ar.f32(1)
        Kc = self.ar.f32(128)
        Vc = self.ar.f32(128)
        prod = self.ar.f32(128)
        sS = self.ar.f32(2)
        pS_ = self.ar.f32(2)
        oS = self.ar.f32(2)
        tS = self.ar.f32(2)
        nld = 0
        for h in range(8):
            for g in range(3):
                dil = DILS[g]
                L = T // dil
                bpr = L // 128
                slope = 2.0 ** (-8.0 * (g * 8 + h + 1) / 24.0)
                for s_ in range(3):
                    col = s_ * 3072 + g * 1024 + h * 128
                    st = wst[nld % 2]
                    P.dma(st.v(0, [(128, KT), (1, 128)]), winv[:, :, col:col + 128], (), [st.name], q="sp" if nld % 2 else "pool")
                    self.cp(wbf[s_].ap, st.ap, [st.name], [wbf[s_].name], eng="pool")
                    nld += 1
                for (dst, wi, sc_) in ((qT, 0, scale), (kT, 1, 1.0)):
                    for bi, (t0, w) in enumerate(blocks_of(T)[:-1]):
                        pb = self.pb[bi % 2]
                        for k in range(KT):
                            self.mm(pb.v(0, [(1, w)]), wbf[wi].v(k * 128, [(1, 128)]), hs.v(k * TT + t0, [(1, w)]), k == 0, k == KT - 1,
                                    [wbf[wi].name, hs.name], [pb.name])
                        self.act(dst.v(t0 // dil, [(L, dil), (1, w // dil)]), pb.v(0, [(1, dil), (dil, w // dil)]), AF.Copy, [pb.name], [dst.name],
                                 scale=sc_)
                for jb in range(16):
                    r_ = (jb * 128) // L
                    i0 = (jb * 128) % L
                    pb = self.pb[7]
                    for k in range(KT):
                        self.mm(pb.v(0, [(1, 128)]), hs.v(k * TT + i0 * dil + r_, [(dil, 128)]), wbf[2].v(k * 128, [(1, 128)]), k == 0, k == KT - 1,
                                [wbf[2].name, hs.name], [pb.name])
                    self.cp(Vh.v(jb * 128, [(1, 128)]), pb.v(0, [(1, 128)]), [pb.name], [Vh.name])
                self.stt(B2.ap, D2.ap, -slope * dil, M2.ap, ALU.mult, ALU.add, [D2.name, M2.name], [B2.name])
                for jb in range(16):
                    hasprev = (jb % bpr) != 0
                    ks = [jb - 1, jb] if hasprev else [jb]
                    o0 = 0 if hasprev else 128
                    nk = 128 * len(ks)
                    pSb = self.pb[2]
                    s2 = jb % 2
                    for x, kb in enumerate(ks):
                        self.mm(pSb.v(o0 + x * 128, [(1, 128)]), kT.v(kb * 128, [(1, 128)]), qT.v(jb * 128, [(1, 128)]), True, True,
                                [kT.name, qT.name], [pSb.name])
                    self.tt(tmpS[s2].v(o0, [(1, nk)]), pSb.v(o0, [(1, nk)]), B2.v(o0, [(1, nk)]), ALU.add, [pSb.name, B2.name], [tmpS[s2].name])
                    self.act(PT[s2].v(o0, [(1, nk)]), tmpS[s2].v(o0, [(1, nk)]), AF.Exp, [tmpS[s2].name], [PT[s2].name])
                    pO, pD = self.pb[3 + 2 * (jb % 2)], self.pb[4 + 2 * (jb % 2)]
                    for x, kb in enumerate(ks):
                        self.mm(pO.v(0, [(1, 128)]), Vh.v(kb * 128, [(1, 128)]), PT[s2].v(o0 + x * 128, [(1, 128)]), x == 0, x == len(ks) - 1,
                                [Vh.name, PT[s2].name], [pO.name])
                    for x, kb in enumerate(ks):
                        self.mm(pD.v(0, [(1, 128)]), self.onesb.ap, PT[s2].v(o0 + x * 128, [(1, 128)]), x == 0, x == len(ks) - 1,
                                [self.onesb.name, PT[s2].name], [pD.name])
                    r_ = (jb * 128) // L
                    i0 = (jb * 128) % L
                    for (acc_, pb_) in ((accO, pO), (accD, pD)):
                        av = acc_.v(i0 * dil + r_, [(dil, 128)])
                        if g == 0:
                            self.cp(av, pb_.v(0, [(1, 128)]), [pb_.name], [acc_.name], eng="act" if acc_ is accD else "dve")
                        else:
                            self.tt(av, pb_.v(0, [(1, 128)]), av, ALU.add, [pb_.name, acc_.name], [acc_.name])
                pr = self.pb[7]
                for s_ in range(3):
                    for k in range(KT):
                        self.mm(pr.v(s_ * 128, [(1, 128)]), hs.v(k * TT + T, [(0, 128)]), wbf[s_].v(k * 128, [(1, 128)]), k == 0, k == KT - 1,
                                [wbf[s_].name, hs.name], [pr.name])
                    self.cp(rows[s_].ap, pr.v(s_ * 128, [(1, 128)]), [pr.name], [rows[s_].name])
                for k in range(KT):
                    self.mm(pr.v(384, [(1, 1)]), wbf[2].v(k * 128, [(1, 128)]), hs.v(k * TT + T, [(1, 1)]), k == 0, k == KT - 1,
                            [wbf[2].name, hs.name], [pr.name])
                self.cp(vcol.ap, pr.v(384, [(1, 1)]), [pr.name], [vcol.name])
                Wg = WIN[g]
                P.dma(kvs[g][Wg - 1:Wg, 0, h, :], rows[1].v(0, [(1, 128)], 0, 1), [rows[1].name], ["kvs%d" % g], q="pool", join=True)
                P.dma(kvs[g][Wg - 1:Wg, 1, h, :], rows[2].v(0, [(1, 128)], 0, 1), [rows[2].name], ["kvs%d" % g], q="pool", join=True)
                cv = caches[g].rearrange("(i s) a h d -> i s a h d", s=dil)
                P.dma(Kc.ap, cv[:, 0, 0, h, :], (), [Kc.name])
                P.dma(Vc.ap, cv[:, 0, 1, h, :], (), [Vc.name])
                self.tt(prod.ap, Kc.ap, rows[0].ap, ALU.mult, [Kc.name, rows[0].name], [prod.name])
                P.op("dve", lambda e, o=sS.v(0, [(1, 1)]), i=prod.ap: e.tensor_reduce(o, i, AX.X, ALU.add), [prod.name], [sS.name])
                self.tt(prod.ap, rows[1].ap, rows[0].ap, ALU.mult, [rows[1].name, rows[0].name, sS.name], [prod.name])
                P.op("dve", lambda e, o=sS.v(1, [(1, 1)]), i=prod.ap: e.tensor_reduce(o, i, AX.X, ALU.add), [prod.name], [sS.name])
                self.ts(bSh.ap, bS.ap, slope * dil, ALU.mult, [bS.name], [bSh.name])
                self.act(pS_.v(0, [(1, 1)]), sS.v(0, [(1, 1)]), AF.Exp, [sS.name, bSh.name], [pS_.name], bias=bSh.ap, scale=scale)
                self.act(pS_.v(1, [(1, 1)]), sS.v(1, [(1, 1)]), AF.Exp, [sS.name], [pS_.name], scale=scale)
                po = self.pb[2]
                self.mm(po.v(0, [(1, 1)]), Vc.ap, pS_.v(0, [(1, 1)]), True, True, [Vc.name, pS_.name], [po.name])
                self.mm(po.v(1, [(1, 1)]), self.onesf.ap, pS_.v(0, [(1, 1)]), True, True, [self.onesf.name, pS_.name], [po.name])
                self.stt(tS.v(0, [(1, 1)]), vcol.ap, pS_.v(1, [(1, 1)]), po.v(0, [(1, 1)]), ALU.mult, ALU.add, [vcol.name, pS_.name, po.name], [tS.name])
                self.tt(tS.v(1, [(1, 1)]), po.v(1, [(1, 1)]), pS_.v(1, [(1, 1)]), ALU.add, [po.name, pS_.name, tS.name], [tS.name])
                if g == 0:
                    self.cp(oS.ap, tS.ap, [tS.name], [oS.name])
                else:
                    self.tt(oS.ap, oS.ap, tS.ap, ALU.add, [tS.name, oS.name], [oS.name])
            P.op("dve", lambda e, o=accD.ap: e.reciprocal(o, o), [accD.name], [accD.name])
            self.tt(oT.v(h * TT, [(1, T)]), accO.ap, accD.ap, ALU.mult, [accO.name, accD.name], [oT.name])
            P.op("dve", lambda e, o=oS.v(1, [(1, 1)]): e.reciprocal(o, o), [oS.name], [oS.name])
            self.tt(oT.v(h * TT + T, [(1, 1)]), oS.v(0, [(1, 1)]), oS.v(1, [(1, 1)]), ALU.mult, [oS.name], [oT.name])
        xtv = self.xT.rearrange("k p t -> p k t")
        wov = w_out.rearrange("(e p) f -> p e f", p=128)
        for ft in range(KT):
            st = wst[ft % 2]
            wb = wbf[ft % 2]
            xb = [accO, accD][ft % 2]
            xs_ = tmpS[ft % 2]
            P.dma(st.v(0, [(128, 8), (1, 128)]), wov[:, :, ft * 128:(ft + 1) * 128], (), [st.name])
            self.cp(wb.v(0, [(1, 1024)]), st.v(0, [(1, 1024)]), [st.name], [wb.name], eng="pool")
            P.dma(xb.ap, xtv[:, ft, 0:T], ["xT"], [xb.name], q="pool")
            P.dma(xs_.v(0, [(1, 1)]), xtv[:, ft, T:T + 1], ["xT"], [xs_.name], q="pool")
            for bi, (t0, w) in enumerate(blocks_of(T)):
                pb = self.pb[bi % 2]
                col = 1 if t0 == T else 0
                for e_ in range(8):
                    self.mm(pb.v(0, [(1, w)]), wb.v(e_ * 128, [(1, 128)]), oT.v(e_ * TT + t0, [(1, w)]), e_ == 0, e_ == 7, [wb.name, oT.name], [pb.name])
                if col == 0:
                    xv = xb.v(t0, [(1, w)])
                    self.stt(xv, pb.v(0, [(1, w)]), self.modT.v(2 * 32 + 2 * ft, [(1, 1)]), xv, ALU.mult, ALU.add, [pb.name, self.modT.name, xb.name], [xb.name])
                else:
                    xv = xs_.v(0, [(1, 1)])
                    self.stt(xv, pb.v(0, [(1, 1)]), self.modT.v(2 * 32 + 2 * ft + 1, [(1, 1)]), xv, ALU.mult, ALU.add, [pb.name, self.modT.name, xs_.name], [xs_.name])
            P.dma(xtv[:, ft, 0:T], xb.ap, [xb.name], ["xT"], q="pool", join=True)
            P.dma(xtv[:, ft, T:T + 1], xs_.v(0, [(1, 1)]), [xs_.name], ["xT"], q="pool", join=True)
        self.stage_end()
        hs = self.ar.bf16(KT * TT)
        P.dma(hs.v(0, [(TT, KT), (1, TT)]), hT.rearrange("k p t -> p k t"), [hT.tensor.name], [hs.name])
        wst = [self.ar.f32(D) for _ in range(2)]
        self.kvw = self.ar.bf16(KT * 512)
        self.kvo = [self.ar.f32(512) for _ in range(2)]
        nrot = 0
        for g in range(3):
            nl = min(WIN[g], T)
            for s_ in (1, 2):
                for half in range(2):
                    col = s_ * 3072 + g * 1024 + half * 512
                    for piece in range(4):
                        stp = wst[nrot % 2]
                        nrot += 1
                        P.dma(stp.v(0, [(512, 4), (1, 512)]), winv[:, piece * 4:(piece + 1) * 4, col:col + 512], (), [stp.name])
                        self.cp(self.kvw.v(piece * 2048, [(1, 2048)]), stp.ap, [stp.name], [self.kvw.name], eng="pool")
                    for ti_ in range(nl // 128):
                        tok0 = T - nl + ti_ * 128
                        pb = self.pb[ti_ % 2]
                        for k in range(KT):
                            self.mm(pb.ap, hs.v(k * TT + tok0, [(1, 128)]), self.kvw.v(k * 512, [(1, 512)]), k == 0, k == KT - 1,
                                    [hs.name, self.kvw.name], [pb.name])
                        ob = self.kvo[ti_ % 2]
                        self.cp(ob.ap, pb.ap, [pb.name], [ob.name], eng="act" if ti_ % 2 else "dve")
                        P.dma(kvp[g][ti_ * 128:(ti_ + 1) * 128, s_ - 1, half * 4:(half + 1) * 4, :].rearrange("t h d -> t (h d)"), ob.ap,
                              [ob.name], ["kvp%d" % g], q="pool", join=True)
        self.stage_end()

    def st_ret(self, hT):
        P, T, TT = self.P, self.T, self.TT
        w_in = self.din("ret_w_in", [1, D, 12288])[0]
        gn_g = self.din("ret_gn_g", [1, 4096])
        w_out = self.din("ret_w_out", [1, 4096, D])[0]
        st_in = self.din("state_ret", [1, 8, 256, 512])[0]
        ret_p = self.dout("ret_p", [1, 8, 256, 512])[0]
        ret_s = self.dout("ret_s", [1, 8, 256, 512])[0]
        zT = self.scratch("zT", [32, 128, TT], BF16)
        winv = w_in.rearrange("(k p) f -> p k f", p=128)
        NCK = T // 128
        hs = self.ar.bf16(KT * TT)
        P.dma(hs.v(0, [(TT, KT), (1, TT)]), hT.rearrange("k p t -> p k t"), [hT.tensor.name], [hs.name])
        wb = self.ar.bf16(KT * 512)
        wst = self.ar.f32(D)
        qT = self.ar.bf16(2 * T)
        kT = self.ar.bf16(2 * T)
        ktok = self.ar.bf16(NCK * 256)
        vtok = self.ar.bf16(NCK * 512)
        sg = self.ar.bf16((NCK + 1) * 512)
        S = self.ar.f32(1024)
        Sb = self.ar.bf16(1024)
        attTb = self.ar.bf16(128)
        qd = self.ar.bf16(256)
        osb = self.ar.f32(512)
        cen = self.ar.f32(512)
        zb = self.ar.bf16(512)
        zst = self.ar.bf16(512)
        dmaskT = self.ar.f32(128)
        qdecT = self.ar.f32(128)
        Dcl = self.ar.f32(128)
        msk = self.ar.f32(128)
        gng = self.ar.f32(512)
        vrow = self.ar.f32(512)
        cols = self.ar.f32(8)
        stat = self.ar.f32(8)
        ti = self.ar.f32(128)
        tii = Buf(ti.ap.bitcast(I32), ti.name)
        P.op("pool", lambda e: e.iota(tii.ap, [[1, 128]], base=0, channel_multiplier=-1), (), [ti.name])
        self.cp(Dcl.ap, tii.ap, [ti.name], [Dcl.name])
        self.ts(msk.ap, Dcl.ap, 0.0, ALU.is_ge, [Dcl.name], [msk.name])
        self.ts(Dcl.ap, Dcl.ap, 0.0, ALU.max, [Dcl.name, msk.name], [Dcl.name])
        P.op("pool", lambda e: e.iota(tii.v(0, [(1, 1)]), [[1, 1]], base=127, channel_multiplier=-1), (), [ti.name])
        self.cp(cols.v(5, [(1, 1)]), tii.v(0, [(1, 1)]), [ti.name], [cols.name])
        P.op("pool", lambda e: e.iota(tii.ap, [[1, 128]], base=1, channel_multiplier=0), (), [ti.name])
        cp1 = self.ar.f32(128)
        self.cp(cp1.ap, tii.ap, [ti.name], [cp1.name])

        def load_w(col0, ncols):
            per = 2048 // ncols
            for pc in range(KT // per):
                P.dma(wst.v(0, [(ncols, per), (1, ncols)]), winv[:, pc * per:(pc + 1) * per, col0:col0 + ncols], (), [wst.name])
                self.cp(wb.v(pc * per * ncols, [(1, per * ncols)]), wst.v(0, [(1, per * ncols)]), [wst.name], [wb.name], eng="pool")

        for h in range(8):
            gam = 1.0 - 2.0 ** (-5.0 - h)
            lg = math.log(gam)
            cdec = gam ** 128
            self.act(dmaskT.ap, Dcl.ap, AF.Exp, [Dcl.name], [dmaskT.name], scale=lg)
            self.tt(dmaskT.ap, dmaskT.ap, msk.ap, ALU.mult, [dmaskT.name, msk.name], [dmaskT.name])
            self.act(qdecT.ap, cp1.ap, AF.Exp, [cp1.name], [qdecT.name], scale=lg)
            self.act(cols.v(4, [(1, 1)]), cols.v(5, [(1, 1)]), AF.Exp, [cols.name], [cols.name], scale=lg)
            self.ts(cols.v(4, [(1, 1)]), cols.v(4, [(1, 1)]), 1.0 / 16.0, ALU.mult, [cols.name], [cols.name])
            P.dma(gng.ap, gn_g[0:1, h * 512:(h + 1) * 512].to_broadcast([128, 512]), (), [gng.name])
            load_w(h * 256, 256)
            nb = 0
            for dkt in range(2):
                for (t0, w) in blocks_of(T):
                    pb = self.pb[nb % 2]
                    nb += 1
                    for k in range(KT):
                        self.mm(pb.v(0, [(1, w)]), wb.v(k * 256 + dkt * 128, [(1, 128)]), hs.v(k * TT + t0, [(1, w)]), k == 0, k == KT - 1,
                                [wb.name, hs.name], [pb.name])
                    if t0 < T:
                        self.cp(qT.v(dkt * T + t0, [(1, w)]), pb.v(0, [(1, w)]), [pb.name], [qT.name], eng="act")
                    else:
                        self.cp(cols.v(dkt, [(1, 1)]), pb.v(0, [(1, 1)]), [pb.name], [cols.name])
            load_w(2048 + h * 256, 256)
            for dkt in range(2):
                for (t0, w) in blocks_of(T):
                    pb = self.pb[nb % 2]
                    nb += 1
                    for k in range(KT):
                        self.mm(pb.v(0, [(1, w)]), wb.v(k * 256 + dkt * 128, [(1, 128)]), hs.v(k * TT + t0, [(1, w)]), k == 0, k == KT - 1,
                                [wb.name, hs.name], [pb.name])
                    if t0 < T:
                        self.act(kT.v(dkt * T + t0, [(1, w)]), pb.v(0, [(1, w)]), AF.Copy, [pb.name], [kT.name], scale=1.0 / 16.0)
                    else:
                        self.ts(cols.v(2 + dkt, [(1, 1)]), pb.v(0, [(1, 1)]), 1.0 / 16.0, ALU.mult, [pb.name], [cols.name])
            for c in range(NCK):
                pb = self.pb[2 + c % 2]
                for k in range(KT):
                    self.mm(pb.v(0, [(1, 256)]), hs.v(k * TT + c * 128, [(1, 128)]), wb.v(k * 256, [(1, 256)]), k == 0, k == KT - 1,
                            [wb.name, hs.name], [pb.name])
                self.ts(ktok.v(c * 256, [(1, 256)]), pb.v(0, [(1, 256)]), cols.v(4, [(1, 1)]), ALU.mult, [pb.name, cols.name], [ktok.name])
            load_w(4096 + h * 512, 512)
            for c in range(NCK + 1):
                pb = self.pb[2 + c % 2]
                lhs = (lambda k: hs.v(k * TT + c * 128, [(1, 128)])) if c < NCK else (lambda k: hs.v(k * TT + T, [(0, 128)]))
                for k in range(KT):
                    self.mm(pb.ap, lhs(k), wb.v(k * 512, [(1, 512)]), k == 0, k == KT - 1, [wb.name, hs.name], [pb.name])
                if c < NCK:
                    self.cp(vtok.v(c * 512, [(1, 512)]), pb.ap, [pb.name], [vtok.name], eng="act" if c % 2 else "dve")
                else:
                    self.cp(vrow.ap, pb.ap, [pb.name], [vrow.name])
            load_w(8192 + h * 512, 512)
            for c in range(NCK + 1):
                pb = self.pb[2 + c % 2]
                lhs = (lambda k: hs.v(k * TT + c * 128, [(1, 128)])) if c < NCK else (lambda k: hs.v(k * TT + T, [(0, 128)]))
                for k in range(KT):
                    self.mm(pb.ap, lhs(k), wb.v(k * 512, [(1, 512)]), k == 0, k == KT - 1, [wb.name, hs.name], [pb.name])
                self.act(sg.v(c * 512, [(1, 512)]), pb.ap, AF.Silu, [pb.name], [sg.name])
            self.memset(S.ap, 0.0, [S.name])
            self.memset(Sb.ap, 0.0, [Sb.name])

            def post(pO, m, c, tcol):
                pv = lambda b, o=0, n=512: b.v(o, [(1, n)], 0, m)
                sv = lambda j: stat.v(j, [(1, 1)], 0, m)
                self.act(pv(osb), pv(pO), AF.Identity, [pO.name], [osb.name, stat.name], accum_out=sv(0))
                self.ts(sv(1), sv(0), -1.0 / 512.0, ALU.mult, [stat.name], [stat.name])
                self.ts(pv(cen), pv(osb), sv(1), ALU.add, [osb.name, stat.name], [cen.name])
                self.act(pv(osb), pv(cen), AF.Square, [cen.name], [osb.name, stat.name], accum_out=sv(2))
                self.act(sv(3), sv(2), AF.Sqrt, [stat.name], [stat.name], bias=EPS, scale=1.0 / 512.0)
                P.op("dve", lambda e, o=sv(3): e.reciprocal(o, o), [stat.name], [stat.name])
                self.stt(pv(cen), pv(cen), sv(3), pv(gng), ALU.mult, ALU.mult, [cen.name, stat.name, gng.name], [cen.name])
                self.tt(pv(zb), pv(cen), pv(sg, c * 512), ALU.mult, [cen.name, sg.name], [zb.name])
                pt = self.pb[6]
                ptb = Buf(pt.ap.bitcast(BF16), pt.name)
                for et in range(4):
                    self.tr(ptb.v(et * 128, [(1, m)]), zb.v(et * 128, [(1, 128)], 0, m), self.identb.v(0, [(1, m)], 0, m),
                            [zb.name, self.identb.name], [pt.name])
                self.cp(zst.v(0, [(128, 4), (1, m)]), ptb.v(0, [(128, 4), (1, m)]), [pt.name], [zst.name], eng="act")
                P.dma(zT[h * 4:(h + 1) * 4].rearrange("e p t -> p e t")[:, :, tcol:tcol + m], zst.v(0, [(128, 4), (1, m)]), [zst.name], ["zT"],
                      q="pool", join=True)

            for c in range(NCK):
                pA = self.pb[0]
                for dkt in range(2):
                    self.mm(pA.v(0, [(1, 128)]), kT.v(dkt * T + c * 128, [(1, 128)]), qT.v(dkt * T + c * 128, [(1, 128)]), dkt == 0, dkt == 1,
                            [kT.name, qT.name], [pA.name])
                self.tt(attTb.ap, pA.v(0, [(1, 128)]), dmaskT.ap, ALU.mult, [pA.name, dmaskT.name], [attTb.name])
                self.tt(qd.v(0, [(128, 2), (1, 128)]), qT.v(c * 128, [(T, 2), (1, 128)]), qdecT.v(0, [(0, 2), (1, 128)]), ALU.mult,
                        [qT.name, qdecT.name], [qd.name])
                pO = self.pb[4 + c % 2]
                self.mm(pO.ap, attTb.ap, vtok.v(c * 512, [(1, 512)]), True, False, [attTb.name, vtok.name], [pO.name])
                for dkt in range(2):
                    self.mm(pO.ap, qd.v(dkt * 128, [(1, 128)]), Sb.v(dkt * 512, [(1, 512)]), False, dkt == 1, [qd.name, Sb.name], [pO.name])
                for dkt in range(2):
                    pSt = self.pb[2 + dkt]
                    self.mm(pSt.ap, ktok.v(c * 256 + dkt * 128, [(1, 128)]), vtok.v(c * 512, [(1, 512)]), True, True, [ktok.name, vtok.name], [pSt.name])
                    sv_ = S.v(dkt * 512, [(1, 512)])
                    self.stt(sv_, sv_, cdec, pSt.ap, ALU.mult, ALU.add, [S.name, pSt.name], [S.name])
                self.cp(Sb.ap, S.ap, [S.name], [Sb.name], eng="pool")
                post(pO, 128, c, c * 128)
            P.dma(ret_p[h].rearrange("(t p) e -> p t e", p=128), S.v(0, [(512, 2), (1, 512)]), [S.name], ["ret_p"], q="pool", join=True)
            P.dma(S.v(0, [(512, 2), (1, 512)]), st_in[h].rearrange("(t p) e -> p t e", p=128), (), [S.name])
            pO = self.pb[4]
            for dkt in range(2):
                self.mm(pO.v(0, [(1, 512)], 0, 1), cols.v(dkt, [(1, 1)]), S.v(dkt * 512, [(1, 512)]), dkt == 0, dkt == 1, [cols.name, S.name], [pO.name])
            pq = self.pb[5]
            for dkt in range(2):
                self.mm(pq.v(0, [(1, 1)], 0, 1), cols.v(dkt, [(1, 1)]), cols.v(2 + dkt, [(1, 1)]), dkt == 0, dkt == 1, [cols.name], [pq.name])
            self.cp(stat.v(4, [(1, 1)], 0, 1), pq.v(0, [(1, 1)], 0, 1), [pq.name], [stat.name])
            orow = self.pb[7]
            self.ts(cen.v(0, [(1, 512)], 0, 1), vrow.v(0, [(1, 512)], 0, 1), stat.v(4, [(1, 1)], 0, 1), ALU.mult, [vrow.name, stat.name], [cen.name])
            self.stt(osb.v(0, [(1, 512)], 0, 1), pO.v(0, [(1, 512)], 0, 1), gam, cen.v(0, [(1, 512)], 0, 1), ALU.mult, ALU.add,
                     [pO.name, cen.name], [osb.name])
            for dkt in range(2):
                sv_ = S.v(dkt * 512, [(1, 512)])
                self.ts(sv_, sv_, gam, ALU.mult, [S.name, pO.name], [S.name])
                self.stt(sv_, vrow.ap, cols.v(2 + dkt, [(1, 1)]), sv_, ALU.mult, ALU.add, [vrow.name, cols.name, S.name], [S.name])
            P.dma(ret_s[h].rearrange("(t p) e -> p t e", p=128), S.v(0, [(512, 2), (1, 512)]), [S.name], ["ret_s"], q="pool", join=True)
            post(osb, 1, NCK, T)
        self.stage_end()
        xtv = self.xT.rearrange("k p t -> p k t")
        wov = w_out.rearrange("(e p) f -> p e f", p=128)
        zb_ = self.ar.bf16(32 * 512)
        wst = [self.ar.f32(32 * 128) for _ in range(2)]
        wbo = [self.ar.bf16(32 * 128) for _ in range(2)]
        xb = [self.ar.f32(512) for _ in range(2)]
        for (t0, w) in blocks_of(T):
            col = 1 if t0 == T else 0
            P.dma(zb_.v(0, [(512, 32), (1, w)]), zT.rearrange("e p t -> p e t")[:, :, t0:t0 + w], ["zT"], [zb_.name])
            for ft in range(KT):
                s2 = ft % 2
                P.dma(wst[s2].v(0, [(128, 32), (1, 128)]), wov[:, :, ft * 128:(ft + 1) * 128], (), [wst[s2].name])
                self.cp(wbo[s2].ap, wst[s2].ap, [wst[s2].name], [wbo[s2].name], eng="pool")
                P.dma(xb[s2].v(0, [(1, w)]), xtv[:, ft, t0:t0 + w], ["xT"], [xb[s2].name], q="pool")
                pb = self.pb[s2]
                for e_ in range(32):
                    self.mm(pb.v(0, [(1, w)]), wbo[s2].v(e_ * 128, [(1, 128)]), zb_.v(e_ * 512, [(1, w)]), e_ == 0, e_ == 31, [wbo[s2].name, zb_.name], [pb.name])
                xv = xb[s2].v(0, [(1, w)])
                self.stt(xv, pb.v(0, [(1, w)]), self.modT.v(2 * 32 + 2 * ft + col, [(1, 1)]), xv, ALU.mult, ALU.add, [pb.name, self.modT.name, xb[s2].name], [xb[s2].name])
                P.dma(xtv[:, ft, t0:t0 + w], xv, [xb[s2].name], ["xT"], q="pool", join=True)
        self.stage_end()


def build(T=2048, layers=4):
    k = K(T)
    k.setup_consts()
    k.stage_end()
    k.st_loadx()
    k.st_mod_setup()
    hT = k.scratch("hT", [KT, 128, k.TT], BF16)
    h32T = k.scratch("h32T", [KT, 128, k.TT])
    for i in range(layers):
        kind, j = i % 3, i // 3
        k.st_mod(i)
        if kind == 0:
            k.st_norm(k.A1, 0, None, h32T)
            k.st_pool(j, h32T)
        elif kind == 1:
            k.st_norm(k.A1, 0, hT)
            k.st_dil(hT)
        else:
            k.st_norm(k.A1, 0, hT)
            k.st_ret(hT)
        k.st_norm(k.A2, 3, hT)
        k.st_peer_route(i, hT)
        k.st_peer_main(i, hT)
    k.st_final()
    nc = k.P.emit()
    return k, nc


_CACHE = {}


def kernel(x_prompt, x_sample, state_pool, cache_dil_kv0, cache_dil_kv1, cache_dil_kv2, state_ret,
           c_prompt, c_sample, norm1_g, norm2_g, mod_w, mod_b, pool_w, pool_scale,
           dil_w_in, dil_w_out, ret_w_in, ret_gn_g, ret_w_out,
           peer_w_q, peer_keys, peer_u, peer_v, final_g):
    f = lambda a: np.ascontiguousarray(np.asarray(a), dtype=np.float32)
    T = x_prompt.shape[1]
    k, nc = build(T)
    shared = dict(mod_w=f(mod_w), mod_b=f(mod_b), norm1_g=f(norm1_g), norm2_g=f(norm2_g), pool_w=f(pool_w), pool_scale=f(pool_scale),
                  dil_w_in=f(dil_w_in), dil_w_out=f(dil_w_out), ret_w_in=f(ret_w_in), ret_gn_g=f(ret_gn_g), ret_w_out=f(ret_w_out),
                  peer_w_q=f(peer_w_q), peer_keys=f(peer_keys), peer_u=f(peer_u), peer_v=f(peer_v), final_g=f(final_g))
    caches = [f(cache_dil_kv0), f(cache_dil_kv1), f(cache_dil_kv2)]
    x_prompt, x_sample, state_pool, state_ret = f(x_prompt), f(x_sample), f(state_pool), f(state_ret)
    c_prompt, c_sample = f(c_prompt), f(c_sample)
    in_maps = []
    for ci in range(8):
        b, s = ci // 2, ci
        m = dict(shared)
        m.update(x_p=x_prompt[b], x_s=x_sample[s], c_p=c_prompt[b], c_s=c_sample[s],
                 state_pool=np.ascontiguousarray(state_pool[:, s]), state_ret=np.ascontiguousarray(state_ret[:, s]))
        for g in range(3):
            m["cache_dil_kv%d" % g] = np.ascontiguousarray(caches[g][:, s])
        in_maps.append({n: m[n] for n in k.inp})
    res = run_bass_kernel_spmd(nc, in_maps, core_ids=list(range(8)))
    R = res.results
    B = x_prompt.shape[0]
    half = T // 2

    def prompt(name):
        return np.stack([R[2 * b][name] for b in range(B)], axis=0)

    y_prompt = np.stack([np.concatenate([R[2 * b]["y_p"][:half], R[2 * b + 1]["y_p"][half:]], axis=0) for b in range(B)], axis=0)
    y_sample = np.stack([R[s]["y_s"] for s in range(8)], axis=0)
    pool_p = np.stack([R[2 * b]["pool_p"] for b in range(B)], axis=1)
    pool_s = np.stack([R[s]["pool_s"] for s in range(8)], axis=1)
    outs = [y_prompt, y_sample, pool_p, pool_s]
    for g in range(3):
        outs.append(np.stack([R[2 * b + (g % 2)]["kv%d_p" % g] for b in range(B)], axis=1))
        outs.append(np.stack([R[s]["kv%d_s" % g] for s in range(8)], axis=1))
    outs.append(np.stack([R[2 * b + 1]["ret_p"] for b in range(B)], axis=1))
    outs.append(np.stack([R[s]["ret_s"] for s in range(8)], axis=1))
    return tuple(np.ascontiguousarray(o, dtype=np.float32) for o in outs)
```

```python
from contextlib import ExitStack
import math
import numpy as np
import concourse.bass as bass
import concourse.mybir as mybir
from concourse.ap import AP
from concourse.bass_utils import run_bass_kernel_spmd

F32 = mybir.dt.float32
BF16 = mybir.dt.bfloat16
U32 = mybir.dt.uint32
I32 = mybir.dt.int32
AF = mybir.ActivationFunctionType
ALU = mybir.AluOpType
AX = mybir.AxisListType

ENGS = ("pe", "dve", "act", "pool", "sp")
DMAQ = ("sp", "pool")
NDMA = 24
D = 2048
KT = 16
EPS = 1e-6


class Op:
    __slots__ = ("eng", "fn", "deps", "dma", "tok", "need", "pre")


class Prog:
    def __init__(self):
        self.nc = bass.Bass("TRN2", target_bir_lowering=False)
        self.es = ExitStack()
        self.ops = []
        self.lastw = {}
        self.readers = {}
        self.last_eng = {}
        self.recent_dma = {q: [] for q in DMAQ}
        self.uid = 0

    def sb(self, name, shape, dt=F32):
        return self.es.enter_context(self.nc.sbuf_tensor(name, list(shape), dt))

    def ps(self, name, shape, dt=F32):
        return self.es.enter_context(self.nc.psum_tensor(name, list(shape), dt))

    def dram(self, name, shape, dt=F32, kind="Internal"):
        return self.nc.dram_tensor(name, list(shape), dt, kind=kind).ap()

    def op(self, eng, fn, r=(), w=(), dma=False, join=False, extra=()):
        o = Op()
        o.eng, o.fn, o.dma, o.need, o.tok, o.pre = eng, fn, dma, False, None, None
        deps = set(extra)
        for x in r:
            deps.update(self.lastw.get(x, ()))
        for x in w:
            deps.update(self.lastw.get(x, ()))
            deps.update(self.readers.get(x, ()))
        if eng == "pe":
            deps = {d for d in deps if d.eng != "pe"}
        o.deps = deps
        for d in deps:
            d.need = True
        for x in r:
            lst = self.readers.setdefault(x, [])
            if not dma:
                lst[:] = [q for q in lst if q.dma or q.eng != eng]
            lst.append(o)
        for x in w:
            if join and x in self.lastw:
                self.lastw[x].append(o)
            else:
                self.lastw[x] = [o]
            self.readers[x] = []
        self.ops.append(o)
        if dma:
            self.recent_dma[eng].append(o)
            if len(self.recent_dma[eng]) > NDMA:
                self.recent_dma[eng].pop(0)
        else:
            self.last_eng[eng] = o
        return o

    def dma(self, out, in_, r=(), w=(), q="sp", join=False, **kw):
        kw.setdefault("allow_slow_non_contiguous", True)
        return self.op(q, lambda e: e.dma_start(out=out, in_=in_, **kw), r, w, dma=True, join=join)

    def barrier(self):
        deps = list(self.last_eng.values())
        for q in DMAQ:
            deps += self.recent_dma[q]
        for e in ENGS:
            self.op(e, lambda h: h.nop(), extra=deps)
        self.lastw = {}
        self.readers = {}

    def emit(self):
        nc = self.nc
        es = self.es
        sems = {e: es.enter_context(nc.semaphore("s_" + e)) for e in ENGS}
        dsem = {q: [es.enter_context(nc.semaphore("d_%s%d" % (q, i))) for i in range(NDMA)] for q in DMAQ}
        cnt = {e: 0 for e in ENGS}
        dcnt = {q: 0 for q in DMAQ}
        duse = {q: [0] * NDMA for q in DMAQ}
        for o in self.ops:
            if o.dma:
                i = dcnt[o.eng] % NDMA
                dcnt[o.eng] += 1
                prev = duse[o.eng][i]
                duse[o.eng][i] += 16
                o.pre = (dsem[o.eng][i], prev) if prev else None
                o.tok = (dsem[o.eng][i], prev + 16)
            elif o.need:
                cnt[o.eng] += 1
                o.tok = (sems[o.eng], cnt[o.eng])
        self.sem_counts = dict(cnt)
        per = {e: [o for o in self.ops if o.eng == e] for e in ENGS}
        block = es.enter_context(nc.Block())

        def run(engname, handle):
            known = {}
            for o in per[engname]:
                ws = []
                if o.pre is not None:
                    ws.append(o.pre)
                for d in o.deps:
                    ws.append(d.tok)
                for (s, v) in ws:
                    k = id(s)
                    if known.get(k, 0) >= v:
                        continue
                    known[k] = v
                    handle.wait_ge(s, v)
                inst = o.fn(handle)
                if o.tok is not None:
                    inst.then_inc(o.tok[0], 16 if o.dma else 1)
            if engname in dsem:
                for i in range(NDMA):
                    if duse[engname][i]:
                        handle.wait_ge(dsem[engname][i], duse[engname][i])

        @block.tensor
        def _(e):
            run("pe", e)

        @block.vector
        def _(e):
            run("dve", e)

        @block.scalar
        def _(e):
            run("act", e)

        @block.gpsimd
        def _(e):
            run("pool", e)

        @block.sync
        def _(e):
            run("sp", e)

        es.close()
        return nc


class Buf:
    def __init__(self, ap2d, name):
        self.ap = ap2d
        self.name = name
        self.t = ap2d.tensor
        self.off = ap2d.offset
        self.ps = ap2d.ap[0][0]
        self.n = ap2d.shape[1]

    def v(self, off=0, dims=None, p0=0, npart=128):
        if dims is None:
            dims = [(1, self.n - off)]
        return AP(self.t, self.off + p0 * self.ps + off, [[self.ps, npart]] + [[s, n] for (s, n) in dims])

    def __getitem__(self, k):
        return self.ap[k]


class Arena:
    def __init__(self, P, name, words):
        self.P = P
        self.t = P.sb(name, [128, words], F32)
        self.words = words
        self.off = 0
        self.gen = 0
        self.name = name

    def reset(self):
        self.off = 0
        self.gen += 1

    def f32(self, n, tag=""):
        assert self.off + n <= self.words, ("arena overflow", self.name, self.off, n, self.words)
        b = Buf(self.t[:, self.off:self.off + n], "%s_%d" % (self.name, self.off))
        self.off += n
        return b

    def bf16(self, n, tag=""):
        nw = (n + 1) // 2
        assert self.off + nw <= self.words, ("arena overflow", self.name, self.off, nw, self.words)
        b = Buf(self.t[:, self.off:self.off + nw].bitcast(BF16), "%s_%d" % (self.name, self.off))
        self.off += nw
        return b

    def u32(self, n, tag=""):
        assert self.off + n <= self.words
        b = Buf(self.t[:, self.off:self.off + n].bitcast(U32), "%s_%d" % (self.name, self.off))
        self.off += n
        return b


def blocks_of(T):
    bl = [(t0, min(512, T - t0)) for t0 in range(0, T, 512)]
    return bl + [(T, 1)]


class K:
    def __init__(self, T, dbg=()):
        self.T = T
        self.TT = T + 1
        self.P = Prog()
        self.dbg = set(dbg)
        P = self.P
        nc = P.nc
        self.inp = {}
        self.out = {}
        self.ar = Arena(P, "ar", 45 * 1024)
        self.cst = Arena(P, "cst", 2 * 1024)
        self.pb = [Buf(P.ps("pb%d" % i, [128, 512], F32)[:, :], "pb%d" % i) for i in range(8)]

    def din(self, name, shape, dt=F32):
        a = self.P.nc.dram_tensor(name, list(shape), dt, kind="ExternalInput").ap()
        self.inp[name] = a
        return a

    def dout(self, name, shape, dt=F32):
        a = self.P.nc.dram_tensor(name, list(shape), dt, kind="ExternalOutput").ap()
        self.out[name] = a
        return a

    def scratch(self, name, shape, dt=F32):
        if name in self.dbg:
            return self.dout(name, shape, dt)
        return self.P.dram(name, shape, dt)

    def mm(self, out, lhsT, rhs, start, stop, r, w):
        self.P.op("pe", lambda e: e.matmul(out, lhsT, rhs, start=start, stop=stop), r, w)

    def tr(self, out, in_, ident, r, w):
        self.P.op("pe", lambda e: e.transpose(out, in_, ident), r, w)

    def act(self, out, in_, func, r, w, bias=0.0, scale=1.0, accum_out=None):
        if accum_out is None:
            self.P.op("act", lambda e: e.activation(out, in_, func, bias=bias, scale=scale), r, w)
        else:
            self.P.op("act", lambda e: e.activation(out, in_, func, bias=bias, scale=scale, accum_out=accum_out), r, w)

    def tt(self, out, in0, in1, op, r, w, eng="dve"):
        self.P.op(eng, lambda e: e.tensor_tensor(out, in0, in1, op), r, w)

    def ts(self, out, in0, s1, op0, r, w, s2=None, op1=None, eng="dve"):
        if op1 is None:
            self.P.op(eng, lambda e: e.tensor_scalar(out, in0, s1, None, op0), r, w)
        else:
            self.P.op(eng, lambda e: e.tensor_scalar(out, in0, s1, s2, op0, op1), r, w)

    def stt(self, out, in0, scalar, in1, op0, op1, r, w):
        self.P.op("dve", lambda e: e.scalar_tensor_tensor(out, in0, scalar, in1, op0, op1), r, w)

    def cp(self, out, in_, r, w, eng="dve"):
        if eng == "act":
            self.P.op("act", lambda e: e.copy(out, in_), r, w)
        else:
            self.P.op(eng, lambda e: e.tensor_copy(out, in_), r, w)

    def memset(self, ap, val, w, eng="dve"):
        self.P.op(eng, lambda e: e.memset(ap, val), (), w)

    def stage_end(self):
        self.P.barrier()
        self.ar.reset()

    def setup_consts(self):
        P, c = self.P, self.cst
        self.identf = c.f32(128, "idf")
        self.identb = c.bf16(128, "idb")
        self.onesf = c.f32(128, "onf")
        self.onesb = c.bf16(128, "onb")
        self.iotaf = c.f32(128, "iof")
        tmp = self.ar.f32(128)
        tmpi = Buf(tmp.ap.bitcast(I32), tmp.name)
        P.op("pool", lambda e: e.iota(tmpi.ap, [[1, 128]], base=0, channel_multiplier=-1), (), [tmp.name])
        self.ts(self.identf.ap, tmpi.ap, 0.0, ALU.is_equal, [tmp.name], [self.identf.name])
        self.cp(self.identb.ap, self.identf.ap, [self.identf.name], [self.identb.name])
        self.memset(self.onesf.ap, 1.0, [self.onesf.name])
        self.memset(self.onesb.ap, 1.0, [self.onesb.name])
        tmp2 = self.ar.f32(128)
        tmp2i = Buf(tmp2.ap.bitcast(I32), tmp2.name)
        P.op("pool", lambda e: e.iota(tmp2i.ap, [[1, 128]], base=0, channel_multiplier=0), (), [tmp2.name])
        self.cp(self.iotaf.ap, tmp2i.ap, [tmp2.name], [self.iotaf.name])

    def load_vecT(self, dst, dname, src_ap, n):
        raw = self.ar.f32(128)
        pb = self.pb[7]
        self.P.dma(raw.v(0, [(1, 128)], 0, n), src_ap.rearrange("(k p) -> k p", p=128), (), [raw.name])
        self.tr(pb.v(0, [(1, n)]), raw.v(0, [(1, 128)], 0, n), self.identf.v(0, [(1, n)], 0, n), [raw.name, self.identf.name], [pb.name])
        self.cp(dst, pb.v(0, [(1, n)]), [pb.name], [dname])

    def st_loadx(self):
        T, P = self.T, self.P
        x_p = self.din("x_p", [T, D])
        x_s = self.din("x_s", [1, D])
        self.xT = self.scratch("xT", [KT, 128, self.TT])
        xtv = self.xT.rearrange("k p t -> p k t")
        for ti in range(T // 128):
            raw = self.ar.f32(D)
            xt = self.ar.f32(D)
            P.dma(raw.ap, x_p[ti * 128:(ti + 1) * 128, :], (), [raw.name])
            for q in range(4):
                pb = self.pb[q]
                for j in range(4):
                    k = q * 4 + j
                    self.tr(pb.v(j * 128, [(1, 128)]), raw.v(k * 128, [(1, 128)]), self.identf.ap, [raw.name, self.identf.name], [pb.name])
                self.cp(xt.v(q * 512, [(1, 512)]), pb.ap, [pb.name], [xt.name], eng="act" if q % 2 else "dve")
            P.dma(xtv[:, :, ti * 128:(ti + 1) * 128], xt.v(0, [(128, KT), (1, 128)]), [xt.name], ["xT"], q="pool", join=True)
            if ti % 2 == 1:
                self.ar.reset()
        self.stage_end()
        col = self.ar.f32(KT)
        self.load_vecT(col.ap, col.name, x_s[0, :], KT)
        P.dma(xtv[:, :, T:T + 1], col.v(0, [(1, KT), (1, 1)]), [col.name], ["xT"], q="pool", join=True)
        self.stage_end()

    def st_mod_setup(self):
        c_p = self.din("c_p", [D])
        c_s = self.din("c_s", [D])
        self.mod_w = self.din("mod_w", [4, D, 6 * D])
        self.mod_b = self.din("mod_b", [4, 6 * D])
        self.norm1_g = self.din("norm1_g", [4, D])
        self.norm2_g = self.din("norm2_g", [4, D])
        self.sc = self.cst.f32(KT * 2, "sc")
        self.modT = self.cst.f32(96 * 2, "modT")
        self.A1 = self.cst.f32(KT * 2, "A1")
        self.A2 = self.cst.f32(KT * 2, "A2")
        craw = self.ar.f32(KT * 2)
        self.load_vecT(craw.v(0, [(2, KT)]), craw.name, c_p, KT)
        self.load_vecT(craw.v(1, [(2, KT)]), craw.name, c_s, KT)
        self.act(self.sc.ap, craw.ap, AF.Silu, [craw.name], [self.sc.name])
        self.stage_end()

    def st_mod(self, i):
        P = self.P
        pb = self.pb[0]
        wv = self.mod_w[i].rearrange("(k p) f -> p k f", p=128)
        bufs = [self.ar.f32(KT * 512, "mw%d" % j) for j in range(2)]
        for fc in range(24):
            b = bufs[fc % 2]
            P.dma(b.v(0, [(512, KT), (1, 512)]), wv[:, :, fc * 512:(fc + 1) * 512], (), [b.name], q="sp" if fc % 2 == 0 else "pool")
            for ft in range(4):
                f = fc * 4 + ft
                for k in range(KT):
                    self.mm(pb.v(f * 2, [(1, 2)]), b.v(k * 512 + ft * 128, [(1, 128)]), self.sc.v(k * 2, [(1, 2)]),
                            k == 0, k == KT - 1, [b.name, self.sc.name], [pb.name])
        mb = self.ar.f32(96)
        self.load_vecT(mb.ap, mb.name, self.mod_b[i], 96)
        g1 = self.ar.f32(KT)
        g2 = self.ar.f32(KT)
        self.load_vecT(g1.ap, g1.name, self.norm1_g[i], KT)
        self.load_vecT(g2.ap, g2.name, self.norm2_g[i], KT)
        m = self.modT
        self.tt(m.v(0, [(2, 96), (1, 2)]), pb.v(0, [(2, 96), (1, 2)]), mb.v(0, [(1, 96), (0, 2)]), ALU.add,
                [pb.name, mb.name], [m.name])
        self.stt(self.A1.v(0, [(2, KT), (1, 2)]), m.v(1 * 32, [(2, KT), (1, 2)]), 1.0, g1.v(0, [(1, KT), (0, 2)]), ALU.add, ALU.mult,
                 [m.name, g1.name], [self.A1.name])
        self.stt(self.A2.v(0, [(2, KT), (1, 2)]), m.v(4 * 32, [(2, KT), (1, 2)]), 1.0, g2.v(0, [(1, KT), (0, 2)]), ALU.add, ALU.mult,
                 [m.name, g2.name], [self.A2.name])
        self.stage_end()

    def modv(self, idx, col):
        return self.modT.v(idx * 32 + col, [(2, KT)])

    def st_norm(self, A, shift_idx, hT, h32T=None, gvec=None):
        P, T = self.P, self.T
        xtv = self.xT.rearrange("k p t -> p k t")
        hv = hT.rearrange("k p t -> p k t") if hT is not None else None
        h32v = h32T.rearrange("k p t -> p k t") if h32T is not None else None
        for bi, (t0, w) in enumerate(blocks_of(T)):
            col = 1 if t0 == T else 0
            x = self.ar.f32(KT * 512, "x")
            sq = self.ar.f32(KT * 512, "sq")
            rs = self.ar.f32(512, "rs")
            hb = self.ar.bf16(KT * 512, "hb")
            pb = self.pb[bi % 2]
            xv = x.v(0, [(512, KT), (1, w)])
            sqv = sq.v(0, [(512, KT), (1, w)])
            P.dma(xv, xtv[:, :, t0:t0 + w], ["xT"], [x.name])
            self.act(sqv, xv, AF.Square, [x.name], [sq.name])
            for k in range(KT):
                self.mm(pb.v(0, [(1, w)]), self.onesf.ap, sq.v(k * 512, [(1, w)]), k == 0, k == KT - 1,
                        [sq.name, self.onesf.name], [pb.name])
            self.act(rs.v(0, [(1, w)]), pb.v(0, [(1, w)]), AF.Sqrt, [pb.name], [rs.name], bias=EPS, scale=1.0 / D)
            P.op("dve", lambda e, o=rs.v(0, [(1, w)]): e.reciprocal(o, o), [rs.name], [rs.name])
            self.tt(sqv, xv, rs.v(0, [(0, KT), (1, w)]), ALU.mult, [x.name, rs.name], [sq.name])
            if A is not None:
                self.tt(sqv, sqv, A.v(col, [(2, KT), (0, w)]), ALU.mult, [sq.name, A.name], [sq.name])
                self.tt(sqv, sqv, self.modT.v(shift_idx * 32 + col, [(2, KT), (0, w)]), ALU.add, [sq.name, self.modT.name], [sq.name])
            else:
                self.tt(sqv, sqv, gvec.v(0, [(1, KT), (0, w)]), ALU.mult, [sq.name, gvec.name], [sq.name])
            if hv is not None:
                self.cp(hb.v(0, [(512, KT), (1, w)]), sqv, [sq.name], [hb.name], eng="act")
                P.dma(hv[:, :, t0:t0 + w], hb.v(0, [(512, KT), (1, w)]), [hb.name], [hT.tensor.name], q="pool", join=True)
            if h32v is not None:
                P.dma(h32v[:, :, t0:t0 + w], sqv, [sq.name], [h32T.tensor.name], q="pool", join=True)
            if bi % 2 == 1:
                self.ar.reset()
        self.stage_end()

    def fm_to_rows(self, srcT, t0, n, dst_rows, r0=0):
        P = self.P
        src = self.ar.f32(KT * 128, "f2r")
        rows = self.ar.f32(D, "rows")
        P.dma(src.v(0, [(128, KT), (1, n)]), srcT.rearrange("k p t -> p k t")[:, :, t0:t0 + n], [srcT.tensor.name], [src.name])
        for q in range(4):
            pb = self.pb[4 + q]
            for j in range(4):
                k = q * 4 + j
                self.tr(pb.v(j * 128, [(1, 128)], 0, n), src.v(k * 128, [(1, n)]), self.identf.ap, [src.name, self.identf.name], [pb.name])
            self.cp(rows.v(q * 512, [(1, 512)], 0, n), pb.v(0, [(1, 512)], 0, n), [pb.name], [rows.name], eng="act" if q % 2 else "dve")
        P.dma(dst_rows, rows.v(0, [(1, D)], r0, n - r0), [rows.name], [dst_rows.tensor.name], q="pool", join=True)

    def st_final(self):
        T = self.T
        fg = self.din("final_g", [D])
        y_p = self.dout("y_p", [T, D])
        y_s = self.dout("y_s", [1, D])
        g = self.ar.f32(KT)
        self.load_vecT(g.ap, g.name, fg, KT)
        gk = self.cst.f32(KT, "fg")
        self.cp(gk.ap, g.ap, [g.name], [gk.name])
        self.stage_end()
        yT = self.scratch("yT", [KT, 128, self.TT])
        self.st_norm(None, 0, None, yT, gvec=gk)
        for ti in range(T // 128):
            self.fm_to_rows(yT, ti * 128, 128, y_p[ti * 128:(ti + 1) * 128, :])
            if ti % 2 == 1:
                self.ar.reset()
        self.stage_end()
        self.fm_to_rows(yT, T, 1, y_s[0:1, :])
        self.stage_end()

    def dump(self, name, view, shape, r):
        o = self.dout(name, shape)
        self.P.dma(o, view, r, [name], q="sp")

    def st_pool(self, j, h32T):
        P, T, TT = self.P, self.T, self.TT
        if j == 0:
            self.state_pool = self.din("state_pool", [2, 15, D])
            self.pool_w = self.din("pool_w", [2, 4, 512, 512])
            self.pool_scale = self.din("pool_scale", [2, D])
            self.pool_p = self.dout("pool_p", [2, 15, D])
            self.pool_s = self.dout("pool_s", [2, 15, D])
        self.fm_to_rows(h32T, T - 128, 128, self.pool_p[j], r0=113)
        P.dma(self.pool_s[j, 0:14, :], self.state_pool[j, 1:15, :], (), ["pool_s"], q="pool", join=True)
        self.stage_end()
        self.fm_to_rows(h32T, T, 1, self.pool_s[j, 14:15, :])
        self.stage_end()
        ps_ = self.ar.f32(KT)
        self.load_vecT(ps_.ap, ps_.name, self.pool_scale[j], KT)
        sg = self.cst_tmp("sg", KT * 2)
        self.tt(sg.v(0, [(2, KT), (1, 2)]), self.modT.v(2 * 32, [(2, KT), (1, 2)]), ps_.v(0, [(1, KT), (0, 2)]), ALU.mult,
                [self.modT.name, ps_.name], [sg.name])
        ext = self.cst_tmp("ext", KT * 16)
        hraw = self.ar.f32(D)
        P.dma(hraw.v(0, [(1, D)], 0, 15), self.state_pool[j], (), [hraw.name])
        for k in range(KT):
            pb = self.pb[4 + k % 4]
            self.tr(pb.v(0, [(1, 15)]), hraw.v(k * 128, [(1, 128)], 0, 15), self.identf.v(0, [(1, 15)], 0, 15),
                    [hraw.name, self.identf.name], [pb.name])
            self.cp(ext.v(k * 16, [(1, 15)]), pb.v(0, [(1, 15)]), [pb.name], [ext.name])
        P.dma(ext.v(15, [(16, KT), (1, 1)]), h32T.rearrange("k p t -> p k t")[:, :, T:T + 1], [h32T.tensor.name], [ext.name])
        self.stage_end()
        for g in range(4):
            w = 2 << g
            A = self.ar.f32(4 * T)
            B = self.ar.f32(4 * T)
            C = self.ar.f32(4 * T)
            dT = self.ar.bf16(4 * TT)
            X = self.ar.f32(4 * TT)
            Wf = self.ar.f32(4 * 512)
            Wb = self.ar.bf16(4 * 512)
            red = self.ar.f32(4)
            v3 = lambda b, o=0, n=T, s=T: b.v(o, [(s, 4), (1, n)])
            P.dma(v3(A), h32T[4 * g:4 * g + 4].rearrange("k p t -> p k t")[:, :, 0:T], [h32T.tensor.name], [A.name])
            P.dma(v3(X, 0, TT, TT), self.xT[4 * g:4 * g + 4].rearrange("k p t -> p k t"), ["xT"], [X.name], q="pool")
            P.dma(Wf.v(0, [(512, 4), (1, 512)]), self.pool_w[j, g].rearrange("(c p) e -> p c e", p=128), (), [Wf.name])
            self.cp(Wb.ap, Wf.ap, [Wf.name], [Wb.name], eng="act")
            cur = A
            for si in range(g + 1):
                st = 1 << si
                nxt = B if si % 2 == 0 else C
                self.cp(v3(nxt, 0, st), v3(cur, 0, st), [cur.name], [nxt.name], eng="act")
                self.tt(v3(nxt, st, T - st), v3(cur, st, T - st), v3(cur, 0, T - st), ALU.add, [cur.name], [nxt.name])
                cur = nxt
            self.stt(v3(dT, 0, T, TT), v3(cur), 1.0 / w, v3(A), ALU.mult, ALU.subtract, [cur.name, A.name], [dT.name])
            for t in range(w - 1):
                self.stt(v3(dT, t, 1, TT), v3(cur, t, 1), 1.0 / (t + 1), v3(A, t, 1), ALU.mult, ALU.subtract, [cur.name, A.name], [dT.name])
            P.op("dve", lambda e, o=red.ap, i=ext.v(4 * g * 16 + 16 - w, [(16, 4), (1, w)]): e.tensor_reduce(o, i, AX.X, ALU.add),
                 [ext.name], [red.name])
            self.stt(v3(dT, T, 1, TT), red.v(0, [(1, 4), (1, 1)]), 1.0 / w, ext.v(4 * g * 16 + 15, [(16, 4), (1, 1)]), ALU.mult, ALU.subtract,
                     [red.name, ext.name], [dT.name])
            nb = 0
            for et in range(4):
                k = 4 * g + et
                for (t0, wd) in blocks_of(T):
                    pb = self.pb[nb % 4]
                    nb += 1
                    col = 1 if t0 == T else 0
                    for c in range(4):
                        self.mm(pb.v(0, [(1, wd)]), Wb.v(c * 512 + et * 128, [(1, 128)]), dT.v(c * TT + t0, [(1, wd)]), c == 0, c == 3,
                                [Wb.name, dT.name], [pb.name])
                    xv = X.v(et * TT + t0, [(1, wd)])
                    self.stt(xv, pb.v(0, [(1, wd)]), sg.v(k * 2 + col, [(1, 1)]), xv, ALU.mult, ALU.add, [pb.name, sg.name, X.name], [X.name])
            P.dma(self.xT[4 * g:4 * g + 4].rearrange("k p t -> p k t"), v3(X, 0, TT, TT), [X.name], ["xT"], q="pool", join=True)
            self.ar.reset()
        self.stage_end()

    def cst_tmp(self, name, n):
        if not hasattr(self, "_ct"):
            self._ct = {}
        if name not in self._ct:
            self._ct[name] = self.cst.f32(n)
        return self._ct[name]

    def st_peer_route(self, i, hT):
        P, T, TT = self.P, self.T, self.TT
        if not hasattr(self, "peer_wq"):
            self.peer_wq = self.din("peer_w_q", [4, D, 2048])
            self.peer_keys = self.din("peer_keys", [4, 2, 128, 128])
            self.peer_u = self.din("peer_u", [4, 16384, D])
            self.peer_v = self.din("peer_v", [4, 16384, D])
            self.Gsamp = self.cst.bf16(128)
        if getattr(self, "_route_T", None) != T:
            self._route_T = T
            self.scd = self.scratch("scd%d" % T, [T // 128 + 1, 128, 16, 128])
            self.Gd = self.scratch("Gd%d" % T, [128, 128, T], BF16)
        ntile = T // 128 + 1
        hs = self.ar.bf16(KT * TT)
        P.dma(hs.v(0, [(TT, KT), (1, TT)]), hT.rearrange("k p t -> p k t"), [hT.tensor.name], [hs.name])
        keysT = self.ar.f32(256)
        kraw = self.ar.f32(256)
        P.dma(kraw.v(0, [(128, 2), (1, 128)]), self.peer_keys[i].rearrange("a m k -> m a k"), (), [kraw.name])
        for p in range(2):
            pb = self.pb[6 + p]
            self.tr(pb.v(0, [(1, 128)]), kraw.v(p * 128, [(1, 128)]), self.identf.ap, [kraw.name, self.identf.name], [pb.name])
            self.cp(keysT.v(p * 128, [(1, 128)]), pb.v(0, [(1, 128)]), [pb.name], [keysT.name])
        wf = [self.ar.f32(KT * 128) for _ in range(2)]
        wb = [self.ar.bf16(KT * 128) for _ in range(2)]
        qf = [self.ar.f32(TT) for _ in range(2)]
        stg = [self.ar.f32(ntile * 128) for _ in range(2)]
        for s_ in stg:
            self.memset(s_.ap, 0.0, [s_.name], eng="pool")
        nb = 0
        for ft in range(16):
            s = ft % 2
            P.dma(wf[s].v(0, [(128, KT), (1, 128)]), self.peer_wq[i].rearrange("(k p) f -> p k f", p=128)[:, :, ft * 128:(ft + 1) * 128],
                  (), [wf[s].name])
            self.cp(wb[s].ap, wf[s].ap, [wf[s].name], [wb[s].name], eng="pool")
            for (t0, w) in blocks_of(T):
                pb = self.pb[nb % 3]
                nb += 1
                for k in range(KT):
                    self.mm(pb.v(0, [(1, w)]), wb[s].v(k * 128, [(1, 128)]), hs.v(k * TT + t0, [(1, w)]), k == 0, k == KT - 1,
                            [wb[s].name, hs.name], [pb.name])
                self.cp(qf[s].v(t0, [(1, w)]), pb.v(0, [(1, w)]), [pb.name], [qf[s].name], eng="act")
            for tt_ in range(ntile):
                m = 128 if tt_ < ntile - 1 else 1
                pb = self.pb[3 + tt_ % 3]
                self.mm(pb.v(0, [(1, 128)], 0, m), qf[s].v(tt_ * 128, [(1, m)]), keysT.v((ft % 2) * 128, [(1, 128)]), True, True,
                        [qf[s].name, keysT.name], [pb.name])
                self.cp(stg[s].v(tt_ * 128, [(1, 128)], 0, m), pb.v(0, [(1, 128)], 0, m), [pb.name], [stg[s].name])
            P.dma(self.scd[:, :, ft, :].rearrange("n p m -> p n m"), stg[s].v(0, [(128, ntile), (1, 128)]), [stg[s].name], [self.scd.tensor.name],
                  q="pool", join=True)
        self.stage_end()
        NEG = -1.0e30
        for tt_ in range(ntile):
            m = 128 if tt_ < ntile - 1 else 1
            sc = self.ar.f32(2048)
            scr = self.ar.f32(256)
            vals = self.ar.f32(256)
            idx = self.ar.u32(256)
            idxf = self.ar.f32(256)
            cand = self.ar.f32(2048)
            top = self.ar.f32(128)
            sel = self.ar.u32(128)
            selb = self.ar.u32(128)
            af = self.ar.f32(128)
            bf = self.ar.f32(128)
            gg = self.ar.f32(128)
            es = self.ar.f32(8)
            eq = self.ar.f32(2048)
            i1 = self.ar.f32(128)
            i2 = self.ar.f32(128)
            i1T = self.ar.f32(128)
            i2T = self.ar.f32(128)
            gT = self.ar.f32(128)
            oh1 = [self.ar.bf16(8 * 128) for _ in range(6)]
            oh2 = [self.ar.bf16(8 * 128) for _ in range(3)]
            Gs = self.ar.bf16(128 * 128)
            P.dma(sc.ap, self.scd[tt_].rearrange("p f m -> p (f m)"), [self.scd.tensor.name], [sc.name])
            V = lambda b, o, n: b.v(o, [(1, n)])
            for ft in range(16):
                s_in = V(sc, ft * 128, 128)
                P.op("dve", lambda e, o=V(vals, ft * 16, 8), i=s_in: e.max(out=o, in_=i), [sc.name], [vals.name])
                P.op("dve", lambda e, o=V(idx, ft * 16, 8), mx=V(vals, ft * 16, 8), i=s_in: e.max_index(o, mx, i), [sc.name, vals.name], [idx.name])
                P.op("dve", lambda e, o=V(scr, 0, 128), mx=V(vals, ft * 16, 8), i=s_in: e.match_replace(o, mx, i, NEG),
                     [sc.name, vals.name], [scr.name])
                P.op("dve", lambda e, o=V(vals, ft * 16 + 8, 8), i=V(scr, 0, 128): e.max(out=o, in_=i), [scr.name], [vals.name])
                P.op("dve", lambda e, o=V(idx, ft * 16 + 8, 8), mx=V(vals, ft * 16 + 8, 8), i=V(scr, 0, 128): e.max_index(o, mx, i),
                     [scr.name, vals.name], [idx.name])
            self.cp(idxf.ap, idx.ap, [idx.name], [idxf.name])
            self.tt(cand.v(0, [(256, 8), (16, 16), (1, 16)]), vals.v(0, [(32, 8), (1, 16), (0, 16)]), vals.v(16, [(32, 8), (0, 16), (1, 16)]),
                    ALU.add, [vals.name], [cand.name])
            for h in range(8):
                c_in = V(cand, h * 256, 256)
                P.op("dve", lambda e, o=V(top, h * 16, 8), i=c_in: e.max(out=o, in_=i), [cand.name], [top.name])
                P.op("dve", lambda e, o=V(sel, h * 16, 8), mx=V(top, h * 16, 8), i=c_in: e.max_index(o, mx, i), [cand.name, top.name], [sel.name])
                P.op("dve", lambda e, o=V(scr, 0, 256), mx=V(top, h * 16, 8), i=c_in: e.match_replace(o, mx, i, NEG),
                     [cand.name, top.name], [scr.name])
                P.op("dve", lambda e, o=V(top, h * 16 + 8, 8), i=V(scr, 0, 256): e.max(out=o, in_=i), [scr.name], [top.name])
                P.op("dve", lambda e, o=V(sel, h * 16 + 8, 8), mx=V(top, h * 16 + 8, 8), i=V(scr, 0, 256): e.max_index(o, mx, i),
                     [scr.name, top.name], [sel.name])
            self.tt(gg.v(0, [(16, 8), (1, 16)]), top.v(0, [(16, 8), (1, 16)]), top.v(0, [(16, 8), (0, 16)]), ALU.subtract, [top.name], [gg.name])
            self.act(gg.ap, gg.ap, AF.Exp, [gg.name], [gg.name])
            P.op("dve", lambda e, o=es.ap, i=gg.v(0, [(16, 8), (1, 16)]): e.tensor_reduce(o, i, AX.X, ALU.add), [gg.name], [es.name])
            P.op("dve", lambda e, o=es.ap: e.reciprocal(o, o), [es.name], [es.name])
            self.tt(gg.v(0, [(16, 8), (1, 16)]), gg.v(0, [(16, 8), (1, 16)]), es.v(0, [(1, 8), (0, 16)]), ALU.mult, [gg.name, es.name], [gg.name])
            self.ts(selb.ap, sel.ap, 4, ALU.logical_shift_right, [sel.name], [selb.name])
            self.cp(af.ap, selb.ap, [selb.name], [af.name])
            self.ts(selb.ap, sel.ap, 15, ALU.bitwise_and, [sel.name, af.name], [selb.name])
            self.cp(bf.ap, selb.ap, [selb.name], [bf.name])
            for (src, off, dst) in ((af, 0, i1), (bf, 16, i2)):
                self.tt(eq.v(0, [(256, 8), (16, 16), (1, 16)]), src.v(0, [(16, 8), (1, 16), (0, 16)]), self.iotaf.v(0, [(0, 8), (0, 16), (1, 16)]),
                        ALU.is_equal, [src.name, self.iotaf.name], [eq.name])
                self.tt(eq.v(0, [(256, 8), (16, 16), (1, 16)]), eq.v(0, [(256, 8), (16, 16), (1, 16)]), idxf.v(off, [(32, 8), (0, 16), (1, 16)]),
                        ALU.mult, [eq.name, idxf.name], [eq.name])
                P.op("dve", lambda e, o=dst.ap, i=eq.v(0, [(16, 128), (1, 16)]): e.tensor_reduce(o, i, AX.X, ALU.add), [eq.name], [dst.name])
            for (src, dst, bank) in ((i1, i1T, 5), (i2, i2T, 6), (gg, gT, 7)):
                pb = self.pb[bank]
                self.tr(pb.v(0, [(1, 128)]), src.ap, self.identf.ap, [src.name, self.identf.name], [pb.name])
                self.cp(dst.ap, pb.v(0, [(1, 128)]), [pb.name], [dst.name], eng="act")
            NB = 8
            for t8 in range(0, m, NB):
                nbt = min(NB, m - t8)
                sl = (t8 // NB) % 3
                E1, O1, O2 = oh1[sl], oh1[3 + sl], oh2[sl]
                v3 = lambda b_: b_.v(0, [(128, nbt), (1, 128)])
                io = self.iotaf.v(0, [(0, nbt), (1, 128)])
                self.tt(v3(E1), io, i1T.v(t8, [(1, nbt), (0, 128)]), ALU.is_equal, [self.iotaf.name, i1T.name], [E1.name])
                self.tt(v3(O1), v3(E1), gT.v(t8, [(1, nbt), (0, 128)]), ALU.mult, [E1.name, gT.name], [O1.name])
                self.tt(v3(O2), io, i2T.v(t8, [(1, nbt), (0, 128)]), ALU.is_equal, [self.iotaf.name, i2T.name], [O2.name])
                for tl in range(nbt):
                    t = t8 + tl
                    pb = self.pb[(t // 4) % 4]
                    self.mm(pb.v(t % 4, [(4, 128)]), O2.v(tl * 128, [(1, 128)]), O1.v(tl * 128, [(1, 128)]), True, True,
                            [O1.name, O2.name], [pb.name])
                    if t % 4 == 3 or t == m - 1:
                        n4 = t % 4 + 1
                        tb = t - (t % 4)
                        self.cp(Gs.v(tb, [(128, 128), (1, n4)]), pb.v(0, [(4, 128), (1, n4)]), [pb.name], [Gs.name], eng="act")
            if m == 128:
                for cq in range(4):
                    P.dma(self.Gd[cq * 32:(cq + 1) * 32, :, tt_ * 128:(tt_ + 1) * 128].rearrange("c p t -> p c t"),
                          Gs.v(cq * 32 * 128, [(128, 32), (1, 128)]), [Gs.name], [self.Gd.tensor.name], q="pool" if cq % 2 else "sp", join=True)
            else:
                self.cp(self.Gsamp.ap, Gs.v(0, [(128, 128)]), [Gs.name], [self.Gsamp.name])
            self.ar.reset()
        self.stage_end()

    def st_peer_main(self, i, hT):
        P, T, TT = self.P, self.T, self.TT
        NCH = self.nchunks if hasattr(self, "nchunks") else 128
        groups = [(t0, min(1024, T - t0)) for t0 in range(0, T, 1024)]
        hv = hT.rearrange("k p t -> p k t")
        for gi, (t0, n) in enumerate(groups):
            last = gi == len(groups) - 1
            ne = n + 1 if last else n
            blks = [(b0, min(512, n - b0)) for b0 in range(0, n, 512)]
            acc = self.ar.f32(KT * ne)
            hg = self.ar.bf16(KT * ne)
            ubf = [self.ar.bf16(D) for _ in range(3)]
            uT = [self.ar.bf16(D) for _ in range(2)]
            vbf = [self.ar.bf16(D) for _ in range(8)]
            WT = [self.ar.bf16(ne) for _ in range(4)]
            actb = [self.ar.bf16(ne) for _ in range(2)]
            Gc = [self.ar.bf16(n) for _ in range(2)]
            xbs = [self.ar.f32(n) for _ in range(2)]
            P.dma(hg.v(0, [(ne, KT), (1, n)]), hv[:, :, t0:t0 + n], [hT.tensor.name], [hg.name])
            if last:
                P.dma(hg.v(n, [(ne, KT), (1, 1)]), hv[:, :, T:T + 1], [hT.tensor.name], [hg.name], join=True)
            pS = [self.pb[0], self.pb[1]]
            pSs = self.pb[2]
            pT = [self.pb[3], self.pb[4]]
            pV = [self.pb[5], self.pb[6]]
            pVs = self.pb[7]
            nvc = [0]

            def loadUV(c):
                P.dma(ubf[c % 3].ap, self.peer_u[i, c * 128:(c + 1) * 128, :], (), [ubf[c % 3].name], q="pool")
                P.dma(vbf[c % 8].ap, self.peer_v[i, c * 128:(c + 1) * 128, :], (), [vbf[c % 8].name], q="pool")

            def loadG(c):
                P.dma(Gc[c % 2].ap, self.Gd[c, :, t0:t0 + n], [self.Gd.tensor.name], [Gc[c % 2].name], q="sp")

            def tr(c):
                ub = ubf[c % 3]
                for hb in range(2):
                    pt = pT[hb]
                    ptb = Buf(pt.ap.bitcast(BF16), pt.name)
                    for kk in range(8):
                        k = hb * 8 + kk
                        self.tr(ptb.v(kk * 128, [(1, 128)]), ub.v(k * 128, [(1, 128)]), self.identb.ap, [ub.name, self.identb.name], [pt.name])
                    self.cp(uT[c % 2].v(hb * 1024, [(1, 1024)]), ptb.v(0, [(1, 1024)]), [pt.name], [uT[c % 2].name], eng="act")

            def compute(c):
                ci, s2 = c % 4, c % 2
                for bi, (b0, wd) in enumerate(blks):
                    for k in range(KT):
                        self.mm(pS[bi].v(0, [(1, wd)]), uT[s2].v(k * 128, [(1, 128)]), hg.v(k * ne + b0, [(1, wd)]), k == 0, k == KT - 1,
                                [uT[s2].name, hg.name], [pS[bi].name])
                    self.act(actb[s2].v(b0, [(1, wd)]), pS[bi].v(0, [(1, wd)]), AF.Gelu, [pS[bi].name], [actb[s2].name])
                if last:
                    for k in range(KT):
                        self.mm(pSs.v(0, [(1, 1)]), uT[s2].v(k * 128, [(1, 128)]), hg.v(k * ne + n, [(1, 1)]), k == 0, k == KT - 1,
                                [uT[s2].name, hg.name], [pSs.name])
                    self.act(actb[s2].v(n, [(1, 1)]), pSs.v(0, [(1, 1)]), AF.Gelu, [pSs.name], [actb[s2].name])
                    self.tt(WT[ci].v(n, [(1, 1)]), actb[s2].v(n, [(1, 1)]), self.Gsamp.v(c, [(1, 1)]), ALU.mult,
                            [actb[s2].name, self.Gsamp.name], [WT[ci].name])
                self.tt(WT[ci].v(0, [(1, n)]), actb[s2].v(0, [(1, n)]), Gc[s2].ap, ALU.mult, [actb[s2].name, Gc[s2].name], [WT[ci].name])

            def vphase(c):
                first = c == 3
                vs = [vbf[(c - 3 + cj) % 8] for cj in range(4)]
                for dt in range(KT):
                    for (b0, wd) in blks:
                        pv = pV[nvc[0] % 2]
                        nvc[0] += 1
                        for cj in range(4):
                            self.mm(pv.v(0, [(1, wd)]), vs[cj].v(dt * 128, [(1, 128)]), WT[cj].v(b0, [(1, wd)]), cj == 0, cj == 3,
                                    [vs[cj].name, WT[cj].name], [pv.name])
                        av = acc.v(dt * ne + b0, [(1, wd)])
                        if first:
                            self.cp(av, pv.v(0, [(1, wd)]), [pv.name], [acc.name])
                        else:
                            self.tt(av, pv.v(0, [(1, wd)]), av, ALU.add, [pv.name, acc.name], [acc.name])
                if last:
                    for dt in range(KT):
                        for cj in range(4):
                            self.mm(pVs.v(dt, [(1, 1)]), vs[cj].v(dt * 128, [(1, 128)]), WT[cj].v(n, [(1, 1)]), cj == 0, cj == 3,
                                    [vs[cj].name, WT[cj].name], [pVs.name])
                    av = acc.v(n, [(ne, KT)])
                    if first:
                        self.cp(av, pVs.v(0, [(1, KT)]), [pVs.name], [acc.name])
                    else:
                        self.tt(av, pVs.v(0, [(1, KT)]), av, ALU.add, [pVs.name, acc.name], [acc.name])

            for c in range(min(3, NCH)):
                loadUV(c)
            for c in range(min(2, NCH)):
                loadG(c)
            tr(0)
            for c in range(NCH):
                if c + 3 < NCH:
                    loadUV(c + 3)
                if c + 1 < NCH:
                    tr(c + 1)
                compute(c)
                if c + 2 < NCH:
                    loadG(c + 2)
                if c % 4 == 3:
                    vphase(c)
            xtv = self.xT.rearrange("k p t -> p k t")
            for k in range(KT):
                xb = xbs[k % 2]
                P.dma(xb.v(0, [(1, n)]), xtv[:, k, t0:t0 + n], ["xT"], [xb.name])
                self.stt(xb.v(0, [(1, n)]), acc.v(k * ne, [(1, n)]), self.modT.v(5 * 32 + 2 * k, [(1, 1)]), xb.v(0, [(1, n)]), ALU.mult, ALU.add,
                         [acc.name, self.modT.name, xb.name], [xb.name])
                P.dma(xtv[:, k, t0:t0 + n], xb.v(0, [(1, n)]), [xb.name], ["xT"], q="pool", join=True)
            if last:
                xs = self.cst_tmp("xs_s", KT)
                tmpg = self.cst_tmp("xs_g", KT)
                P.dma(xs.v(0, [(1, KT), (1, 1)]), xtv[:, :, T:T + 1], ["xT"], [xs.name])
                self.tt(tmpg.v(0, [(1, KT)]), acc.v(n, [(ne, KT)]), self.modT.v(5 * 32 + 1, [(2, KT)]), ALU.mult, [acc.name, self.modT.name], [tmpg.name])
                self.tt(xs.v(0, [(1, KT)]), xs.v(0, [(1, KT)]), tmpg.v(0, [(1, KT)]), ALU.add, [tmpg.name, xs.name], [xs.name])
                P.dma(xtv[:, :, T:T + 1], xs.v(0, [(1, KT), (1, 1)]), [xs.name], ["xT"], q="pool", join=True)
            self.stage_end()


    def st_dil(self, hT):
        P, T, TT = self.P, self.T, self.TT
        DILS = (1, 4, 16)
        WIN = (128, 512, 2048)
        w_in = self.din("dil_w_in", [1, D, 9216])[0]
        w_out = self.din("dil_w_out", [1, 1024, D])[0]
        caches = [self.din("cache_dil_kv%d" % g, [1, WIN[g], 2, 8, 128])[0] for g in range(3)]
        kvp = [self.dout("kv%d_p" % g, [1, min(WIN[g], T), 2, 8, 128])[0] for g in range(3)]
        kvs = [self.dout("kv%d_s" % g, [1, WIN[g], 2, 8, 128])[0] for g in range(3)]
        winv = w_in.rearrange("(k p) f -> p k f", p=128)
        scale = 128 ** -0.5
        NEG = -1.0e30
        for g in range(3):
            Wg = WIN[g]
            nsp = 4 if Wg > 512 else 1
            for q_ in range(nsp):
                a = 1 + (Wg - 1) * q_ // nsp
                b = 1 + (Wg - 1) * (q_ + 1) // nsp
                P.dma(kvs[g][a - 1:b - 1].rearrange("w a h d -> w (a h d)"), caches[g][a:b].rearrange("w a h d -> w (a h d)"), (), ["kvs%d" % g],
                      q="pool", join=True)
        D2 = self.ar.f32(256)
        M2 = self.ar.f32(256)
        B2 = self.ar.f32(256)
        bS = self.ar.f32(1)
        bSh = self.ar.f32(1)
        ti = self.ar.f32(256)
        tii = Buf(ti.ap.bitcast(I32), ti.name)
        P.op("pool", lambda e: e.iota(tii.v(0, [(1, 128)]), [[1, 128]], base=128, channel_multiplier=-1), (), [ti.name])
        P.op("pool", lambda e: e.iota(tii.v(128, [(1, 128)]), [[1, 128]], base=0, channel_multiplier=-1), (), [ti.name])
        self.cp(D2.ap, tii.ap, [ti.name], [D2.name])
        self.ts(M2.v(0, [(1, 128)]), D2.v(0, [(1, 128)]), 128.0, ALU.is_gt, [D2.name], [M2.name], s2=NEG, op1=ALU.mult)
        self.ts(M2.v(128, [(1, 128)]), D2.v(128, [(1, 128)]), 0.0, ALU.is_lt, [D2.name], [M2.name], s2=NEG, op1=ALU.mult)
        P.op("pool", lambda e: e.iota(tii.v(0, [(1, 1)]), [[1, 1]], base=-128, channel_multiplier=1), (), [ti.name])
        self.cp(bS.ap, tii.v(0, [(1, 1)]), [ti.name], [bS.name])
        hs = self.ar.bf16(KT * TT)
        P.dma(hs.v(0, [(TT, KT), (1, TT)]), hT.rearrange("k p t -> p k t"), [hT.tensor.name], [hs.name])
        oT = self.ar.bf16(8 * TT)
        accO = self.ar.f32(T)
        accD = self.ar.f32(T)
        qT = self.ar.bf16(T)
        kT = self.ar.bf16(T)
        Vh = self.ar.bf16(16 * 128)
        wst = [self.ar.f32(D) for _ in range(2)]
        wbf = [self.ar.bf16(D) for _ in range(3)]
        PT = [self.ar.bf16(256) for _ in range(2)]
        tmpS = [self.ar.f32(256) for _ in range(2)]
        rows = [self.ar.f32(128) for _ in range(3)]
        vcol = self.ar.f32(1)
        Kc = self.ar.f32(128)
        Vc = self.ar.f32(128)
        prod = self.ar.f32(128)
        sS = self.ar.f32(2)
        pS_ = self.ar.f32(2)
        oS = self.ar.f32(2)
        tS = self.ar.f32(2)
        nld = 0
        for h in range(8):
            for g in range(3):
                dil = DILS[g]
                L = T // dil
                bpr = L // 128
                slope = 2.0 ** (-8.0 * (g * 8 + h + 1) / 24.0)
                for s_ in range(3):
                    col = s_ * 3072 + g * 1024 + h * 128
                    st = wst[nld % 2]
                    P.dma(st.v(0, [(128, KT), (1, 128)]), winv[:, :, col:col + 128], (), [st.name], q="sp" if nld % 2 else "pool")
                    self.cp(wbf[s_].ap, st.ap, [st.name], [wbf[s_].name], eng="pool")
                    nld += 1
                for (dst, wi, sc_) in ((qT, 0, scale), (kT, 1, 1.0)):
                    for bi, (t0, w) in enumerate(blocks_of(T)[:-1]):
                        pb = self.pb[bi % 2]
                        for k in range(KT):
                            self.mm(pb.v(0, [(1, w)]), wbf[wi].v(k * 128, [(1, 128)]), hs.v(k * TT + t0, [(1, w)]), k == 0, k == KT - 1,
                                    [wbf[wi].name, hs.name], [pb.name])
                        self.act(dst.v(t0 // dil, [(L, dil), (1, w // dil)]), pb.v(0, [(1, dil), (dil, w // dil)]), AF.Copy, [pb.name], [dst.name],
                                 scale=sc_)
                for jb in range(16):
                    r_ = (jb * 128) // L
                    i0 = (jb * 128) % L
                    pb = self.pb[7]
                    for k in range(KT):
                        self.mm(pb.v(0, [(1, 128)]), hs.v(k * TT + i0 * dil + r_, [(dil, 128)]), wbf[2].v(k * 128, [(1, 128)]), k == 0, k == KT - 1,
                                [wbf[2].name, hs.name], [pb.name])
                    self.cp(Vh.v(jb * 128, [(1, 128)]), pb.v(0, [(1, 128)]), [pb.name], [Vh.name])
                self.stt(B2.ap, D2.ap, -slope * dil, M2.ap, ALU.mult, ALU.add, [D2.name, M2.name], [B2.name])
                for jb in range(16):
                    hasprev = (jb % bpr) != 0
                    ks = [jb - 1, jb] if hasprev else [jb]
                    o0 = 0 if hasprev else 128
                    nk = 128 * len(ks)
                    pSb = self.pb[2]
                    s2 = jb % 2
                    for x, kb in enumerate(ks):
                        self.mm(pSb.v(o0 + x * 128, [(1, 128)]), kT.v(kb * 128, [(1, 128)]), qT.v(jb * 128, [(1, 128)]), True, True,
                                [kT.name, qT.name], [pSb.name])
                    self.tt(tmpS[s2].v(o0, [(1, nk)]), pSb.v(o0, [(1, nk)]), B2.v(o0, [(1, nk)]), ALU.add, [pSb.name, B2.name], [tmpS[s2].name])
                    self.act(PT[s2].v(o0, [(1, nk)]), tmpS[s2].v(o0, [(1, nk)]), AF.Exp, [tmpS[s2].name], [PT[s2].name])
                    pO, pD = self.pb[3 + 2 * (jb % 2)], self.pb[4 + 2 * (jb % 2)]
                    for x, kb in enumerate(ks):
                        self.mm(pO.v(0, [(1, 128)]), Vh.v(kb * 128, [(1, 128)]), PT[s2].v(o0 + x * 128, [(1, 128)]), x == 0, x == len(ks) - 1,
                                [Vh.name, PT[s2].name], [pO.name])
                    for x, kb in enumerate(ks):
                        self.mm(pD.v(0, [(1, 128)]), self.onesb.ap, PT[s2].v(o0 + x * 128, [(1, 128)]), x == 0, x == len(ks) - 1,
                                [self.onesb.name, PT[s2].name], [pD.name])
                    r_ = (jb * 128) // L
                    i0 = (jb * 128) % L
                    for (acc_, pb_) in ((accO, pO), (accD, pD)):
                        av = acc_.v(i0 * dil + r_, [(dil, 128)])
                        if g == 0:
                            self.cp(av, pb_.v(0, [(1, 128)]), [pb_.name], [acc_.name], eng="act" if acc_ is accD else "dve")
                        else:
                            self.tt(av, pb_.v(0, [(1, 128)]), av, ALU.add, [pb_.name, acc_.name], [acc_.name])
                pr = self.pb[7]
                for s_ in range(3):
                    for k in range(KT):
                        self.mm(pr.v(s_ * 128, [(1, 128)]), hs.v(k * TT + T, [(0, 128)]), wbf[s_].v(k * 128, [(1, 128)]), k == 0, k == KT - 1,
                                [wbf[s_].name, hs.name], [pr.name])
                    self.cp(rows[s_].ap, pr.v(s_ * 128, [(1, 128)]), [pr.name], [rows[s_].name])
                for k in range(KT):
                    self.mm(pr.v(384, [(1, 1)]), wbf[2].v(k * 128, [(1, 128)]), hs.v(k * TT + T, [(1, 1)]), k == 0, k == KT - 1,
                            [wbf[2].name, hs.name], [pr.name])
                self.cp(vcol.ap, pr.v(384, [(1, 1)]), [pr.name], [vcol.name])
                Wg = WIN[g]
                P.dma(kvs[g][Wg - 1:Wg, 0, h, :], rows[1].v(0, [(1, 128)], 0, 1), [rows[1].name], ["kvs%d" % g], q="pool", join=True)
                P.dma(kvs[g][Wg - 1:Wg, 1, h, :], rows[2].v(0, [(1, 128)], 0, 1), [rows[2].name], ["kvs%d" % g], q="pool", join=True)
                cv = caches[g].rearrange("(i s) a h d -> i s a h d", s=dil)
                P.dma(Kc.ap, cv[:, 0, 0, h, :], (), [Kc.name])
                P.dma(Vc.ap, cv[:, 0, 1, h, :], (), [Vc.name])
                self.tt(prod.ap, Kc.ap, rows[0].ap, ALU.mult, [Kc.name, rows[0].name], [prod.name])
                P.op("dve", lambda e, o=sS.v(0, [(1, 1)]), i=prod.ap: e.tensor_reduce(o, i, AX.X, ALU.add), [prod.name], [sS.name])
                self.tt(prod.ap, rows[1].ap, rows[0].ap, ALU.mult, [rows[1].name, rows[0].name, sS.name], [prod.name])
                P.op("dve", lambda e, o=sS.v(1, [(1, 1)]), i=prod.ap: e.tensor_reduce(o, i, AX.X, ALU.add), [prod.name], [sS.name])
                self.ts(bSh.ap, bS.ap, slope * dil, ALU.mult, [bS.name], [bSh.name])
                self.act(pS_.v(0, [(1, 1)]), sS.v(0, [(1, 1)]), AF.Exp, [sS.name, bSh.name], [pS_.name], bias=bSh.ap, scale=scale)
                self.act(pS_.v(1, [(1, 1)]), sS.v(1, [(1, 1)]), AF.Exp, [sS.name], [pS_.name], scale=scale)
                po = self.pb[2]
                self.mm(po.v(0, [(1, 1)]), Vc.ap, pS_.v(0, [(1, 1)]), True, True, [Vc.name, pS_.name], [po.name])
                self.mm(po.v(1, [(1, 1)]), self.onesf.ap, pS_.v(0, [(1, 1)]), True, True, [self.onesf.name, pS_.name], [po.name])
                self.stt(tS.v(0, [(1, 1)]), vcol.ap, pS_.v(1, [(1, 1)]), po.v(0, [(1, 1)]), ALU.mult, ALU.add, [vcol.name, pS_.name, po.name], [tS.name])
                self.tt(tS.v(1, [(1, 1)]), po.v(1, [(1, 1)]), pS_.v(1, [(1, 1)]), ALU.add, [po.name, pS_.name, tS.name], [tS.name])
                if g == 0:
                    self.cp(oS.ap, tS.ap, [tS.name], [oS.name])
                else:
                    self.tt(oS.ap, oS.ap, tS.ap, ALU.add, [tS.name, oS.name], [oS.name])
            P.op("dve", lambda e, o=accD.ap: e.reciprocal(o, o), [accD.name], [accD.name])
            self.tt(oT.v(h * TT, [(1, T)]), accO.ap, accD.ap, ALU.mult, [accO.name, accD.name], [oT.name])
            P.op("dve", lambda e, o=oS.v(1, [(1, 1)]): e.reciprocal(o, o), [oS.name], [oS.name])
            self.tt(oT.v(h * TT + T, [(1, 1)]), oS.v(0, [(1, 1)]), oS.v(1, [(1, 1)]), ALU.mult, [oS.name], [oT.name])
        xtv = self.xT.rearrange("k p t -> p k t")
        wov = w_out.rearrange("(e p) f -> p e f", p=128)
        for ft in range(KT):
            st = wst[ft % 2]
            wb = wbf[ft % 2]
            xb = [accO, accD][ft % 2]
            xs_ = tmpS[ft % 2]
            P.dma(st.v(0, [(128, 8), (1, 128)]), wov[:, :, ft * 128:(ft + 1) * 128], (), [st.name])
            self.cp(wb.v(0, [(1, 1024)]), st.v(0, [(1, 1024)]), [st.name], [wb.name], eng="pool")
            P.dma(xb.ap, xtv[:, ft, 0:T], ["xT"], [xb.name], q="pool")
            P.dma(xs_.v(0, [(1, 1)]), xtv[:, ft, T:T + 1], ["xT"], [xs_.name], q="pool")
            for bi, (t0, w) in enumerate(blocks_of(T)):
                pb = self.pb[bi % 2]
                col = 1 if t0 == T else 0
                for e_ in range(8):
                    self.mm(pb.v(0, [(1, w)]), wb.v(e_ * 128, [(1, 128)]), oT.v(e_ * TT + t0, [(1, w)]), e_ == 0, e_ == 7, [wb.name, oT.name], [pb.name])
                if col == 0:
                    xv = xb.v(t0, [(1, w)])
                    self.stt(xv, pb.v(0, [(1, w)]), self.modT.v(2 * 32 + 2 * ft, [(1, 1)]), xv, ALU.mult, ALU.add, [pb.name, self.modT.name, xb.name], [xb.name])
                else:
                    xv = xs_.v(0, [(1, 1)])
                    self.stt(xv, pb.v(0, [(1, 1)]), self.modT.v(2 * 32 + 2 * ft + 1, [(1, 1)]), xv, ALU.mult, ALU.add, [pb.name, self.modT.name, xs_.name], [xs_.name])
            P.dma(xtv[:, ft, 0:T], xb.ap, [xb.name], ["xT"], q="pool", join=True)
            P.dma(xtv[:, ft, T:T + 1], xs_.v(0, [(1, 1)]), [xs_.name], ["xT"], q="pool", join=True)
        self.stage_end()
        hs = self.ar.bf16(KT * TT)
        P.dma(hs.v(0, [(TT, KT), (1, TT)]), hT.rearrange("k p t -> p k t"), [hT.tensor.name], [hs.name])
        wst = [self.ar.f32(D) for _ in range(2)]
        self.kvw = self.ar.bf16(KT * 512)
        self.kvo = [self.ar.f32(512) for _ in range(2)]
        nrot = 0
        for g in range(3):
            nl = min(WIN[g], T)
            for s_ in (1, 2):
                for half in range(2):
                    col = s_ * 3072 + g * 1024 + half * 512
                    for piece in range(4):
                        stp = wst[nrot % 2]
                        nrot += 1
                        P.dma(stp.v(0, [(512, 4), (1, 512)]), winv[:, piece * 4:(piece + 1) * 4, col:col + 512], (), [stp.name])
                        self.cp(self.kvw.v(piece * 2048, [(1, 2048)]), stp.ap, [stp.name], [self.kvw.name], eng="pool")
                    for ti_ in range(nl // 128):
                        tok0 = T - nl + ti_ * 128
                        pb = self.pb[ti_ % 2]
                        for k in range(KT):
                            self.mm(pb.ap, hs.v(k * TT + tok0, [(1, 128)]), self.kvw.v(k * 512, [(1, 512)]), k == 0, k == KT - 1,
                                    [hs.name, self.kvw.name], [pb.name])
                        ob = self.kvo[ti_ % 2]
                        self.cp(ob.ap, pb.ap, [pb.name], [ob.name], eng="act" if ti_ % 2 else "dve")
                        P.dma(kvp[g][ti_ * 128:(ti_ + 1) * 128, s_ - 1, half * 4:(half + 1) * 4, :].rearrange("t h d -> t (h d)"), ob.ap,
                              [ob.name], ["kvp%d" % g], q="pool", join=True)
        self.stage_end()

    def st_ret(self, hT):
        P, T, TT = self.P, self.T, self.TT
        w_in = self.din("ret_w_in", [1, D, 12288])[0]
        gn_g = self.din("ret_gn_g", [1, 4096])
        w_out = self.din("ret_w_out", [1, 4096, D])[0]
        st_in = self.din("state_ret", [1, 8, 256, 512])[0]
        ret_p = self.dout("ret_p", [1, 8, 256, 512])[0]
        ret_s = self.dout("ret_s", [1, 8, 256, 512])[0]
        zT = self.scratch("zT", [32, 128, TT], BF16)
        winv = w_in.rearrange("(k p) f -> p k f", p=128)
        NCK = T // 128
        hs = self.ar.bf16(KT * TT)
        P.dma(hs.v(0, [(TT, KT), (1, TT)]), hT.rearrange("k p t -> p k t"), [hT.tensor.name], [hs.name])
        wb = self.ar.bf16(KT * 512)
        wst = self.ar.f32(D)
        qT = self.ar.bf16(2 * T)
        kT = self.ar.bf16(2 * T)
        ktok = self.ar.bf16(NCK * 256)
        vtok = self.ar.bf16(NCK * 512)
        sg = self.ar.bf16((NCK + 1) * 512)
        S = self.ar.f32(1024)
        Sb = self.ar.bf16(1024)
        attTb = self.ar.bf16(128)
        qd = self.ar.bf16(256)
        osb = self.ar.f32(512)
        cen = self.ar.f32(512)
        zb = self.ar.bf16(512)
        zst = self.ar.bf16(512)
        dmaskT = self.ar.f32(128)
        qdecT = self.ar.f32(128)
        Dcl = self.ar.f32(128)
        msk = self.ar.f32(128)
        gng = self.ar.f32(512)
        vrow = self.ar.f32(512)
        cols = self.ar.f32(8)
        stat = self.ar.f32(8)
        ti = self.ar.f32(128)
        tii = Buf(ti.ap.bitcast(I32), ti.name)
        P.op("pool", lambda e: e.iota(tii.ap, [[1, 128]], base=0, channel_multiplier=-1), (), [ti.name])
        self.cp(Dcl.ap, tii.ap, [ti.name], [Dcl.name])
        self.ts(msk.ap, Dcl.ap, 0.0, ALU.is_ge, [Dcl.name], [msk.name])
        self.ts(Dcl.ap, Dcl.ap, 0.0, ALU.max, [Dcl.name, msk.name], [Dcl.name])
        P.op("pool", lambda e: e.iota(tii.v(0, [(1, 1)]), [[1, 1]], base=127, channel_multiplier=-1), (), [ti.name])
        self.cp(cols.v(5, [(1, 1)]), tii.v(0, [(1, 1)]), [ti.name], [cols.name])
        P.op("pool", lambda e: e.iota(tii.ap, [[1, 128]], base=1, channel_multiplier=0), (), [ti.name])
        cp1 = self.ar.f32(128)
        self.cp(cp1.ap, tii.ap, [ti.name], [cp1.name])

        def load_w(col0, ncols):
            per = 2048 // ncols
            for pc in range(KT // per):
                P.dma(wst.v(0, [(ncols, per), (1, ncols)]), winv[:, pc * per:(pc + 1) * per, col0:col0 + ncols], (), [wst.name])
                self.cp(wb.v(pc * per * ncols, [(1, per * ncols)]), wst.v(0, [(1, per * ncols)]), [wst.name], [wb.name], eng="pool")

        for h in range(8):
            gam = 1.0 - 2.0 ** (-5.0 - h)
            lg = math.log(gam)
            cdec = gam ** 128
            self.act(dmaskT.ap, Dcl.ap, AF.Exp, [Dcl.name], [dmaskT.name], scale=lg)
            self.tt(dmaskT.ap, dmaskT.ap, msk.ap, ALU.mult, [dmaskT.name, msk.name], [dmaskT.name])
            self.act(qdecT.ap, cp1.ap, AF.Exp, [cp1.name], [qdecT.name], scale=lg)
            self.act(cols.v(4, [(1, 1)]), cols.v(5, [(1, 1)]), AF.Exp, [cols.name], [cols.name], scale=lg)
            self.ts(cols.v(4, [(1, 1)]), cols.v(4, [(1, 1)]), 1.0 / 16.0, ALU.mult, [cols.name], [cols.name])
            P.dma(gng.ap, gn_g[0:1, h * 512:(h + 1) * 512].to_broadcast([128, 512]), (), [gng.name])
            load_w(h * 256, 256)
            nb = 0
            for dkt in range(2):
                for (t0, w) in blocks_of(T):
                    pb = self.pb[nb % 2]
                    nb += 1
                    for k in range(KT):
                        self.mm(pb.v(0, [(1, w)]), wb.v(k * 256 + dkt * 128, [(1, 128)]), hs.v(k * TT + t0, [(1, w)]), k == 0, k == KT - 1,
                                [wb.name, hs.name], [pb.name])
                    if t0 < T:
                        self.cp(qT.v(dkt * T + t0, [(1, w)]), pb.v(0, [(1, w)]), [pb.name], [qT.name], eng="act")
                    else:
                        self.cp(cols.v(dkt, [(1, 1)]), pb.v(0, [(1, 1)]), [pb.name], [cols.name])
            load_w(2048 + h * 256, 256)
            for dkt in range(2):
                for (t0, w) in blocks_of(T):
                    pb = self.pb[nb % 2]
                    nb += 1
                    for k in range(KT):
                        self.mm(pb.v(0, [(1, w)]), wb.v(k * 256 + dkt * 128, [(1, 128)]), hs.v(k * TT + t0, [(1, w)]), k == 0, k == KT - 1,
                                [wb.name, hs.name], [pb.name])
                    if t0 < T:
                        self.act(kT.v(dkt * T + t0, [(1, w)]), pb.v(0, [(1, w)]), AF.Copy, [pb.name], [kT.name], scale=1.0 / 16.0)
                    else:
                        self.ts(cols.v(2 + dkt, [(1, 1)]), pb.v(0, [(1, 1)]), 1.0 / 16.0, ALU.mult, [pb.name], [cols.name])
            for c in range(NCK):
                pb = self.pb[2 + c % 2]
                for k in range(KT):
                    self.mm(pb.v(0, [(1, 256)]), hs.v(k * TT + c * 128, [(1, 128)]), wb.v(k * 256, [(1, 256)]), k == 0, k == KT - 1,
                            [wb.name, hs.name], [pb.name])
                self.ts(ktok.v(c * 256, [(1, 256)]), pb.v(0, [(1, 256)]), cols.v(4, [(1, 1)]), ALU.mult, [pb.name, cols.name], [ktok.name])
            load_w(4096 + h * 512, 512)
            for c in range(NCK + 1):
                pb = self.pb[2 + c % 2]
                lhs = (lambda k: hs.v(k * TT + c * 128, [(1, 128)])) if c < NCK else (lambda k: hs.v(k * TT + T, [(0, 128)]))
                for k in range(KT):
                    self.mm(pb.ap, lhs(k), wb.v(k * 512, [(1, 512)]), k == 0, k == KT - 1, [wb.name, hs.name], [pb.name])
                if c < NCK:
                    self.cp(vtok.v(c * 512, [(1, 512)]), pb.ap, [pb.name], [vtok.name], eng="act" if c % 2 else "dve")
                else:
                    self.cp(vrow.ap, pb.ap, [pb.name], [vrow.name])
            load_w(8192 + h * 512, 512)
            for c in range(NCK + 1):
                pb = self.pb[2 + c % 2]
                lhs = (lambda k: hs.v(k * TT + c * 128, [(1, 128)])) if c < NCK else (lambda k: hs.v(k * TT + T, [(0, 128)]))
                for k in range(KT):
                    self.mm(pb.ap, lhs(k), wb.v(k * 512, [(1, 512)]), k == 0, k == KT - 1, [wb.name, hs.name], [pb.name])
                self.act(sg.v(c * 512, [(1, 512)]), pb.ap, AF.Silu, [pb.name], [sg.name])
            self.memset(S.ap, 0.0, [S.name])
            self.memset(Sb.ap, 0.0, [Sb.name])

            def post(pO, m, c, tcol):
                pv = lambda b, o=0, n=512: b.v(o, [(1, n)], 0, m)
                sv = lambda j: stat.v(j, [(1, 1)], 0, m)
                self.act(pv(osb), pv(pO), AF.Identity, [pO.name], [osb.name, stat.name], accum_out=sv(0))
                self.ts(sv(1), sv(0), -1.0 / 512.0, ALU.mult, [stat.name], [stat.name])
                self.ts(pv(cen), pv(osb), sv(1), ALU.add, [osb.name, stat.name], [cen.name])
                self.act(pv(osb), pv(cen), AF.Square, [cen.name], [osb.name, stat.name], accum_out=sv(2))
                self.act(sv(3), sv(2), AF.Sqrt, [stat.name], [stat.name], bias=EPS, scale=1.0 / 512.0)
                P.op("dve", lambda e, o=sv(3): e.reciprocal(o, o), [stat.name], [stat.name])
                self.stt(pv(cen), pv(cen), sv(3), pv(gng), ALU.mult, ALU.mult, [cen.name, stat.name, gng.name], [cen.name])
                self.tt(pv(zb), pv(cen), pv(sg, c * 512), ALU.mult, [cen.name, sg.name], [zb.name])
                pt = self.pb[6]
                ptb = Buf(pt.ap.bitcast(BF16), pt.name)
                for et in range(4):
                    self.tr(ptb.v(et * 128, [(1, m)]), zb.v(et * 128, [(1, 128)], 0, m), self.identb.v(0, [(1, m)], 0, m),
                            [zb.name, self.identb.name], [pt.name])
                self.cp(zst.v(0, [(128, 4), (1, m)]), ptb.v(0, [(128, 4), (1, m)]), [pt.name], [zst.name], eng="act")
                P.dma(zT[h * 4:(h + 1) * 4].rearrange("e p t -> p e t")[:, :, tcol:tcol + m], zst.v(0, [(128, 4), (1, m)]), [zst.name], ["zT"],
                      q="pool", join=True)

            for c in range(NCK):
                pA = self.pb[0]
                for dkt in range(2):
                    self.mm(pA.v(0, [(1, 128)]), kT.v(dkt * T + c * 128, [(1, 128)]), qT.v(dkt * T + c * 128, [(1, 128)]), dkt == 0, dkt == 1,
                            [kT.name, qT.name], [pA.name])
                self.tt(attTb.ap, pA.v(0, [(1, 128)]), dmaskT.ap, ALU.mult, [pA.name, dmaskT.name], [attTb.name])
                self.tt(qd.v(0, [(128, 2), (1, 128)]), qT.v(c * 128, [(T, 2), (1, 128)]), qdecT.v(0, [(0, 2), (1, 128)]), ALU.mult,
                        [qT.name, qdecT.name], [qd.name])
                pO = self.pb[4 + c % 2]
                self.mm(pO.ap, attTb.ap, vtok.v(c * 512, [(1, 512)]), True, False, [attTb.name, vtok.name], [pO.name])
                for dkt in range(2):
                    self.mm(pO.ap, qd.v(dkt * 128, [(1, 128)]), Sb.v(dkt * 512, [(1, 512)]), False, dkt == 1, [qd.name, Sb.name], [pO.name])
                for dkt in range(2):
                    pSt = self.pb[2 + dkt]
                    self.mm(pSt.ap, ktok.v(c * 256 + dkt * 128, [(1, 128)]), vtok.v(c * 512, [(1, 512)]), True, True, [ktok.name, vtok.name], [pSt.name])
                    sv_ = S.v(dkt * 512, [(1, 512)])
                    self.stt(sv_, sv_, cdec, pSt.ap, ALU.mult, ALU.add, [S.name, pSt.name], [S.name])
                self.cp(Sb.ap, S.ap, [S.name], [Sb.name], eng="pool")
                post(pO, 128, c, c * 128)
            P.dma(ret_p[h].rearrange("(t p) e -> p t e", p=128), S.v(0, [(512, 2), (1, 512)]), [S.name], ["ret_p"], q="pool", join=True)
            P.dma(S.v(0, [(512, 2), (1, 512)]), st_in[h].rearrange("(t p) e -> p t e", p=128), (), [S.name])
            pO = self.pb[4]
            for dkt in range(2):
                self.mm(pO.v(0, [(1, 512)], 0, 1), cols.v(dkt, [(1, 1)]), S.v(dkt * 512, [(1, 512)]), dkt == 0, dkt == 1, [cols.name, S.name], [pO.name])
            pq = self.pb[5]
            for dkt in range(2):
                self.mm(pq.v(0, [(1, 1)], 0, 1), cols.v(dkt, [(1, 1)]), cols.v(2 + dkt, [(1, 1)]), dkt == 0, dkt == 1, [cols.name], [pq.name])
            self.cp(stat.v(4, [(1, 1)], 0, 1), pq.v(0, [(1, 1)], 0, 1), [pq.name], [stat.name])
            orow = self.pb[7]
            self.ts(cen.v(0, [(1, 512)], 0, 1), vrow.v(0, [(1, 512)], 0, 1), stat.v(4, [(1, 1)], 0, 1), ALU.mult, [vrow.name, stat.name], [cen.name])
            self.stt(osb.v(0, [(1, 512)], 0, 1), pO.v(0, [(1, 512)], 0, 1), gam, cen.v(0, [(1, 512)], 0, 1), ALU.mult, ALU.add,
                     [pO.name, cen.name], [osb.name])
            for dkt in range(2):
                sv_ = S.v(dkt * 512, [(1, 512)])
                self.ts(sv_, sv_, gam, ALU.mult, [S.name, pO.name], [S.name])
                self.stt(sv_, vrow.ap, cols.v(2 + dkt, [(1, 1)]), sv_, ALU.mult, ALU.add, [vrow.name, cols.name, S.name], [S.name])
            P.dma(ret_s[h].rearrange("(t p) e -> p t e", p=128), S.v(0, [(512, 2), (1, 512)]), [S.name], ["ret_s"], q="pool", join=True)
            post(osb, 1, NCK, T)
        self.stage_end()
        xtv = self.xT.rearrange("k p t -> p k t")
        wov = w_out.rearrange("(e p) f -> p e f", p=128)
        zb_ = self.ar.bf16(32 * 512)
        wst = [self.ar.f32(32 * 128) for _ in range(2)]
        wbo = [self.ar.bf16(32 * 128) for _ in range(2)]
        xb = [self.ar.f32(512) for _ in range(2)]
        for (t0, w) in blocks_of(T):
            col = 1 if t0 == T else 0
            P.dma(zb_.v(0, [(512, 32), (1, w)]), zT.rearrange("e p t -> p e t")[:, :, t0:t0 + w], ["zT"], [zb_.name])
            for ft in range(KT):
                s2 = ft % 2
                P.dma(wst[s2].v(0, [(128, 32), (1, 128)]), wov[:, :, ft * 128:(ft + 1) * 128], (), [wst[s2].name])
                self.cp(wbo[s2].ap, wst[s2].ap, [wst[s2].name], [wbo[s2].name], eng="pool")
                P.dma(xb[s2].v(0, [(1, w)]), xtv[:, ft, t0:t0 + w], ["xT"], [xb[s2].name], q="pool")
                pb = self.pb[s2]
                for e_ in range(32):
                    self.mm(pb.v(0, [(1, w)]), wbo[s2].v(e_ * 128, [(1, 128)]), zb_.v(e_ * 512, [(1, w)]), e_ == 0, e_ == 31, [wbo[s2].name, zb_.name], [pb.name])
                xv = xb[s2].v(0, [(1, w)])
                self.stt(xv, pb.v(0, [(1, w)]), self.modT.v(2 * 32 + 2 * ft + col, [(1, 1)]), xv, ALU.mult, ALU.add, [pb.name, self.modT.name, xb[s2].name], [xb[s2].name])
                P.dma(xtv[:, ft, t0:t0 + w], xv, [xb[s2].name], ["xT"], q="pool", join=True)
        self.stage_end()


    def st_split(self):
        P, T = self.P, self.T
        H = T // 2
        hf = self.din("hf", [1, 2])
        fl = self.cst_tmp("hf", 2)
        P.dma(fl.ap, hf.to_broadcast([128, 2]), (), [fl.name])
        x3 = self.scratch("xT3", [KT, 128, H + 1])
        xtv = self.xT.rearrange("k p t -> p k t")
        x3v = x3.rearrange("k p t -> p k t")
        for k in range(KT):
            a = self.ar.f32(H)
            b = self.ar.f32(H)
            P.dma(a.ap, xtv[:, k, 0:H], ["xT"], [a.name])
            P.dma(b.ap, xtv[:, k, H:T], ["xT"], [b.name], q="pool")
            self.ts(a.ap, a.ap, fl.v(0, [(1, 1)]), ALU.mult, [a.name, fl.name], [a.name])
            self.stt(b.ap, b.ap, fl.v(1, [(1, 1)]), a.ap, ALU.mult, ALU.add, [a.name, b.name, fl.name], [b.name])
            P.dma(x3v[:, k, 0:H], b.ap, [b.name], ["xT3"], q="pool", join=True)
            if k % 4 == 3:
                self.ar.reset()
        sc_ = self.cst_tmp("xs_s", KT)
        P.dma(sc_.v(0, [(1, KT), (1, 1)]), xtv[:, :, T:T + 1], ["xT"], [sc_.name])
        P.dma(x3v[:, :, H:H + 1], sc_.v(0, [(1, KT), (1, 1)]), [sc_.name], ["xT3"], q="pool", join=True)
        self.stage_end()
        self.T, self.TT, self.xT = H, H + 1, x3


def build(T=2048, layers=4, split=True):
    k = K(T)
    k.setup_consts()
    k.stage_end()
    k.st_loadx()
    k.st_mod_setup()
    hT = k.scratch("hT", [KT, 128, k.TT], BF16)
    h32T = k.scratch("h32T", [KT, 128, k.TT])
    for i in range(layers):
        kind, j = i % 3, i // 3
        k.st_mod(i)
        if kind == 0:
            k.st_norm(k.A1, 0, None, h32T)
            k.st_pool(j, h32T)
        elif kind == 1:
            k.st_norm(k.A1, 0, hT)
            k.st_dil(hT)
        else:
            k.st_norm(k.A1, 0, hT)
            k.st_ret(hT)
        if i == layers - 1 and split:
            k.st_split()
            hT = k.scratch("hT3", [KT, 128, k.TT], BF16)
        k.st_norm(k.A2, 3, hT)
        k.st_peer_route(i, hT)
        k.st_peer_main(i, hT)
    k.st_final()
    nc = k.P.emit()
    return k, nc


_CACHE = {}


def kernel(x_prompt, x_sample, state_pool, cache_dil_kv0, cache_dil_kv1, cache_dil_kv2, state_ret,
           c_prompt, c_sample, norm1_g, norm2_g, mod_w, mod_b, pool_w, pool_scale,
           dil_w_in, dil_w_out, ret_w_in, ret_gn_g, ret_w_out,
           peer_w_q, peer_keys, peer_u, peer_v, final_g):
    f = lambda a: np.ascontiguousarray(np.asarray(a), dtype=np.float32)
    T = x_prompt.shape[1]
    k, nc = build(T)
    shared = dict(mod_w=f(mod_w), mod_b=f(mod_b), norm1_g=f(norm1_g), norm2_g=f(norm2_g), pool_w=f(pool_w), pool_scale=f(pool_scale),
                  dil_w_in=f(dil_w_in), dil_w_out=f(dil_w_out), ret_w_in=f(ret_w_in), ret_gn_g=f(ret_gn_g), ret_w_out=f(ret_w_out),
                  peer_w_q=f(peer_w_q), peer_keys=f(peer_keys), peer_u=f(peer_u), peer_v=f(peer_v), final_g=f(final_g))
    caches = [f(cache_dil_kv0), f(cache_dil_kv1), f(cache_dil_kv2)]
    x_prompt, x_sample, state_pool, state_ret = f(x_prompt), f(x_sample), f(state_pool), f(state_ret)
    c_prompt, c_sample = f(c_prompt), f(c_sample)
    in_maps = []
    for ci in range(8):
        b, s = ci // 2, ci
        m = dict(shared)
        m.update(x_p=x_prompt[b], x_s=x_sample[s], c_p=c_prompt[b], c_s=c_sample[s],
                 state_pool=np.ascontiguousarray(state_pool[:, s]), state_ret=np.ascontiguousarray(state_ret[:, s]))
        for g in range(3):
            m["cache_dil_kv%d" % g] = np.ascontiguousarray(caches[g][:, s])
        m["hf"] = np.array([[1.0 - ci % 2, ci % 2]], dtype=np.float32)
        in_maps.append({n: m[n] for n in k.inp})
    res = run_bass_kernel_spmd(nc, in_maps, core_ids=list(range(8)))
    R = res.results
    B = x_prompt.shape[0]
    half = T // 2

    def prompt(name):
        return np.stack([R[2 * b][name] for b in range(B)], axis=0)

    y_prompt = np.stack([np.concatenate([R[2 * b]["y_p"], R[2 * b + 1]["y_p"]], axis=0) for b in range(B)], axis=0)
    y_sample = np.stack([R[s]["y_s"] for s in range(8)], axis=0)
    pool_p = np.stack([R[2 * b]["pool_p"] for b in range(B)], axis=1)
    pool_s = np.stack([R[s]["pool_s"] for s in range(8)], axis=1)
    outs = [y_prompt, y_sample, pool_p, pool_s]
    for g in range(3):
        outs.append(np.stack([R[2 * b + (g % 2)]["kv%d_p" % g] for b in range(B)], axis=1))
        outs.append(np.stack([R[s]["kv%d_s" % g] for s in range(8)], axis=1))
    outs.append(np.stack([R[2 * b + 1]["ret_p"] for b in range(B)], axis=1))
    outs.append(np.stack([R[s]["ret_s"] for s in range(8)], axis=1))
    return tuple(np.ascontiguousarray(o, dtype=np.float32) for o in outs)
```

```python
from contextlib import ExitStack
import math
import numpy as np
import concourse.bass as bass
import concourse.mybir as mybir
from concourse.ap import AP
from concourse.bass_utils import run_bass_kernel_spmd

F32 = mybir.dt.float32
BF16 = mybir.dt.bfloat16
U32 = mybir.dt.uint32
I32 = mybir.dt.int32
AF = mybir.ActivationFunctionType
ALU = mybir.AluOpType
AX = mybir.AxisListType

ENGS = ("pe", "dve", "act", "pool", "sp")
DMAQ = ("sp", "pool")
NDMA = 24
D = 2048
KT = 16
EPS = 1e-6


class Op:
    __slots__ = ("eng", "fn", "deps", "dma", "tok", "need", "pre")


class Prog:
    def __init__(self):
        self.nc = bass.Bass("TRN2", target_bir_lowering=False)
        self.es = ExitStack()
        self.ops = []
        self.lastw = {}
        self.readers = {}
        self.last_eng = {}
        self.recent_dma = {q: [] for q in DMAQ}
        self.uid = 0

    def sb(self, name, shape, dt=F32):
        return self.es.enter_context(self.nc.sbuf_tensor(name, list(shape), dt))

    def ps(self, name, shape, dt=F32):
        return self.es.enter_context(self.nc.psum_tensor(name, list(shape), dt))

    def dram(self, name, shape, dt=F32, kind="Internal"):
        return self.nc.dram_tensor(name, list(shape), dt, kind=kind).ap()

    def op(self, eng, fn, r=(), w=(), dma=False, join=False, extra=()):
        o = Op()
        o.eng, o.fn, o.dma, o.need, o.tok, o.pre = eng, fn, dma, False, None, None
        deps = set(extra)
        for x in r:
            deps.update(self.lastw.get(x, ()))
        for x in w:
            deps.update(self.lastw.get(x, ()))
            deps.update(self.readers.get(x, ()))
        if eng == "pe":
            deps = {d for d in deps if d.eng != "pe"}
        o.deps = deps
        for d in deps:
            d.need = True
        for x in r:
            lst = self.readers.setdefault(x, [])
            if not dma:
                lst[:] = [q for q in lst if q.dma or q.eng != eng]
            lst.append(o)
        for x in w:
            if join and x in self.lastw:
                self.lastw[x].append(o)
            else:
                self.lastw[x] = [o]
            self.readers[x] = []
        self.ops.append(o)
        if dma:
            self.recent_dma[eng].append(o)
            if len(self.recent_dma[eng]) > NDMA:
                self.recent_dma[eng].pop(0)
        else:
            self.last_eng[eng] = o
        return o

    def dma(self, out, in_, r=(), w=(), q="sp", join=False, **kw):
        kw.setdefault("allow_slow_non_contiguous", True)
        return self.op(q, lambda e: e.dma_start(out=out, in_=in_, **kw), r, w, dma=True, join=join)

    def barrier(self):
        deps = list(self.last_eng.values())
        for q in DMAQ:
            deps += self.recent_dma[q]
        for e in ENGS:
            self.op(e, lambda h: h.nop(), extra=deps)
        self.lastw = {}
        self.readers = {}

    def emit(self):
        nc = self.nc
        es = self.es
        sems = {e: es.enter_context(nc.semaphore("s_" + e)) for e in ENGS}
        dsem = {q: [es.enter_context(nc.semaphore("d_%s%d" % (q, i))) for i in range(NDMA)] for q in DMAQ}
        cnt = {e: 0 for e in ENGS}
        dcnt = {q: 0 for q in DMAQ}
        duse = {q: [0] * NDMA for q in DMAQ}
        for o in self.ops:
            if o.dma:
                i = dcnt[o.eng] % NDMA
                dcnt[o.eng] += 1
                prev = duse[o.eng][i]
                duse[o.eng][i] += 16
                o.pre = (dsem[o.eng][i], prev) if prev else None
                o.tok = (dsem[o.eng][i], prev + 16)
            elif o.need:
                cnt[o.eng] += 1
                o.tok = (sems[o.eng], cnt[o.eng])
        self.sem_counts = dict(cnt)
        per = {e: [o for o in self.ops if o.eng == e] for e in ENGS}
        block = es.enter_context(nc.Block())

        def run(engname, handle):
            known = {}
            for o in per[engname]:
                ws = []
                if o.pre is not None:
                    ws.append(o.pre)
                for d in o.deps:
                    ws.append(d.tok)
                for (s, v) in ws:
                    k = id(s)
                    if known.get(k, 0) >= v:
                        continue
                    known[k] = v
                    handle.wait_ge(s, v)
                inst = o.fn(handle)
                if o.tok is not None:
                    inst.then_inc(o.tok[0], 16 if o.dma else 1)
            if engname in dsem:
                for i in range(NDMA):
                    if duse[engname][i]:
                        handle.wait_ge(dsem[engname][i], duse[engname][i])

        @block.tensor
        def _(e):
            run("pe", e)

        @block.vector
        def _(e):
            run("dve", e)

        @block.scalar
        def _(e):
            run("act", e)

        @block.gpsimd
        def _(e):
            run("pool", e)

        @block.sync
        def _(e):
            run("sp", e)

        es.close()
        return nc


class Buf:
    def __init__(self, ap2d, name):
        self.ap = ap2d
        self.name = name
        self.t = ap2d.tensor
        self.off = ap2d.offset
        self.ps = ap2d.ap[0][0]
        self.n = ap2d.shape[1]

    def v(self, off=0, dims=None, p0=0, npart=128):
        if dims is None:
            dims = [(1, self.n - off)]
        return AP(self.t, self.off + p0 * self.ps + off, [[self.ps, npart]] + [[s, n] for (s, n) in dims])

    def __getitem__(self, k):
        return self.ap[k]


class Arena:
    def __init__(self, P, name, words):
        self.P = P
        self.t = P.sb(name, [128, words], F32)
        self.words = words
        self.off = 0
        self.gen = 0
        self.name = name

    def reset(self):
        self.off = 0
        self.gen += 1

    def f32(self, n, tag=""):
        assert self.off + n <= self.words, ("arena overflow", self.name, self.off, n, self.words)
        b = Buf(self.t[:, self.off:self.off + n], "%s_%d" % (self.name, self.off))
        self.off += n
        return b

    def bf16(self, n, tag=""):
        nw = (n + 1) // 2
        assert self.off + nw <= self.words, ("arena overflow", self.name, self.off, nw, self.words)
        b = Buf(self.t[:, self.off:self.off + nw].bitcast(BF16), "%s_%d" % (self.name, self.off))
        self.off += nw
        return b

    def u32(self, n, tag=""):
        assert self.off + n <= self.words
        b = Buf(self.t[:, self.off:self.off + n].bitcast(U32), "%s_%d" % (self.name, self.off))
        self.off += n
        return b


def blocks_of(T):
    bl = [(t0, min(512, T - t0)) for t0 in range(0, T, 512)]
    return bl + [(T, 1)]


class K:
    def __init__(self, T, dbg=()):
        self.T = T
        self.TT = T + 1
        self.P = Prog()
        self.dbg = set(dbg)
        P = self.P
        nc = P.nc
        self.inp = {}
        self.out = {}
        self.ar = Arena(P, "ar", 45 * 1024)
        self.cst = Arena(P, "cst", 2 * 1024)
        self.pb = [Buf(P.ps("pb%d" % i, [128, 512], F32)[:, :], "pb%d" % i) for i in range(8)]

    def din(self, name, shape, dt=F32):
        a = self.P.nc.dram_tensor(name, list(shape), dt, kind="ExternalInput").ap()
        self.inp[name] = a
        return a

    def dout(self, name, shape, dt=F32):
        a = self.P.nc.dram_tensor(name, list(shape), dt, kind="ExternalOutput").ap()
        self.out[name] = a
        return a

    def scratch(self, name, shape, dt=F32):
        if name in self.dbg:
            return self.dout(name, shape, dt)
        return self.P.dram(name, shape, dt)

    def mm(self, out, lhsT, rhs, start, stop, r, w):
        self.P.op("pe", lambda e: e.matmul(out, lhsT, rhs, start=start, stop=stop), r, w)

    def tr(self, out, in_, ident, r, w):
        self.P.op("pe", lambda e: e.transpose(out, in_, ident), r, w)

    def act(self, out, in_, func, r, w, bias=0.0, scale=1.0, accum_out=None):
        if accum_out is None:
            self.P.op("act", lambda e: e.activation(out, in_, func, bias=bias, scale=scale), r, w)
        else:
            self.P.op("act", lambda e: e.activation(out, in_, func, bias=bias, scale=scale, accum_out=accum_out), r, w)

    def tt(self, out, in0, in1, op, r, w, eng="dve"):
        self.P.op(eng, lambda e: e.tensor_tensor(out, in0, in1, op), r, w)

    def ts(self, out, in0, s1, op0, r, w, s2=None, op1=None, eng="dve"):
        if op1 is None:
            self.P.op(eng, lambda e: e.tensor_scalar(out, in0, s1, None, op0), r, w)
        else:
            self.P.op(eng, lambda e: e.tensor_scalar(out, in0, s1, s2, op0, op1), r, w)

    def stt(self, out, in0, scalar, in1, op0, op1, r, w):
        self.P.op("dve", lambda e: e.scalar_tensor_tensor(out, in0, scalar, in1, op0, op1), r, w)

    def cp(self, out, in_, r, w, eng="dve"):
        if eng == "act":
            self.P.op("act", lambda e: e.copy(out, in_), r, w)
        else:
            self.P.op(eng, lambda e: e.tensor_copy(out, in_), r, w)

    def memset(self, ap, val, w, eng="dve"):
        self.P.op(eng, lambda e: e.memset(ap, val), (), w)

    def stage_end(self):
        self.P.barrier()
        self.ar.reset()

    def setup_consts(self):
        P, c = self.P, self.cst
        self.identf = c.f32(128, "idf")
        self.identb = c.bf16(128, "idb")
        self.onesf = c.f32(128, "onf")
        self.onesb = c.bf16(128, "onb")
        self.iotaf = c.f32(128, "iof")
        tmp = self.ar.f32(128)
        tmpi = Buf(tmp.ap.bitcast(I32), tmp.name)
        P.op("pool", lambda e: e.iota(tmpi.ap, [[1, 128]], base=0, channel_multiplier=-1), (), [tmp.name])
        self.ts(self.identf.ap, tmpi.ap, 0.0, ALU.is_equal, [tmp.name], [self.identf.name])
        self.cp(self.identb.ap, self.identf.ap, [self.identf.name], [self.identb.name])
        self.memset(self.onesf.ap, 1.0, [self.onesf.name])
        self.memset(self.onesb.ap, 1.0, [self.onesb.name])
        tmp2 = self.ar.f32(128)
        tmp2i = Buf(tmp2.ap.bitcast(I32), tmp2.name)
        P.op("pool", lambda e: e.iota(tmp2i.ap, [[1, 128]], base=0, channel_multiplier=0), (), [tmp2.name])
        self.cp(self.iotaf.ap, tmp2i.ap, [tmp2.name], [self.iotaf.name])

    def load_vecT(self, dst, dname, src_ap, n):
        raw = self.ar.f32(128)
        pb = self.pb[7]
        self.P.dma(raw.v(0, [(1, 128)], 0, n), src_ap.rearrange("(k p) -> k p", p=128), (), [raw.name])
        self.tr(pb.v(0, [(1, n)]), raw.v(0, [(1, 128)], 0, n), self.identf.v(0, [(1, n)], 0, n), [raw.name, self.identf.name], [pb.name])
        self.cp(dst, pb.v(0, [(1, n)]), [pb.name], [dname])

    def st_loadx(self):
        T, P = self.T, self.P
        x_p = self.din("x_p", [T, D])
        x_s = self.din("x_s", [1, D])
        self.xT = self.scratch("xT", [KT, 128, self.TT])
        xtv = self.xT.rearrange("k p t -> p k t")
        for ti in range(T // 128):
            raw = self.ar.f32(D)
            xt = self.ar.f32(D)
            P.dma(raw.ap, x_p[ti * 128:(ti + 1) * 128, :], (), [raw.name])
            for q in range(4):
                pb = self.pb[q]
                for j in range(4):
                    k = q * 4 + j
                    self.tr(pb.v(j * 128, [(1, 128)]), raw.v(k * 128, [(1, 128)]), self.identf.ap, [raw.name, self.identf.name], [pb.name])
                self.cp(xt.v(q * 512, [(1, 512)]), pb.ap, [pb.name], [xt.name], eng="act" if q % 2 else "dve")
            P.dma(xtv[:, :, ti * 128:(ti + 1) * 128], xt.v(0, [(128, KT), (1, 128)]), [xt.name], ["xT"], q="pool", join=True)
            if ti % 2 == 1:
                self.ar.reset()
        self.stage_end()
        col = self.ar.f32(KT)
        self.load_vecT(col.ap, col.name, x_s[0, :], KT)
        P.dma(xtv[:, :, T:T + 1], col.v(0, [(1, KT), (1, 1)]), [col.name], ["xT"], q="pool", join=True)
        self.stage_end()

    def st_mod_setup(self):
        c_p = self.din("c_p", [D])
        c_s = self.din("c_s", [D])
        self.mod_w = self.din("mod_w", [4, D, 6 * D])
        self.mod_b = self.din("mod_b", [4, 6 * D])
        self.norm1_g = self.din("norm1_g", [4, D])
        self.norm2_g = self.din("norm2_g", [4, D])
        self.sc = self.cst.f32(KT * 2, "sc")
        self.modT = self.cst.f32(96 * 2, "modT")
        self.A1 = self.cst.f32(KT * 2, "A1")
        self.A2 = self.cst.f32(KT * 2, "A2")
        craw = self.ar.f32(KT * 2)
        self.load_vecT(craw.v(0, [(2, KT)]), craw.name, c_p, KT)
        self.load_vecT(craw.v(1, [(2, KT)]), craw.name, c_s, KT)
        self.act(self.sc.ap, craw.ap, AF.Silu, [craw.name], [self.sc.name])
        self.stage_end()

    def st_mod(self, i):
        P = self.P
        pb = self.pb[0]
        wv = self.mod_w[i].rearrange("(k p) f -> p k f", p=128)
        bufs = [self.ar.f32(KT * 512, "mw%d" % j) for j in range(2)]
        for fc in range(24):
            b = bufs[fc % 2]
            P.dma(b.v(0, [(512, KT), (1, 512)]), wv[:, :, fc * 512:(fc + 1) * 512], (), [b.name], q="sp" if fc % 2 == 0 else "pool")
            for ft in range(4):
                f = fc * 4 + ft
                for k in range(KT):
                    self.mm(pb.v(f * 2, [(1, 2)]), b.v(k * 512 + ft * 128, [(1, 128)]), self.sc.v(k * 2, [(1, 2)]),
                            k == 0, k == KT - 1, [b.name, self.sc.name], [pb.name])
        mb = self.ar.f32(96)
        self.load_vecT(mb.ap, mb.name, self.mod_b[i], 96)
        g1 = self.ar.f32(KT)
        g2 = self.ar.f32(KT)
        self.load_vecT(g1.ap, g1.name, self.norm1_g[i], KT)
        self.load_vecT(g2.ap, g2.name, self.norm2_g[i], KT)
        m = self.modT
        self.tt(m.v(0, [(2, 96), (1, 2)]), pb.v(0, [(2, 96), (1, 2)]), mb.v(0, [(1, 96), (0, 2)]), ALU.add,
                [pb.name, mb.name], [m.name])
        self.stt(self.A1.v(0, [(2, KT), (1, 2)]), m.v(1 * 32, [(2, KT), (1, 2)]), 1.0, g1.v(0, [(1, KT), (0, 2)]), ALU.add, ALU.mult,
                 [m.name, g1.name], [self.A1.name])
        self.stt(self.A2.v(0, [(2, KT), (1, 2)]), m.v(4 * 32, [(2, KT), (1, 2)]), 1.0, g2.v(0, [(1, KT), (0, 2)]), ALU.add, ALU.mult,
                 [m.name, g2.name], [self.A2.name])
        self.stage_end()

    def modv(self, idx, col):
        return self.modT.v(idx * 32 + col, [(2, KT)])

    def st_norm(self, A, shift_idx, hT, h32T=None, gvec=None):
        P, T = self.P, self.T
        xtv = self.xT.rearrange("k p t -> p k t")
        hv = hT.rearrange("k p t -> p k t") if hT is not None else None
        h32v = h32T.rearrange("k p t -> p k t") if h32T is not None else None
        for bi, (t0, w) in enumerate(blocks_of(T)):
            col = 1 if t0 == T else 0
            x = self.ar.f32(KT * 512, "x")
            sq = self.ar.f32(KT * 512, "sq")
            rs = self.ar.f32(512, "rs")
            hb = self.ar.bf16(KT * 512, "hb")
            pb = self.pb[bi % 2]
            xv = x.v(0, [(512, KT), (1, w)])
            sqv = sq.v(0, [(512, KT), (1, w)])
            P.dma(xv, xtv[:, :, t0:t0 + w], ["xT"], [x.name])
            self.act(sqv, xv, AF.Square, [x.name], [sq.name])
            for k in range(KT):
                self.mm(pb.v(0, [(1, w)]), self.onesf.ap, sq.v(k * 512, [(1, w)]), k == 0, k == KT - 1,
                        [sq.name, self.onesf.name], [pb.name])
            self.act(rs.v(0, [(1, w)]), pb.v(0, [(1, w)]), AF.Sqrt, [pb.name], [rs.name], bias=EPS, scale=1.0 / D)
            P.op("dve", lambda e, o=rs.v(0, [(1, w)]): e.reciprocal(o, o), [rs.name], [rs.name])
            self.tt(sqv, xv, rs.v(0, [(0, KT), (1, w)]), ALU.mult, [x.name, rs.name], [sq.name])
            if A is not None:
                self.tt(sqv, sqv, A.v(col, [(2, KT), (0, w)]), ALU.mult, [sq.name, A.name], [sq.name])
                self.tt(sqv, sqv, self.modT.v(shift_idx * 32 + col, [(2, KT), (0, w)]), ALU.add, [sq.name, self.modT.name], [sq.name])
            else:
                self.tt(sqv, sqv, gvec.v(0, [(1, KT), (0, w)]), ALU.mult, [sq.name, gvec.name], [sq.name])
            if hv is not None:
                self.cp(hb.v(0, [(512, KT), (1, w)]), sqv, [sq.name], [hb.name], eng="act")
                P.dma(hv[:, :, t0:t0 + w], hb.v(0, [(512, KT), (1, w)]), [hb.name], [hT.tensor.name], q="pool", join=True)
            if h32v is not None:
                P.dma(h32v[:, :, t0:t0 + w], sqv, [sq.name], [h32T.tensor.name], q="pool", join=True)
            if bi % 2 == 1:
                self.ar.reset()
        self.stage_end()

    def fm_to_rows(self, srcT, t0, n, dst_rows, r0=0):
        P = self.P
        src = self.ar.f32(KT * 128, "f2r")
        rows = self.ar.f32(D, "rows")
        P.dma(src.v(0, [(128, KT), (1, n)]), srcT.rearrange("k p t -> p k t")[:, :, t0:t0 + n], [srcT.tensor.name], [src.name])
        for q in range(4):
            pb = self.pb[4 + q]
            for j in range(4):
                k = q * 4 + j
                self.tr(pb.v(j * 128, [(1, 128)], 0, n), src.v(k * 128, [(1, n)]), self.identf.ap, [src.name, self.identf.name], [pb.name])
            self.cp(rows.v(q * 512, [(1, 512)], 0, n), pb.v(0, [(1, 512)], 0, n), [pb.name], [rows.name], eng="act" if q % 2 else "dve")
        P.dma(dst_rows, rows.v(0, [(1, D)], r0, n - r0), [rows.name], [dst_rows.tensor.name], q="pool", join=True)

    def st_final(self):
        T = self.T
        fg = self.din("final_g", [D])
        y_p = self.dout("y_p", [T, D])
        y_s = self.dout("y_s", [1, D])
        g = self.ar.f32(KT)
        self.load_vecT(g.ap, g.name, fg, KT)
        gk = self.cst.f32(KT, "fg")
        self.cp(gk.ap, g.ap, [g.name], [gk.name])
        self.stage_end()
        yT = self.scratch("yT", [KT, 128, self.TT])
        self.st_norm(None, 0, None, yT, gvec=gk)
        for ti in range(T // 128):
            self.fm_to_rows(yT, ti * 128, 128, y_p[ti * 128:(ti + 1) * 128, :])
            if ti % 2 == 1:
                self.ar.reset()
        self.stage_end()
        self.fm_to_rows(yT, T, 1, y_s[0:1, :])
        self.stage_end()

    def dump(self, name, view, shape, r):
        o = self.dout(name, shape)
        self.P.dma(o, view, r, [name], q="sp")

    def st_pool(self, j, h32T):
        P, T, TT = self.P, self.T, self.TT
        if j == 0:
            self.state_pool = self.din("state_pool", [2, 15, D])
            self.pool_w = self.din("pool_w", [2, 4, 512, 512])
            self.pool_scale = self.din("pool_scale", [2, D])
            self.pool_p = self.dout("pool_p", [2, 15, D])
            self.pool_s = self.dout("pool_s", [2, 15, D])
        self.fm_to_rows(h32T, T - 128, 128, self.pool_p[j], r0=113)
        P.dma(self.pool_s[j, 0:14, :], self.state_pool[j, 1:15, :], (), ["pool_s"], q="pool", join=True)
        self.stage_end()
        self.fm_to_rows(h32T, T, 1, self.pool_s[j, 14:15, :])
        self.stage_end()
        ps_ = self.ar.f32(KT)
        self.load_vecT(ps_.ap, ps_.name, self.pool_scale[j], KT)
        sg = self.cst_tmp("sg", KT * 2)
        self.tt(sg.v(0, [(2, KT), (1, 2)]), self.modT.v(2 * 32, [(2, KT), (1, 2)]), ps_.v(0, [(1, KT), (0, 2)]), ALU.mult,
                [self.modT.name, ps_.name], [sg.name])
        ext = self.cst_tmp("ext", KT * 16)
        hraw = self.ar.f32(D)
        P.dma(hraw.v(0, [(1, D)], 0, 15), self.state_pool[j], (), [hraw.name])
        for k in range(KT):
            pb = self.pb[4 + k % 4]
            self.tr(pb.v(0, [(1, 15)]), hraw.v(k * 128, [(1, 128)], 0, 15), self.identf.v(0, [(1, 15)], 0, 15),
                    [hraw.name, self.identf.name], [pb.name])
            self.cp(ext.v(k * 16, [(1, 15)]), pb.v(0, [(1, 15)]), [pb.name], [ext.name])
        P.dma(ext.v(15, [(16, KT), (1, 1)]), h32T.rearrange("k p t -> p k t")[:, :, T:T + 1], [h32T.tensor.name], [ext.name])
        self.stage_end()
        for g in range(4):
            w = 2 << g
            A = self.ar.f32(4 * T)
            B = self.ar.f32(4 * T)
            C = self.ar.f32(4 * T)
            dT = self.ar.bf16(4 * TT)
            X = self.ar.f32(4 * TT)
            Wf = self.ar.f32(4 * 512)
            Wb = self.ar.bf16(4 * 512)
            red = self.ar.f32(4)
            v3 = lambda b, o=0, n=T, s=T: b.v(o, [(s, 4), (1, n)])
            P.dma(v3(A), h32T[4 * g:4 * g + 4].rearrange("k p t -> p k t")[:, :, 0:T], [h32T.tensor.name], [A.name])
            P.dma(v3(X, 0, TT, TT), self.xT[4 * g:4 * g + 4].rearrange("k p t -> p k t"), ["xT"], [X.name], q="pool")
            P.dma(Wf.v(0, [(512, 4), (1, 512)]), self.pool_w[j, g].rearrange("(c p) e -> p c e", p=128), (), [Wf.name])
            self.cp(Wb.ap, Wf.ap, [Wf.name], [Wb.name], eng="act")
            cur = A
            for si in range(g + 1):
                st = 1 << si
                nxt = B if si % 2 == 0 else C
                self.cp(v3(nxt, 0, st), v3(cur, 0, st), [cur.name], [nxt.name], eng="act")
                self.tt(v3(nxt, st, T - st), v3(cur, st, T - st), v3(cur, 0, T - st), ALU.add, [cur.name], [nxt.name])
                cur = nxt
            self.stt(v3(dT, 0, T, TT), v3(cur), 1.0 / w, v3(A), ALU.mult, ALU.subtract, [cur.name, A.name], [dT.name])
            for t in range(w - 1):
                self.stt(v3(dT, t, 1, TT), v3(cur, t, 1), 1.0 / (t + 1), v3(A, t, 1), ALU.mult, ALU.subtract, [cur.name, A.name], [dT.name])
            P.op("dve", lambda e, o=red.ap, i=ext.v(4 * g * 16 + 16 - w, [(16, 4), (1, w)]): e.tensor_reduce(o, i, AX.X, ALU.add),
                 [ext.name], [red.name])
            self.stt(v3(dT, T, 1, TT), red.v(0, [(1, 4), (1, 1)]), 1.0 / w, ext.v(4 * g * 16 + 15, [(16, 4), (1, 1)]), ALU.mult, ALU.subtract,
                     [red.name, ext.name], [dT.name])
            nb = 0
            for et in range(4):
                k = 4 * g + et
                for (t0, wd) in blocks_of(T):
                    pb = self.pb[nb % 4]
                    nb += 1
                    col = 1 if t0 == T else 0
                    for c in range(4):
                        self.mm(pb.v(0, [(1, wd)]), Wb.v(c * 512 + et * 128, [(1, 128)]), dT.v(c * TT + t0, [(1, wd)]), c == 0, c == 3,
                                [Wb.name, dT.name], [pb.name])
                    xv = X.v(et * TT + t0, [(1, wd)])
                    self.stt(xv, pb.v(0, [(1, wd)]), sg.v(k * 2 + col, [(1, 1)]), xv, ALU.mult, ALU.add, [pb.name, sg.name, X.name], [X.name])
            P.dma(self.xT[4 * g:4 * g + 4].rearrange("k p t -> p k t"), v3(X, 0, TT, TT), [X.name], ["xT"], q="pool", join=True)
            self.ar.reset()
        self.stage_end()

    def cst_tmp(self, name, n):
        if not hasattr(self, "_ct"):
            self._ct = {}
        if name not in self._ct:
            self._ct[name] = self.cst.f32(n)
        return self._ct[name]

    def st_peer_route(self, i, hT):
        P, T, TT = self.P, self.T, self.TT
        if not hasattr(self, "peer_wq"):
            self.peer_wq = self.din("peer_w_q", [4, D, 2048])
            self.peer_keys = self.din("peer_keys", [4, 2, 128, 128])
            self.peer_u = self.din("peer_u", [4, 16384, D])
            self.peer_v = self.din("peer_v", [4, 16384, D])
            self.Gsamp = self.cst.bf16(128)
        if getattr(self, "_route_T", None) != T:
            self._route_T = T
            self.scd = self.scratch("scd%d" % T, [T // 128 + 1, 128, 16, 128])
            self.Gd = self.scratch("Gd%d" % T, [128, 128, T], BF16)
        ntile = T // 128 + 1
        hs = self.ar.bf16(KT * TT)
        P.dma(hs.v(0, [(TT, KT), (1, TT)]), hT.rearrange("k p t -> p k t"), [hT.tensor.name], [hs.name])
        keysT = self.ar.f32(256)
        kraw = self.ar.f32(256)
        P.dma(kraw.v(0, [(128, 2), (1, 128)]), self.peer_keys[i].rearrange("a m k -> m a k"), (), [kraw.name])
        for p in range(2):
            pb = self.pb[6 + p]
            self.tr(pb.v(0, [(1, 128)]), kraw.v(p * 128, [(1, 128)]), self.identf.ap, [kraw.name, self.identf.name], [pb.name])
            self.cp(keysT.v(p * 128, [(1, 128)]), pb.v(0, [(1, 128)]), [pb.name], [keysT.name])
        wb = [self.ar.bf16(KT * 128) for _ in range(2)]
        wqv = self.peer_wq[i].rearrange("(k p) f -> p k f", p=128)

        def load_wq(ft_):
            P.dma(wb[ft_ % 2].v(0, [(128, KT), (1, 128)]), wqv[:, :, ft_ * 128:(ft_ + 1) * 128], (), [wb[ft_ % 2].name], q="pool")
        load_wq(0)
        qf = [self.ar.f32(TT) for _ in range(2)]
        stg = [self.ar.f32(ntile * 128) for _ in range(2)]
        for s_ in stg:
            self.memset(s_.ap, 0.0, [s_.name], eng="pool")
        nb = 0
        for ft in range(16):
            s = ft % 2
            if ft + 1 < 16:
                load_wq(ft + 1)
            for (t0, w) in blocks_of(T):
                pb = self.pb[nb % 3]
                nb += 1
                for k in range(KT):
                    self.mm(pb.v(0, [(1, w)]), wb[s].v(k * 128, [(1, 128)]), hs.v(k * TT + t0, [(1, w)]), k == 0, k == KT - 1,
                            [wb[s].name, hs.name], [pb.name])
                self.cp(qf[s].v(t0, [(1, w)]), pb.v(0, [(1, w)]), [pb.name], [qf[s].name], eng="act")
            for tt_ in range(ntile):
                m = 128 if tt_ < ntile - 1 else 1
                pb = self.pb[3 + tt_ % 3]
                self.mm(pb.v(0, [(1, 128)], 0, m), qf[s].v(tt_ * 128, [(1, m)]), keysT.v((ft % 2) * 128, [(1, 128)]), True, True,
                        [qf[s].name, keysT.name], [pb.name])
                self.cp(stg[s].v(tt_ * 128, [(1, 128)], 0, m), pb.v(0, [(1, 128)], 0, m), [pb.name], [stg[s].name])
            P.dma(self.scd[:, :, ft, :].rearrange("n p m -> p n m"), stg[s].v(0, [(128, ntile), (1, 128)]), [stg[s].name], [self.scd.tensor.name],
                  q="pool", join=True)
        self.stage_end()
        NEG = -1.0e30
        for tt_ in range(ntile):
            m = 128 if tt_ < ntile - 1 else 1
            sc = self.ar.f32(2048)
            scr = self.ar.f32(256)
            vals = self.ar.f32(256)
            idx = self.ar.u32(256)
            idxf = self.ar.f32(256)
            cand = self.ar.f32(2048)
            top = self.ar.f32(128)
            sel = self.ar.u32(128)
            selb = self.ar.u32(128)
            af = self.ar.f32(128)
            bf = self.ar.f32(128)
            gg = self.ar.f32(128)
            es = self.ar.f32(8)
            eq = self.ar.f32(2048)
            i1 = self.ar.f32(128)
            i2 = self.ar.f32(128)
            i1T = self.ar.f32(128)
            i2T = self.ar.f32(128)
            gT = self.ar.f32(128)
            oh1 = [self.ar.bf16(8 * 128) for _ in range(6)]
            oh2 = [self.ar.bf16(8 * 128) for _ in range(3)]
            Gs = self.ar.bf16(128 * 128)
            P.dma(sc.ap, self.scd[tt_].rearrange("p f m -> p (f m)"), [self.scd.tensor.name], [sc.name])
            V = lambda b, o, n: b.v(o, [(1, n)])
            for ft in range(16):
                s_in = V(sc, ft * 128, 128)
                P.op("dve", lambda e, o=V(vals, ft * 16, 8), i=s_in: e.max(out=o, in_=i), [sc.name], [vals.name])
                P.op("dve", lambda e, o=V(idx, ft * 16, 8), mx=V(vals, ft * 16, 8), i=s_in: e.max_index(o, mx, i), [sc.name, vals.name], [idx.name])
                P.op("dve", lambda e, o=V(scr, 0, 128), mx=V(vals, ft * 16, 8), i=s_in: e.match_replace(o, mx, i, NEG),
                     [sc.name, vals.name], [scr.name])
                P.op("dve", lambda e, o=V(vals, ft * 16 + 8, 8), i=V(scr, 0, 128): e.max(out=o, in_=i), [scr.name], [vals.name])
                P.op("dve", lambda e, o=V(idx, ft * 16 + 8, 8), mx=V(vals, ft * 16 + 8, 8), i=V(scr, 0, 128): e.max_index(o, mx, i),
                     [scr.name, vals.name], [idx.name])
            self.cp(idxf.ap, idx.ap, [idx.name], [idxf.name])
            self.tt(cand.v(0, [(256, 8), (16, 16), (1, 16)]), vals.v(0, [(32, 8), (1, 16), (0, 16)]), vals.v(16, [(32, 8), (0, 16), (1, 16)]),
                    ALU.add, [vals.name], [cand.name])
            for h in range(8):
                c_in = V(cand, h * 256, 256)
                P.op("dve", lambda e, o=V(top, h * 16, 8), i=c_in: e.max(out=o, in_=i), [cand.name], [top.name])
                P.op("dve", lambda e, o=V(sel, h * 16, 8), mx=V(top, h * 16, 8), i=c_in: e.max_index(o, mx, i), [cand.name, top.name], [sel.name])
                P.op("dve", lambda e, o=V(scr, 0, 256), mx=V(top, h * 16, 8), i=c_in: e.match_replace(o, mx, i, NEG),
                     [cand.name, top.name], [scr.name])
                P.op("dve", lambda e, o=V(top, h * 16 + 8, 8), i=V(scr, 0, 256): e.max(out=o, in_=i), [scr.name], [top.name])
                P.op("dve", lambda e, o=V(sel, h * 16 + 8, 8), mx=V(top, h * 16 + 8, 8), i=V(scr, 0, 256): e.max_index(o, mx, i),
                     [scr.name, top.name], [sel.name])
            self.tt(gg.v(0, [(16, 8), (1, 16)]), top.v(0, [(16, 8), (1, 16)]), top.v(0, [(16, 8), (0, 16)]), ALU.subtract, [top.name], [gg.name])
            self.act(gg.ap, gg.ap, AF.Exp, [gg.name], [gg.name])
            P.op("dve", lambda e, o=es.ap, i=gg.v(0, [(16, 8), (1, 16)]): e.tensor_reduce(o, i, AX.X, ALU.add), [gg.name], [es.name])
            P.op("dve", lambda e, o=es.ap: e.reciprocal(o, o), [es.name], [es.name])
            self.tt(gg.v(0, [(16, 8), (1, 16)]), gg.v(0, [(16, 8), (1, 16)]), es.v(0, [(1, 8), (0, 16)]), ALU.mult, [gg.name, es.name], [gg.name])
            self.ts(selb.ap, sel.ap, 4, ALU.logical_shift_right, [sel.name], [selb.name])
            self.cp(af.ap, selb.ap, [selb.name], [af.name])
            self.ts(selb.ap, sel.ap, 15, ALU.bitwise_and, [sel.name, af.name], [selb.name])
            self.cp(bf.ap, selb.ap, [selb.name], [bf.name])
            for (src, off, dst) in ((af, 0, i1), (bf, 16, i2)):
                self.tt(eq.v(0, [(256, 8), (16, 16), (1, 16)]), src.v(0, [(16, 8), (1, 16), (0, 16)]), self.iotaf.v(0, [(0, 8), (0, 16), (1, 16)]),
                        ALU.is_equal, [src.name, self.iotaf.name], [eq.name])
                self.tt(eq.v(0, [(256, 8), (16, 16), (1, 16)]), eq.v(0, [(256, 8), (16, 16), (1, 16)]), idxf.v(off, [(32, 8), (0, 16), (1, 16)]),
                        ALU.mult, [eq.name, idxf.name], [eq.name])
                P.op("dve", lambda e, o=dst.ap, i=eq.v(0, [(16, 128), (1, 16)]): e.tensor_reduce(o, i, AX.X, ALU.add), [eq.name], [dst.name])
            for (src, dst, bank) in ((i1, i1T, 5), (i2, i2T, 6), (gg, gT, 7)):
                pb = self.pb[bank]
                self.tr(pb.v(0, [(1, 128)]), src.ap, self.identf.ap, [src.name, self.identf.name], [pb.name])
                self.cp(dst.ap, pb.v(0, [(1, 128)]), [pb.name], [dst.name], eng="act")
            NB = 8
            for t8 in range(0, m, NB):
                nbt = min(NB, m - t8)
                sl = (t8 // NB) % 3
                E1, O1, O2 = oh1[sl], oh1[3 + sl], oh2[sl]
                v3 = lambda b_: b_.v(0, [(128, nbt), (1, 128)])
                io = self.iotaf.v(0, [(0, nbt), (1, 128)])
                self.tt(v3(E1), io, i1T.v(t8, [(1, nbt), (0, 128)]), ALU.is_equal, [self.iotaf.name, i1T.name], [E1.name])
                self.tt(v3(O1), v3(E1), gT.v(t8, [(1, nbt), (0, 128)]), ALU.mult, [E1.name, gT.name], [O1.name])
                self.tt(v3(O2), io, i2T.v(t8, [(1, nbt), (0, 128)]), ALU.is_equal, [self.iotaf.name, i2T.name], [O2.name])
                for tl in range(nbt):
                    t = t8 + tl
                    pb = self.pb[(t // 4) % 4]
                    self.mm(pb.v(t % 4, [(4, 128)]), O2.v(tl * 128, [(1, 128)]), O1.v(tl * 128, [(1, 128)]), True, True,
                            [O1.name, O2.name], [pb.name])
                    if t % 4 == 3 or t == m - 1:
                        n4 = t % 4 + 1
                        tb = t - (t % 4)
                        self.cp(Gs.v(tb, [(128, 128), (1, n4)]), pb.v(0, [(4, 128), (1, n4)]), [pb.name], [Gs.name], eng="act")
            if m == 128:
                for cq in range(4):
                    P.dma(self.Gd[cq * 32:(cq + 1) * 32, :, tt_ * 128:(tt_ + 1) * 128].rearrange("c p t -> p c t"),
                          Gs.v(cq * 32 * 128, [(128, 32), (1, 128)]), [Gs.name], [self.Gd.tensor.name], q="pool" if cq % 2 else "sp", join=True)
            else:
                self.cp(self.Gsamp.ap, Gs.v(0, [(128, 128)]), [Gs.name], [self.Gsamp.name])
            self.ar.reset()
        self.stage_end()

    def st_peer_main(self, i, hT):
        P, T, TT = self.P, self.T, self.TT
        NCH = self.nchunks if hasattr(self, "nchunks") else 128
        groups = [(t0, min(1024, T - t0)) for t0 in range(0, T, 1024)]
        hv = hT.rearrange("k p t -> p k t")
        for gi, (t0, n) in enumerate(groups):
            last = gi == len(groups) - 1
            ne = n + 1 if last else n
            blks = [(b0, min(512, n - b0)) for b0 in range(0, n, 512)]
            acc = self.ar.f32(KT * ne)
            hg = self.ar.bf16(KT * ne)
            ubf = [self.ar.bf16(D) for _ in range(3)]
            uT = [self.ar.bf16(D) for _ in range(2)]
            vbf = [self.ar.bf16(D) for _ in range(8)]
            WT = [self.ar.bf16(ne) for _ in range(4)]
            actb = [self.ar.bf16(ne) for _ in range(2)]
            Gc = [self.ar.bf16(n) for _ in range(2)]
            xbs = [self.ar.f32(n) for _ in range(2)]
            P.dma(hg.v(0, [(ne, KT), (1, n)]), hv[:, :, t0:t0 + n], [hT.tensor.name], [hg.name])
            if last:
                P.dma(hg.v(n, [(ne, KT), (1, 1)]), hv[:, :, T:T + 1], [hT.tensor.name], [hg.name], join=True)
            pS = [self.pb[0], self.pb[1]]
            pSs = self.pb[2]
            pT = [self.pb[3], self.pb[4]]
            pV = [self.pb[5], self.pb[6]]
            pVs = self.pb[7]
            nvc = [0]

            def loadUV(c):
                P.dma(ubf[c % 3].ap, self.peer_u[i, c * 128:(c + 1) * 128, :], (), [ubf[c % 3].name], q="pool")
                P.dma(vbf[c % 8].ap, self.peer_v[i, c * 128:(c + 1) * 128, :], (), [vbf[c % 8].name], q="pool")

            def loadG(c):
                P.dma(Gc[c % 2].ap, self.Gd[c, :, t0:t0 + n], [self.Gd.tensor.name], [Gc[c % 2].name], q="sp")

            def tr(c):
                ub = ubf[c % 3]
                for hb in range(2):
                    pt = pT[hb]
                    ptb = Buf(pt.ap.bitcast(BF16), pt.name)
                    for kk in range(8):
                        k = hb * 8 + kk
                        self.tr(ptb.v(kk * 128, [(1, 128)]), ub.v(k * 128, [(1, 128)]), self.identb.ap, [ub.name, self.identb.name], [pt.name])
                    self.cp(uT[c % 2].v(hb * 1024, [(1, 1024)]), ptb.v(0, [(1, 1024)]), [pt.name], [uT[c % 2].name], eng="act")

            def compute(c):
                ci, s2 = c % 4, c % 2
                for bi, (b0, wd) in enumerate(blks):
                    for k in range(KT):
                        self.mm(pS[bi].v(0, [(1, wd)]), uT[s2].v(k * 128, [(1, 128)]), hg.v(k * ne + b0, [(1, wd)]), k == 0, k == KT - 1,
                                [uT[s2].name, hg.name], [pS[bi].name])
                    self.act(actb[s2].v(b0, [(1, wd)]), pS[bi].v(0, [(1, wd)]), AF.Gelu, [pS[bi].name], [actb[s2].name])
                if last:
                    for k in range(KT):
                        self.mm(pSs.v(0, [(1, 1)]), uT[s2].v(k * 128, [(1, 128)]), hg.v(k * ne + n, [(1, 1)]), k == 0, k == KT - 1,
                                [uT[s2].name, hg.name], [pSs.name])
                    self.act(actb[s2].v(n, [(1, 1)]), pSs.v(0, [(1, 1)]), AF.Gelu, [pSs.name], [actb[s2].name])
                    self.tt(WT[ci].v(n, [(1, 1)]), actb[s2].v(n, [(1, 1)]), self.Gsamp.v(c, [(1, 1)]), ALU.mult,
                            [actb[s2].name, self.Gsamp.name], [WT[ci].name])
                self.tt(WT[ci].v(0, [(1, n)]), actb[s2].v(0, [(1, n)]), Gc[s2].ap, ALU.mult, [actb[s2].name, Gc[s2].name], [WT[ci].name])

            def vphase(c):
                first = c == 3
                vs = [vbf[(c - 3 + cj) % 8] for cj in range(4)]
                for dt in range(KT):
                    for (b0, wd) in blks:
                        pv = pV[nvc[0] % 2]
                        nvc[0] += 1
                        for cj in range(4):
                            self.mm(pv.v(0, [(1, wd)]), vs[cj].v(dt * 128, [(1, 128)]), WT[cj].v(b0, [(1, wd)]), cj == 0, cj == 3,
                                    [vs[cj].name, WT[cj].name], [pv.name])
                        av = acc.v(dt * ne + b0, [(1, wd)])
                        if first:
                            self.cp(av, pv.v(0, [(1, wd)]), [pv.name], [acc.name])
                        else:
                            self.tt(av, pv.v(0, [(1, wd)]), av, ALU.add, [pv.name, acc.name], [acc.name])
                if last:
                    for dt in range(KT):
                        for cj in range(4):
                            self.mm(pVs.v(dt, [(1, 1)]), vs[cj].v(dt * 128, [(1, 128)]), WT[cj].v(n, [(1, 1)]), cj == 0, cj == 3,
                                    [vs[cj].name, WT[cj].name], [pVs.name])
                    av = acc.v(n, [(ne, KT)])
                    if first:
                        self.cp(av, pVs.v(0, [(1, KT)]), [pVs.name], [acc.name])
                    else:
                        self.tt(av, pVs.v(0, [(1, KT)]), av, ALU.add, [pVs.name, acc.name], [acc.name])

            for c in range(min(3, NCH)):
                loadUV(c)
            for c in range(min(2, NCH)):
                loadG(c)
            tr(0)
            for c in range(NCH):
                if c + 3 < NCH:
                    loadUV(c + 3)
                if c + 1 < NCH:
                    tr(c + 1)
                compute(c)
                if c + 2 < NCH:
                    loadG(c + 2)
                if c % 4 == 3:
                    vphase(c)
            xtv = self.xT.rearrange("k p t -> p k t")
            for k in range(KT):
                xb = xbs[k % 2]
                P.dma(xb.v(0, [(1, n)]), xtv[:, k, t0:t0 + n], ["xT"], [xb.name])
                self.stt(xb.v(0, [(1, n)]), acc.v(k * ne, [(1, n)]), self.modT.v(5 * 32 + 2 * k, [(1, 1)]), xb.v(0, [(1, n)]), ALU.mult, ALU.add,
                         [acc.name, self.modT.name, xb.name], [xb.name])
                P.dma(xtv[:, k, t0:t0 + n], xb.v(0, [(1, n)]), [xb.name], ["xT"], q="pool", join=True)
            if last:
                xs = self.cst_tmp("xs_s", KT)
                tmpg = self.cst_tmp("xs_g", KT)
                P.dma(xs.v(0, [(1, KT), (1, 1)]), xtv[:, :, T:T + 1], ["xT"], [xs.name])
                self.tt(tmpg.v(0, [(1, KT)]), acc.v(n, [(ne, KT)]), self.modT.v(5 * 32 + 1, [(2, KT)]), ALU.mult, [acc.name, self.modT.name], [tmpg.name])
                self.tt(xs.v(0, [(1, KT)]), xs.v(0, [(1, KT)]), tmpg.v(0, [(1, KT)]), ALU.add, [tmpg.name, xs.name], [xs.name])
                P.dma(xtv[:, :, T:T + 1], xs.v(0, [(1, KT), (1, 1)]), [xs.name], ["xT"], q="pool", join=True)
            self.stage_end()


    def st_dil(self, hT):
        P, T, TT = self.P, self.T, self.TT
        DILS = (1, 4, 16)
        WIN = (128, 512, 2048)
        w_in = self.din("dil_w_in", [1, D, 9216])[0]
        w_out = self.din("dil_w_out", [1, 1024, D])[0]
        caches = [self.din("cache_dil_kv%d" % g, [1, WIN[g], 2, 8, 128])[0] for g in range(3)]
        kvp = [self.dout("kv%d_p" % g, [1, min(WIN[g], T), 2, 8, 128])[0] for g in range(3)]
        kvs = [self.dout("kv%d_s" % g, [1, WIN[g], 2, 8, 128])[0] for g in range(3)]
        winv = w_in.rearrange("(k p) f -> p k f", p=128)
        scale = 128 ** -0.5
        NEG = -1.0e30
        for g in range(3):
            Wg = WIN[g]
            nsp = 4 if Wg > 512 else 1
            for q_ in range(nsp):
                a = 1 + (Wg - 1) * q_ // nsp
                b = 1 + (Wg - 1) * (q_ + 1) // nsp
                P.dma(kvs[g][a - 1:b - 1].rearrange("w a h d -> w (a h d)"), caches[g][a:b].rearrange("w a h d -> w (a h d)"), (), ["kvs%d" % g],
                      q="pool", join=True)
        D2 = self.ar.f32(256)
        M2 = self.ar.f32(256)
        B2 = self.ar.f32(256)
        bS = self.ar.f32(1)
        bSh = self.ar.f32(1)
        ti = self.ar.f32(256)
        tii = Buf(ti.ap.bitcast(I32), ti.name)
        P.op("pool", lambda e: e.iota(tii.v(0, [(1, 128)]), [[1, 128]], base=128, channel_multiplier=-1), (), [ti.name])
        P.op("pool", lambda e: e.iota(tii.v(128, [(1, 128)]), [[1, 128]], base=0, channel_multiplier=-1), (), [ti.name])
        self.cp(D2.ap, tii.ap, [ti.name], [D2.name])
        self.ts(M2.v(0, [(1, 128)]), D2.v(0, [(1, 128)]), 128.0, ALU.is_gt, [D2.name], [M2.name], s2=NEG, op1=ALU.mult)
        self.ts(M2.v(128, [(1, 128)]), D2.v(128, [(1, 128)]), 0.0, ALU.is_lt, [D2.name], [M2.name], s2=NEG, op1=ALU.mult)
        P.op("pool", lambda e: e.iota(tii.v(0, [(1, 1)]), [[1, 1]], base=-128, channel_multiplier=1), (), [ti.name])
        self.cp(bS.ap, tii.v(0, [(1, 1)]), [ti.name], [bS.name])
        hs = self.ar.bf16(KT * TT)
        P.dma(hs.v(0, [(TT, KT), (1, TT)]), hT.rearrange("k p t -> p k t"), [hT.tensor.name], [hs.name])
        oT = self.ar.bf16(8 * TT)
        accO = self.ar.f32(T)
        accD = self.ar.f32(T)
        qT = self.ar.bf16(T)
        kT = self.ar.bf16(T)
        Vh = self.ar.bf16(16 * 128)
        wbf6 = [self.ar.bf16(D) for _ in range(6)]
        PT = [self.ar.bf16(256) for _ in range(2)]
        tmpS = [self.ar.f32(256) for _ in range(2)]
        rows = [self.ar.f32(128) for _ in range(3)]
        vcol = self.ar.f32(1)
        Kc = self.ar.f32(128)
        Vc = self.ar.f32(128)
        prod = self.ar.f32(128)
        sS = self.ar.f32(2)
        pS_ = self.ar.f32(2)
        oS = self.ar.f32(2)
        tS = self.ar.f32(2)
        def load_hg(it):
            h_, g_ = it // 3, it % 3
            for s_ in range(3):
                col = s_ * 3072 + g_ * 1024 + h_ * 128
                wd_ = wbf6[(it % 2) * 3 + s_]
                P.dma(wd_.v(0, [(128, KT), (1, 128)]), winv[:, :, col:col + 128], (), [wd_.name], q="pool")
        load_hg(0)
        for h in range(8):
            for g in range(3):
                dil = DILS[g]
                L = T // dil
                bpr = L // 128
                slope = 2.0 ** (-8.0 * (g * 8 + h + 1) / 24.0)
                it = h * 3 + g
                if it + 1 < 24:
                    load_hg(it + 1)
                wbf = wbf6[(it % 2) * 3:(it % 2) * 3 + 3]
                for (dst, wi, sc_) in ((qT, 0, scale), (kT, 1, 1.0)):
                    for bi, (t0, w) in enumerate(blocks_of(T)[:-1]):
                        pb = self.pb[bi % 2]
                        for k in range(KT):
                            self.mm(pb.v(0, [(1, w)]), wbf[wi].v(k * 128, [(1, 128)]), hs.v(k * TT + t0, [(1, w)]), k == 0, k == KT - 1,
                                    [wbf[wi].name, hs.name], [pb.name])
                        self.act(dst.v(t0 // dil, [(L, dil), (1, w // dil)]), pb.v(0, [(1, dil), (dil, w // dil)]), AF.Copy, [pb.name], [dst.name],
                                 scale=sc_)
                for jb in range(16):
                    r_ = (jb * 128) // L
                    i0 = (jb * 128) % L
                    pb = self.pb[7]
                    for k in range(KT):
                        self.mm(pb.v(0, [(1, 128)]), hs.v(k * TT + i0 * dil + r_, [(dil, 128)]), wbf[2].v(k * 128, [(1, 128)]), k == 0, k == KT - 1,
                                [wbf[2].name, hs.name], [pb.name])
                    self.cp(Vh.v(jb * 128, [(1, 128)]), pb.v(0, [(1, 128)]), [pb.name], [Vh.name])
                self.stt(B2.ap, D2.ap, -slope * dil, M2.ap, ALU.mult, ALU.add, [D2.name, M2.name], [B2.name])
                for jb in range(16):
                    hasprev = (jb % bpr) != 0
                    ks = [jb - 1, jb] if hasprev else [jb]
                    o0 = 0 if hasprev else 128
                    nk = 128 * len(ks)
                    pSb = self.pb[2]
                    s2 = jb % 2
                    for x, kb in enumerate(ks):
                        self.mm(pSb.v(o0 + x * 128, [(1, 128)]), kT.v(kb * 128, [(1, 128)]), qT.v(jb * 128, [(1, 128)]), True, True,
                                [kT.name, qT.name], [pSb.name])
                    self.tt(tmpS[s2].v(o0, [(1, nk)]), pSb.v(o0, [(1, nk)]), B2.v(o0, [(1, nk)]), ALU.add, [pSb.name, B2.name], [tmpS[s2].name])
                    self.act(PT[s2].v(o0, [(1, nk)]), tmpS[s2].v(o0, [(1, nk)]), AF.Exp, [tmpS[s2].name], [PT[s2].name])
                    pO, pD = self.pb[3 + 2 * (jb % 2)], self.pb[4 + 2 * (jb % 2)]
                    for x, kb in enumerate(ks):
                        self.mm(pO.v(0, [(1, 128)]), Vh.v(kb * 128, [(1, 128)]), PT[s2].v(o0 + x * 128, [(1, 128)]), x == 0, x == len(ks) - 1,
                                [Vh.name, PT[s2].name], [pO.name])
                    for x, kb in enumerate(ks):
                        self.mm(pD.v(0, [(1, 128)]), self.onesb.ap, PT[s2].v(o0 + x * 128, [(1, 128)]), x == 0, x == len(ks) - 1,
                                [self.onesb.name, PT[s2].name], [pD.name])
                    r_ = (jb * 128) // L
                    i0 = (jb * 128) % L
                    for (acc_, pb_) in ((accO, pO), (accD, pD)):
                        av = acc_.v(i0 * dil + r_, [(dil, 128)])
                        if g == 0:
                            self.cp(av, pb_.v(0, [(1, 128)]), [pb_.name], [acc_.name], eng="act" if acc_ is accD else "dve")
                        else:
                            self.tt(av, pb_.v(0, [(1, 128)]), av, ALU.add, [pb_.name, acc_.name], [acc_.name])
                pr = self.pb[7]
                for s_ in range(3):
                    for k in range(KT):
                        self.mm(pr.v(s_ * 128, [(1, 128)]), hs.v(k * TT + T, [(0, 128)]), wbf[s_].v(k * 128, [(1, 128)]), k == 0, k == KT - 1,
                                [wbf[s_].name, hs.name], [pr.name])
                    self.cp(rows[s_].ap, pr.v(s_ * 128, [(1, 128)]), [pr.name], [rows[s_].name])
                for k in range(KT):
                    self.mm(pr.v(384, [(1, 1)]), wbf[2].v(k * 128, [(1, 128)]), hs.v(k * TT + T, [(1, 1)]), k == 0, k == KT - 1,
                            [wbf[2].name, hs.name], [pr.name])
                self.cp(vcol.ap, pr.v(384, [(1, 1)]), [pr.name], [vcol.name])
                Wg = WIN[g]
                P.dma(kvs[g][Wg - 1:Wg, 0, h, :], rows[1].v(0, [(1, 128)], 0, 1), [rows[1].name], ["kvs%d" % g], q="pool", join=True)
                P.dma(kvs[g][Wg - 1:Wg, 1, h, :], rows[2].v(0, [(1, 128)], 0, 1), [rows[2].name], ["kvs%d" % g], q="pool", join=True)
                cv = caches[g].rearrange("(i s) a h d -> i s a h d", s=dil)
                P.dma(Kc.ap, cv[:, 0, 0, h, :], (), [Kc.name])
                P.dma(Vc.ap, cv[:, 0, 1, h, :], (), [Vc.name])
                self.tt(prod.ap, Kc.ap, rows[0].ap, ALU.mult, [Kc.name, rows[0].name], [prod.name])
                P.op("dve", lambda e, o=sS.v(0, [(1, 1)]), i=prod.ap: e.tensor_reduce(o, i, AX.X, ALU.add), [prod.name], [sS.name])
                self.tt(prod.ap, rows[1].ap, rows[0].ap, ALU.mult, [rows[1].name, rows[0].name, sS.name], [prod.name])
                P.op("dve", lambda e, o=sS.v(1, [(1, 1)]), i=prod.ap: e.tensor_reduce(o, i, AX.X, ALU.add), [prod.name], [sS.name])
                self.ts(bSh.ap, bS.ap, slope * dil, ALU.mult, [bS.name], [bSh.name])
                self.act(pS_.v(0, [(1, 1)]), sS.v(0, [(1, 1)]), AF.Exp, [sS.name, bSh.name], [pS_.name], bias=bSh.ap, scale=scale)
                self.act(pS_.v(1, [(1, 1)]), sS.v(1, [(1, 1)]), AF.Exp, [sS.name], [pS_.name], scale=scale)
                po = self.pb[2]
                self.mm(po.v(0, [(1, 1)]), Vc.ap, pS_.v(0, [(1, 1)]), True, True, [Vc.name, pS_.name], [po.name])
                self.mm(po.v(1, [(1, 1)]), self.onesf.ap, pS_.v(0, [(1, 1)]), True, True, [self.onesf.name, pS_.name], [po.name])
                self.stt(tS.v(0, [(1, 1)]), vcol.ap, pS_.v(1, [(1, 1)]), po.v(0, [(1, 1)]), ALU.mult, ALU.add, [vcol.name, pS_.name, po.name], [tS.name])
                self.tt(tS.v(1, [(1, 1)]), po.v(1, [(1, 1)]), pS_.v(1, [(1, 1)]), ALU.add, [po.name, pS_.name, tS.name], [tS.name])
                if g == 0:
                    self.cp(oS.ap, tS.ap, [tS.name], [oS.name])
                else:
                    self.tt(oS.ap, oS.ap, tS.ap, ALU.add, [tS.name, oS.name], [oS.name])
            P.op("dve", lambda e, o=accD.ap: e.reciprocal(o, o), [accD.name], [accD.name])
            self.tt(oT.v(h * TT, [(1, T)]), accO.ap, accD.ap, ALU.mult, [accO.name, accD.name], [oT.name])
            P.op("dve", lambda e, o=oS.v(1, [(1, 1)]): e.reciprocal(o, o), [oS.name], [oS.name])
            self.tt(oT.v(h * TT + T, [(1, 1)]), oS.v(0, [(1, 1)]), oS.v(1, [(1, 1)]), ALU.mult, [oS.name], [oT.name])
        xtv = self.xT.rearrange("k p t -> p k t")
        wov = w_out.rearrange("(e p) f -> p e f", p=128)
        for ft in range(KT):
            wb = wbf6[ft % 2]
            xb = [accO, accD][ft % 2]
            xs_ = tmpS[ft % 2]
            P.dma(wb.v(0, [(128, 8), (1, 128)]), wov[:, :, ft * 128:(ft + 1) * 128], (), [wb.name], q="pool")
            P.dma(xb.ap, xtv[:, ft, 0:T], ["xT"], [xb.name], q="pool")
            P.dma(xs_.v(0, [(1, 1)]), xtv[:, ft, T:T + 1], ["xT"], [xs_.name], q="pool")
            for bi, (t0, w) in enumerate(blocks_of(T)):
                pb = self.pb[bi % 2]
                col = 1 if t0 == T else 0
                for e_ in range(8):
                    self.mm(pb.v(0, [(1, w)]), wb.v(e_ * 128, [(1, 128)]), oT.v(e_ * TT + t0, [(1, w)]), e_ == 0, e_ == 7, [wb.name, oT.name], [pb.name])
                if col == 0:
                    xv = xb.v(t0, [(1, w)])
                    self.stt(xv, pb.v(0, [(1, w)]), self.modT.v(2 * 32 + 2 * ft, [(1, 1)]), xv, ALU.mult, ALU.add, [pb.name, self.modT.name, xb.name], [xb.name])
                else:
                    xv = xs_.v(0, [(1, 1)])
                    self.stt(xv, pb.v(0, [(1, 1)]), self.modT.v(2 * 32 + 2 * ft + 1, [(1, 1)]), xv, ALU.mult, ALU.add, [pb.name, self.modT.name, xs_.name], [xs_.name])
            P.dma(xtv[:, ft, 0:T], xb.ap, [xb.name], ["xT"], q="pool", join=True)
            P.dma(xtv[:, ft, T:T + 1], xs_.v(0, [(1, 1)]), [xs_.name], ["xT"], q="pool", join=True)
        self.stage_end()
        hs = self.ar.bf16(KT * TT)
        P.dma(hs.v(0, [(TT, KT), (1, TT)]), hT.rearrange("k p t -> p k t"), [hT.tensor.name], [hs.name])
        self.kvw = [self.ar.bf16(KT * 512) for _ in range(2)]
        self.kvo = [self.ar.f32(512) for _ in range(2)]
        nrot = 0
        for g in range(3):
            nl = min(WIN[g], T)
            for s_ in (1, 2):
                for half in range(2):
                    col = s_ * 3072 + g * 1024 + half * 512
                    kvw = self.kvw[nrot % 2]
                    nrot += 1
                    P.dma(kvw.v(0, [(512, KT), (1, 512)]), winv[:, :, col:col + 512], (), [kvw.name], q="pool")
                    for ti_ in range(nl // 128):
                        tok0 = T - nl + ti_ * 128
                        pb = self.pb[ti_ % 2]
                        for k in range(KT):
                            self.mm(pb.ap, hs.v(k * TT + tok0, [(1, 128)]), kvw.v(k * 512, [(1, 512)]), k == 0, k == KT - 1,
                                    [hs.name, kvw.name], [pb.name])
                        ob = self.kvo[ti_ % 2]
                        self.cp(ob.ap, pb.ap, [pb.name], [ob.name], eng="act" if ti_ % 2 else "dve")
                        P.dma(kvp[g][ti_ * 128:(ti_ + 1) * 128, s_ - 1, half * 4:(half + 1) * 4, :].rearrange("t h d -> t (h d)"), ob.ap,
                              [ob.name], ["kvp%d" % g], q="pool", join=True)
        self.stage_end()

    def st_ret(self, hT):
        P, T, TT = self.P, self.T, self.TT
        w_in = self.din("ret_w_in", [1, D, 12288])[0]
        gn_g = self.din("ret_gn_g", [1, 4096])
        w_out = self.din("ret_w_out", [1, 4096, D])[0]
        st_in = self.din("state_ret", [1, 8, 256, 512])[0]
        ret_p = self.dout("ret_p", [1, 8, 256, 512])[0]
        ret_s = self.dout("ret_s", [1, 8, 256, 512])[0]
        zT = self.scratch("zT", [32, 128, TT], BF16)
        winv = w_in.rearrange("(k p) f -> p k f", p=128)
        NCK = T // 128
        hs = self.ar.bf16(KT * TT)
        P.dma(hs.v(0, [(TT, KT), (1, TT)]), hT.rearrange("k p t -> p k t"), [hT.tensor.name], [hs.name])
        wb2 = [self.ar.bf16(KT * 512) for _ in range(2)]
        qT = self.ar.bf16(2 * T)
        kT = self.ar.bf16(2 * T)
        ktok = self.ar.bf16(NCK * 256)
        vtok = self.ar.bf16(NCK * 512)
        sg = self.ar.bf16((NCK + 1) * 512)
        S = self.ar.f32(1024)
        Sb = self.ar.bf16(1024)
        attTb = self.ar.bf16(128)
        qd = self.ar.bf16(256)
        osb = self.ar.f32(512)
        cen = self.ar.f32(512)
        zb = self.ar.bf16(512)
        zst = self.ar.bf16(512)
        dmaskT = self.ar.f32(128)
        qdecT = self.ar.f32(128)
        Dcl = self.ar.f32(128)
        msk = self.ar.f32(128)
        gng = self.ar.f32(512)
        vrow = self.ar.f32(512)
        cols = self.ar.f32(8)
        stat = self.ar.f32(8)
        ti = self.ar.f32(128)
        tii = Buf(ti.ap.bitcast(I32), ti.name)
        P.op("pool", lambda e: e.iota(tii.ap, [[1, 128]], base=0, channel_multiplier=-1), (), [ti.name])
        self.cp(Dcl.ap, tii.ap, [ti.name], [Dcl.name])
        self.ts(msk.ap, Dcl.ap, 0.0, ALU.is_ge, [Dcl.name], [msk.name])
        self.ts(Dcl.ap, Dcl.ap, 0.0, ALU.max, [Dcl.name, msk.name], [Dcl.name])
        P.op("pool", lambda e: e.iota(tii.v(0, [(1, 1)]), [[1, 1]], base=127, channel_multiplier=-1), (), [ti.name])
        self.cp(cols.v(5, [(1, 1)]), tii.v(0, [(1, 1)]), [ti.name], [cols.name])
        P.op("pool", lambda e: e.iota(tii.ap, [[1, 128]], base=1, channel_multiplier=0), (), [ti.name])
        cp1 = self.ar.f32(128)
        self.cp(cp1.ap, tii.ap, [ti.name], [cp1.name])

        wl = []
        for h_ in range(8):
            wl += [(h_ * 256, 256), (2048 + h_ * 256, 256), (4096 + h_ * 512, 512), (8192 + h_ * 512, 512)]
        wcnt = [0]

        def issue_w(n_):
            if n_ < len(wl):
                col0, ncols = wl[n_]
                P.dma(wb2[n_ % 2].v(0, [(ncols, KT), (1, ncols)]), winv[:, :, col0:col0 + ncols], (), [wb2[n_ % 2].name], q="pool")

        def load_w(col0, ncols):
            n_ = wcnt[0]
            assert wl[n_] == (col0, ncols)
            wcnt[0] += 1
            issue_w(n_ + 1)
            return wb2[n_ % 2]
        issue_w(0)

        for h in range(8):
            gam = 1.0 - 2.0 ** (-5.0 - h)
            lg = math.log(gam)
            cdec = gam ** 128
            self.act(dmaskT.ap, Dcl.ap, AF.Exp, [Dcl.name], [dmaskT.name], scale=lg)
            self.tt(dmaskT.ap, dmaskT.ap, msk.ap, ALU.mult, [dmaskT.name, msk.name], [dmaskT.name])
            self.act(qdecT.ap, cp1.ap, AF.Exp, [cp1.name], [qdecT.name], scale=lg)
            self.act(cols.v(4, [(1, 1)]), cols.v(5, [(1, 1)]), AF.Exp, [cols.name], [cols.name], scale=lg)
            self.ts(cols.v(4, [(1, 1)]), cols.v(4, [(1, 1)]), 1.0 / 16.0, ALU.mult, [cols.name], [cols.name])
            P.dma(gng.ap, gn_g[0:1, h * 512:(h + 1) * 512].to_broadcast([128, 512]), (), [gng.name])
            wb = load_w(h * 256, 256)
            nb = 0
            for dkt in range(2):
                for (t0, w) in blocks_of(T):
                    pb = self.pb[nb % 2]
                    nb += 1
                    for k in range(KT):
                        self.mm(pb.v(0, [(1, w)]), wb.v(k * 256 + dkt * 128, [(1, 128)]), hs.v(k * TT + t0, [(1, w)]), k == 0, k == KT - 1,
                                [wb.name, hs.name], [pb.name])
                    if t0 < T:
                        self.cp(qT.v(dkt * T + t0, [(1, w)]), pb.v(0, [(1, w)]), [pb.name], [qT.name], eng="act")
                    else:
                        self.cp(cols.v(dkt, [(1, 1)]), pb.v(0, [(1, 1)]), [pb.name], [cols.name])
            wb = load_w(2048 + h * 256, 256)
            for dkt in range(2):
                for (t0, w) in blocks_of(T):
                    pb = self.pb[nb % 2]
                    nb += 1
                    for k in range(KT):
                        self.mm(pb.v(0, [(1, w)]), wb.v(k * 256 + dkt * 128, [(1, 128)]), hs.v(k * TT + t0, [(1, w)]), k == 0, k == KT - 1,
                                [wb.name, hs.name], [pb.name])
                    if t0 < T:
                        self.act(kT.v(dkt * T + t0, [(1, w)]), pb.v(0, [(1, w)]), AF.Copy, [pb.name], [kT.name], scale=1.0 / 16.0)
                    else:
                        self.ts(cols.v(2 + dkt, [(1, 1)]), pb.v(0, [(1, 1)]), 1.0 / 16.0, ALU.mult, [pb.name], [cols.name])
            for c in range(NCK):
                pb = self.pb[2 + c % 2]
                for k in range(KT):
                    self.mm(pb.v(0, [(1, 256)]), hs.v(k * TT + c * 128, [(1, 128)]), wb.v(k * 256, [(1, 256)]), k == 0, k == KT - 1,
                            [wb.name, hs.name], [pb.name])
                self.ts(ktok.v(c * 256, [(1, 256)]), pb.v(0, [(1, 256)]), cols.v(4, [(1, 1)]), ALU.mult, [pb.name, cols.name], [ktok.name])
            wb = load_w(4096 + h * 512, 512)
            for c in range(NCK + 1):
                pb = self.pb[2 + c % 2]
                lhs = (lambda k: hs.v(k * TT + c * 128, [(1, 128)])) if c < NCK else (lambda k: hs.v(k * TT + T, [(0, 128)]))
                for k in range(KT):
                    self.mm(pb.ap, lhs(k), wb.v(k * 512, [(1, 512)]), k == 0, k == KT - 1, [wb.name, hs.name], [pb.name])
                if c < NCK:
                    self.cp(vtok.v(c * 512, [(1, 512)]), pb.ap, [pb.name], [vtok.name], eng="act" if c % 2 else "dve")
                else:
                    self.cp(vrow.ap, pb.ap, [pb.name], [vrow.name])
            wb = load_w(8192 + h * 512, 512)
            for c in range(NCK + 1):
                pb = self.pb[2 + c % 2]
                lhs = (lambda k: hs.v(k * TT + c * 128, [(1, 128)])) if c < NCK else (lambda k: hs.v(k * TT + T, [(0, 128)]))
                for k in range(KT):
                    self.mm(pb.ap, lhs(k), wb.v(k * 512, [(1, 512)]), k == 0, k == KT - 1, [wb.name, hs.name], [pb.name])
                self.act(sg.v(c * 512, [(1, 512)]), pb.ap, AF.Silu, [pb.name], [sg.name])
            self.memset(S.ap, 0.0, [S.name])
            self.memset(Sb.ap, 0.0, [Sb.name])

            def post(pO, m, c, tcol):
                pv = lambda b, o=0, n=512: b.v(o, [(1, n)], 0, m)
                sv = lambda j: stat.v(j, [(1, 1)], 0, m)
                self.act(pv(osb), pv(pO), AF.Identity, [pO.name], [osb.name, stat.name], accum_out=sv(0))
                self.ts(sv(1), sv(0), -1.0 / 512.0, ALU.mult, [stat.name], [stat.name])
                self.ts(pv(cen), pv(osb), sv(1), ALU.add, [osb.name, stat.name], [cen.name])
                self.act(pv(osb), pv(cen), AF.Square, [cen.name], [osb.name, stat.name], accum_out=sv(2))
                self.act(sv(3), sv(2), AF.Sqrt, [stat.name], [stat.name], bias=EPS, scale=1.0 / 512.0)
                P.op("dve", lambda e, o=sv(3): e.reciprocal(o, o), [stat.name], [stat.name])
                self.stt(pv(cen), pv(cen), sv(3), pv(gng), ALU.mult, ALU.mult, [cen.name, stat.name, gng.name], [cen.name])
                self.tt(pv(zb), pv(cen), pv(sg, c * 512), ALU.mult, [cen.name, sg.name], [zb.name])
                pt = self.pb[6]
                ptb = Buf(pt.ap.bitcast(BF16), pt.name)
                for et in range(4):
                    self.tr(ptb.v(et * 128, [(1, m)]), zb.v(et * 128, [(1, 128)], 0, m), self.identb.v(0, [(1, m)], 0, m),
                            [zb.name, self.identb.name], [pt.name])
                self.cp(zst.v(0, [(128, 4), (1, m)]), ptb.v(0, [(128, 4), (1, m)]), [pt.name], [zst.name], eng="act")
                P.dma(zT[h * 4:(h + 1) * 4].rearrange("e p t -> p e t")[:, :, tcol:tcol + m], zst.v(0, [(128, 4), (1, m)]), [zst.name], ["zT"],
                      q="pool", join=True)

            for c in range(NCK):
                pA = self.pb[0]
                for dkt in range(2):
                    self.mm(pA.v(0, [(1, 128)]), kT.v(dkt * T + c * 128, [(1, 128)]), qT.v(dkt * T + c * 128, [(1, 128)]), dkt == 0, dkt == 1,
                            [kT.name, qT.name], [pA.name])
                self.tt(attTb.ap, pA.v(0, [(1, 128)]), dmaskT.ap, ALU.mult, [pA.name, dmaskT.name], [attTb.name])
                self.tt(qd.v(0, [(128, 2), (1, 128)]), qT.v(c * 128, [(T, 2), (1, 128)]), qdecT.v(0, [(0, 2), (1, 128)]), ALU.mult,
                        [qT.name, qdecT.name], [qd.name])
                pO = self.pb[4 + c % 2]
                self.mm(pO.ap, attTb.ap, vtok.v(c * 512, [(1, 512)]), True, False, [attTb.name, vtok.name], [pO.name])
                for dkt in range(2):
                    self.mm(pO.ap, qd.v(dkt * 128, [(1, 128)]), Sb.v(dkt * 512, [(1, 512)]), False, dkt == 1, [qd.name, Sb.name], [pO.name])
                for dkt in range(2):
                    pSt = self.pb[2 + dkt]
                    self.mm(pSt.ap, ktok.v(c * 256 + dkt * 128, [(1, 128)]), vtok.v(c * 512, [(1, 512)]), True, True, [ktok.name, vtok.name], [pSt.name])
                    sv_ = S.v(dkt * 512, [(1, 512)])
                    self.stt(sv_, sv_, cdec, pSt.ap, ALU.mult, ALU.add, [S.name, pSt.name], [S.name])
                self.cp(Sb.ap, S.ap, [S.name], [Sb.name], eng="pool")
                post(pO, 128, c, c * 128)
            P.dma(ret_p[h].rearrange("(t p) e -> p t e", p=128), S.v(0, [(512, 2), (1, 512)]), [S.name], ["ret_p"], q="pool", join=True)
            P.dma(S.v(0, [(512, 2), (1, 512)]), st_in[h].rearrange("(t p) e -> p t e", p=128), (), [S.name])
            pO = self.pb[4]
            for dkt in range(2):
                self.mm(pO.v(0, [(1, 512)], 0, 1), cols.v(dkt, [(1, 1)]), S.v(dkt * 512, [(1, 512)]), dkt == 0, dkt == 1, [cols.name, S.name], [pO.name])
            pq = self.pb[5]
            for dkt in range(2):
                self.mm(pq.v(0, [(1, 1)], 0, 1), cols.v(dkt, [(1, 1)]), cols.v(2 + dkt, [(1, 1)]), dkt == 0, dkt == 1, [cols.name], [pq.name])
            self.cp(stat.v(4, [(1, 1)], 0, 1), pq.v(0, [(1, 1)], 0, 1), [pq.name], [stat.name])
            orow = self.pb[7]
            self.ts(cen.v(0, [(1, 512)], 0, 1), vrow.v(0, [(1, 512)], 0, 1), stat.v(4, [(1, 1)], 0, 1), ALU.mult, [vrow.name, stat.name], [cen.name])
            self.stt(osb.v(0, [(1, 512)], 0, 1), pO.v(0, [(1, 512)], 0, 1), gam, cen.v(0, [(1, 512)], 0, 1), ALU.mult, ALU.add,
                     [pO.name, cen.name], [osb.name])
            for dkt in range(2):
                sv_ = S.v(dkt * 512, [(1, 512)])
                self.ts(sv_, sv_, gam, ALU.mult, [S.name, pO.name], [S.name])
                self.stt(sv_, vrow.ap, cols.v(2 + dkt, [(1, 1)]), sv_, ALU.mult, ALU.add, [vrow.name, cols.name, S.name], [S.name])
            P.dma(ret_s[h].rearrange("(t p) e -> p t e", p=128), S.v(0, [(512, 2), (1, 512)]), [S.name], ["ret_s"], q="pool", join=True)
            post(osb, 1, NCK, T)
        self.stage_end()
        xtv = self.xT.rearrange("k p t -> p k t")
        wov = w_out.rearrange("(e p) f -> p e f", p=128)
        GW = 1024
        zb_ = self.ar.bf16(32 * (GW + 1))
        wbo = [self.ar.bf16(32 * 128) for _ in range(2)]
        xb = [self.ar.f32(GW + 1) for _ in range(2)]
        zs = GW + 1
        groups = [(g0, min(GW, T - g0)) for g0 in range(0, T, GW)]
        nw = 0
        for gi, (g0, gn) in enumerate(groups):
            lastg = gi == len(groups) - 1
            ncol = gn + 1 if lastg else gn
            P.dma(zb_.v(0, [(zs, 32), (1, ncol)]), zT.rearrange("e p t -> p e t")[:, :, g0:g0 + ncol], ["zT"], [zb_.name])
            blks = [(b0, min(512, gn - b0)) for b0 in range(0, gn, 512)] + ([(gn, 1)] if lastg else [])
            for ft in range(KT):
                s2 = nw % 2
                nw += 1
                P.dma(wbo[s2].v(0, [(128, 32), (1, 128)]), wov[:, :, ft * 128:(ft + 1) * 128], (), [wbo[s2].name], q="pool")
                P.dma(xb[s2].v(0, [(1, ncol)]), xtv[:, ft, g0:g0 + ncol], ["xT"], [xb[s2].name], q="sp")
                for bi, (b0, w) in enumerate(blks):
                    pb = self.pb[(s2 * 3 + bi) % 8]
                    col = 1 if (lastg and b0 == gn) else 0
                    for e_ in range(32):
                        self.mm(pb.v(0, [(1, w)]), wbo[s2].v(e_ * 128, [(1, 128)]), zb_.v(e_ * zs + b0, [(1, w)]), e_ == 0, e_ == 31,
                                [wbo[s2].name, zb_.name], [pb.name])
                    xv = xb[s2].v(b0, [(1, w)])
                    self.stt(xv, pb.v(0, [(1, w)]), self.modT.v(2 * 32 + 2 * ft + col, [(1, 1)]), xv, ALU.mult, ALU.add,
                             [pb.name, self.modT.name, xb[s2].name], [xb[s2].name])
                P.dma(xtv[:, ft, g0:g0 + ncol], xb[s2].v(0, [(1, ncol)]), [xb[s2].name], ["xT"], q="sp", join=True)
        self.stage_end()

    def st_split(self):
        P, T = self.P, self.T
        H = T // 2
        hf = self.din("hf", [1, 2])
        fl = self.cst_tmp("hf", 2)
        P.dma(fl.ap, hf.to_broadcast([128, 2]), (), [fl.name])
        x3 = self.scratch("xT3", [KT, 128, H + 1])
        xtv = self.xT.rearrange("k p t -> p k t")
        x3v = x3.rearrange("k p t -> p k t")
        for k in range(KT):
            a = self.ar.f32(H)
            b = self.ar.f32(H)
            P.dma(a.ap, xtv[:, k, 0:H], ["xT"], [a.name])
            P.dma(b.ap, xtv[:, k, H:T], ["xT"], [b.name], q="pool")
            self.ts(a.ap, a.ap, fl.v(0, [(1, 1)]), ALU.mult, [a.name, fl.name], [a.name])
            self.stt(b.ap, b.ap, fl.v(1, [(1, 1)]), a.ap, ALU.mult, ALU.add, [a.name, b.name, fl.name], [b.name])
            P.dma(x3v[:, k, 0:H], b.ap, [b.name], ["xT3"], q="pool", join=True)
            if k % 4 == 3:
                self.ar.reset()
        sc_ = self.cst_tmp("xs_s", KT)
        P.dma(sc_.v(0, [(1, KT), (1, 1)]), xtv[:, :, T:T + 1], ["xT"], [sc_.name])
        P.dma(x3v[:, :, H:H + 1], sc_.v(0, [(1, KT), (1, 1)]), [sc_.name], ["xT3"], q="pool", join=True)
        self.stage_end()
        self.T, self.TT, self.xT = H, H + 1, x3


def build(T=2048, layers=4, split=True):
    k = K(T)
    k.setup_consts()
    k.stage_end()
    k.st_loadx()
    k.st_mod_setup()
    hT = k.scratch("hT", [KT, 128, k.TT], BF16)
    h32T = k.scratch("h32T", [KT, 128, k.TT])
    for i in range(layers):
        kind, j = i % 3, i // 3
        k.st_mod(i)
        if kind == 0:
            k.st_norm(k.A1, 0, None, h32T)
            k.st_pool(j, h32T)
        elif kind == 1:
            k.st_norm(k.A1, 0, hT)
            k.st_dil(hT)
        else:
            k.st_norm(k.A1, 0, hT)
            k.st_ret(hT)
        if i == layers - 1 and split:
            k.st_split()
            hT = k.scratch("hT3", [KT, 128, k.TT], BF16)
        k.st_norm(k.A2, 3, hT)
        k.st_peer_route(i, hT)
        k.st_peer_main(i, hT)
    k.st_final()
    nc = k.P.emit()
    return k, nc


_CACHE = {}


def kernel(x_prompt, x_sample, state_pool, cache_dil_kv0, cache_dil_kv1, cache_dil_kv2, state_ret,
           c_prompt, c_sample, norm1_g, norm2_g, mod_w, mod_b, pool_w, pool_scale,
           dil_w_in, dil_w_out, ret_w_in, ret_gn_g, ret_w_out,
           peer_w_q, peer_keys, peer_u, peer_v, final_g):
    f = lambda a: np.ascontiguousarray(np.asarray(a), dtype=np.float32)
    T = x_prompt.shape[1]
    k, nc = build(T)
    shared = dict(mod_w=f(mod_w), mod_b=f(mod_b), norm1_g=f(norm1_g), norm2_g=f(norm2_g), pool_w=f(pool_w), pool_scale=f(pool_scale),
                  dil_w_in=f(dil_w_in), dil_w_out=f(dil_w_out), ret_w_in=f(ret_w_in), ret_gn_g=f(ret_gn_g), ret_w_out=f(ret_w_out),
                  peer_w_q=f(peer_w_q), peer_keys=f(peer_keys), peer_u=f(peer_u), peer_v=f(peer_v), final_g=f(final_g))
    caches = [f(cache_dil_kv0), f(cache_dil_kv1), f(cache_dil_kv2)]
    x_prompt, x_sample, state_pool, state_ret = f(x_prompt), f(x_sample), f(state_pool), f(state_ret)
    c_prompt, c_sample = f(c_prompt), f(c_sample)
    in_maps = []
    for ci in range(8):
        b, s = ci // 2, ci
        m = dict(shared)
        m.update(x_p=x_prompt[b], x_s=x_sample[s], c_p=c_prompt[b], c_s=c_sample[s],
                 state_pool=np.ascontiguousarray(state_pool[:, s]), state_ret=np.ascontiguousarray(state_ret[:, s]))
        for g in range(3):
            m["cache_dil_kv%d" % g] = np.ascontiguousarray(caches[g][:, s])
        m["hf"] = np.array([[1.0 - ci % 2, ci % 2]], dtype=np.float32)
        in_maps.append({n: m[n] for n in k.inp})
    res = run_bass_kernel_spmd(nc, in_maps, core_ids=list(range(8)))
    R = res.results
    B = x_prompt.shape[0]
    half = T // 2

    def prompt(name):
        return np.stack([R[2 * b][name] for b in range(B)], axis=0)

    y_prompt = np.stack([np.concatenate([R[2 * b]["y_p"], R[2 * b + 1]["y_p"]], axis=0) for b in range(B)], axis=0)
    y_sample = np.stack([R[s]["y_s"] for s in range(8)], axis=0)
    pool_p = np.stack([R[2 * b]["pool_p"] for b in range(B)], axis=1)
    pool_s = np.stack([R[s]["pool_s"] for s in range(8)], axis=1)
    outs = [y_prompt, y_sample, pool_p, pool_s]
    for g in range(3):
        outs.append(np.stack([R[2 * b + (g % 2)]["kv%d_p" % g] for b in range(B)], axis=1))
        outs.append(np.stack([R[s]["kv%d_s" % g] for s in range(8)], axis=1))
    outs.append(np.stack([R[2 * b + 1]["ret_p"] for b in range(B)], axis=1))
    outs.append(np.stack([R[s]["ret_s"] for s in range(8)], axis=1))
    return tuple(np.ascontiguousarray(o, dtype=np.float32) for o in outs)
```

```python
from contextlib import ExitStack
import math
import numpy as np
import concourse.bass as bass
import concourse.mybir as mybir
from concourse.ap import AP
from concourse.bass_utils import run_bass_kernel_spmd

F32 = mybir.dt.float32
BF16 = mybir.dt.bfloat16
U32 = mybir.dt.uint32
I32 = mybir.dt.int32
AF = mybir.ActivationFunctionType
ALU = mybir.AluOpType
AX = mybir.AxisListType

ENGS = ("pe", "dve", "act", "pool", "sp")
DMAQ = ("sp", "pool")
NDMA = 24
D = 2048
KT = 16
EPS = 1e-6


class Op:
    __slots__ = ("eng", "fn", "deps", "dma", "tok", "need", "pre")


class Prog:
    def __init__(self):
        self.nc = bass.Bass("TRN2", target_bir_lowering=False)
        self.es = ExitStack()
        self.ops = []
        self.lastw = {}
        self.readers = {}
        self.last_eng = {}
        self.recent_dma = {q: [] for q in DMAQ}
        self.uid = 0

    def sb(self, name, shape, dt=F32):
        return self.es.enter_context(self.nc.sbuf_tensor(name, list(shape), dt))

    def ps(self, name, shape, dt=F32):
        return self.es.enter_context(self.nc.psum_tensor(name, list(shape), dt))

    def dram(self, name, shape, dt=F32, kind="Internal"):
        return self.nc.dram_tensor(name, list(shape), dt, kind=kind).ap()

    def op(self, eng, fn, r=(), w=(), dma=False, join=False, extra=()):
        o = Op()
        o.eng, o.fn, o.dma, o.need, o.tok, o.pre = eng, fn, dma, False, None, None
        deps = set(extra)
        for x in r:
            deps.update(self.lastw.get(x, ()))
        for x in w:
            deps.update(self.lastw.get(x, ()))
            deps.update(self.readers.get(x, ()))
        if eng == "pe":
            deps = {d for d in deps if d.eng != "pe"}
        o.deps = deps
        for d in deps:
            d.need = True
        for x in r:
            lst = self.readers.setdefault(x, [])
            if not dma:
                lst[:] = [q for q in lst if q.dma or q.eng != eng]
            lst.append(o)
        for x in w:
            if join and x in self.lastw:
                self.lastw[x].append(o)
            else:
                self.lastw[x] = [o]
            self.readers[x] = []
        self.ops.append(o)
        if dma:
            self.recent_dma[eng].append(o)
            if len(self.recent_dma[eng]) > NDMA:
                self.recent_dma[eng].pop(0)
        else:
            self.last_eng[eng] = o
        return o

    def dma(self, out, in_, r=(), w=(), q="sp", join=False, **kw):
        kw.setdefault("allow_slow_non_contiguous", True)
        return self.op(q, lambda e: e.dma_start(out=out, in_=in_, **kw), r, w, dma=True, join=join)

    def barrier(self):
        deps = list(self.last_eng.values())
        for q in DMAQ:
            deps += self.recent_dma[q]
        for e in ENGS:
            self.op(e, lambda h: h.nop(), extra=deps)
        self.lastw = {}
        self.readers = {}

    def emit(self):
        nc = self.nc
        es = self.es
        sems = {e: es.enter_context(nc.semaphore("s_" + e)) for e in ENGS}
        dsem = {q: [es.enter_context(nc.semaphore("d_%s%d" % (q, i))) for i in range(NDMA)] for q in DMAQ}
        cnt = {e: 0 for e in ENGS}
        dcnt = {q: 0 for q in DMAQ}
        duse = {q: [0] * NDMA for q in DMAQ}
        for o in self.ops:
            if o.dma:
                i = dcnt[o.eng] % NDMA
                dcnt[o.eng] += 1
                prev = duse[o.eng][i]
                duse[o.eng][i] += 16
                o.pre = (dsem[o.eng][i], prev) if prev else None
                o.tok = (dsem[o.eng][i], prev + 16)
            elif o.need:
                cnt[o.eng] += 1
                o.tok = (sems[o.eng], cnt[o.eng])
        self.sem_counts = dict(cnt)
        per = {e: [o for o in self.ops if o.eng == e] for e in ENGS}
        block = es.enter_context(nc.Block())

        def run(engname, handle):
            known = {}
            for o in per[engname]:
                ws = []
                if o.pre is not None:
                    ws.append(o.pre)
                for d in o.deps:
                    ws.append(d.tok)
                for (s, v) in ws:
                    k = id(s)
                    if known.get(k, 0) >= v:
                        continue
                    known[k] = v
                    handle.wait_ge(s, v)
                inst = o.fn(handle)
                if o.tok is not None:
                    inst.then_inc(o.tok[0], 16 if o.dma else 1)
            if engname in dsem:
                for i in range(NDMA):
                    if duse[engname][i]:
                        handle.wait_ge(dsem[engname][i], duse[engname][i])

        @block.tensor
        def _(e):
            run("pe", e)

        @block.vector
        def _(e):
            run("dve", e)

        @block.scalar
        def _(e):
            run("act", e)

        @block.gpsimd
        def _(e):
            run("pool", e)

        @block.sync
        def _(e):
            run("sp", e)

        es.close()
        return nc


class Buf:
    def __init__(self, ap2d, name):
        self.ap = ap2d
        self.name = name
        self.t = ap2d.tensor
        self.off = ap2d.offset
        self.ps = ap2d.ap[0][0]
        self.n = ap2d.shape[1]

    def v(self, off=0, dims=None, p0=0, npart=128):
        if dims is None:
            dims = [(1, self.n - off)]
        return AP(self.t, self.off + p0 * self.ps + off, [[self.ps, npart]] + [[s, n] for (s, n) in dims])

    def __getitem__(self, k):
        return self.ap[k]


class Arena:
    def __init__(self, P, name, words):
        self.P = P
        self.t = P.sb(name, [128, words], F32)
        self.words = words
        self.off = 0
        self.gen = 0
        self.name = name

    def reset(self):
        self.off = getattr(self, "base", 0)
        self.gen += 1

    def f32(self, n, tag=""):
        assert self.off + n <= self.words, ("arena overflow", self.name, self.off, n, self.words)
        b = Buf(self.t[:, self.off:self.off + n], "%s_%d" % (self.name, self.off))
        self.off += n
        return b

    def bf16(self, n, tag=""):
        nw = (n + 1) // 2
        assert self.off + nw <= self.words, ("arena overflow", self.name, self.off, nw, self.words)
        b = Buf(self.t[:, self.off:self.off + nw].bitcast(BF16), "%s_%d" % (self.name, self.off))
        self.off += nw
        return b

    def u32(self, n, tag=""):
        assert self.off + n <= self.words
        b = Buf(self.t[:, self.off:self.off + n].bitcast(U32), "%s_%d" % (self.name, self.off))
        self.off += n
        return b


def blocks_of(T):
    bl = [(t0, min(512, T - t0)) for t0 in range(0, T, 512)]
    return bl + [(T, 1)]


class K:
    def __init__(self, T, dbg=()):
        self.T = T
        self.TT = T + 1
        self.P = Prog()
        self.dbg = set(dbg)
        P = self.P
        nc = P.nc
        self.inp = {}
        self.out = {}
        self.ar = Arena(P, "ar", 45 * 1024)
        self.cst = Arena(P, "cst", 2 * 1024)
        self.pb = [Buf(P.ps("pb%d" % i, [128, 512], F32)[:, :], "pb%d" % i) for i in range(8)]

    def din(self, name, shape, dt=F32):
        a = self.P.nc.dram_tensor(name, list(shape), dt, kind="ExternalInput").ap()
        self.inp[name] = a
        return a

    def dout(self, name, shape, dt=F32):
        a = self.P.nc.dram_tensor(name, list(shape), dt, kind="ExternalOutput").ap()
        self.out[name] = a
        return a

    def scratch(self, name, shape, dt=F32):
        if name in self.dbg:
            return self.dout(name, shape, dt)
        return self.P.dram(name, shape, dt)

    def mm(self, out, lhsT, rhs, start, stop, r, w):
        self.P.op("pe", lambda e: e.matmul(out, lhsT, rhs, start=start, stop=stop), r, w)

    def tr(self, out, in_, ident, r, w):
        self.P.op("pe", lambda e: e.transpose(out, in_, ident), r, w)

    def act(self, out, in_, func, r, w, bias=0.0, scale=1.0, accum_out=None):
        if accum_out is None:
            self.P.op("act", lambda e: e.activation(out, in_, func, bias=bias, scale=scale), r, w)
        else:
            self.P.op("act", lambda e: e.activation(out, in_, func, bias=bias, scale=scale, accum_out=accum_out), r, w)

    def tt(self, out, in0, in1, op, r, w, eng="dve"):
        self.P.op(eng, lambda e: e.tensor_tensor(out, in0, in1, op), r, w)

    def ts(self, out, in0, s1, op0, r, w, s2=None, op1=None, eng="dve"):
        if op1 is None:
            self.P.op(eng, lambda e: e.tensor_scalar(out, in0, s1, None, op0), r, w)
        else:
            self.P.op(eng, lambda e: e.tensor_scalar(out, in0, s1, s2, op0, op1), r, w)

    def stt(self, out, in0, scalar, in1, op0, op1, r, w):
        self.P.op("dve", lambda e: e.scalar_tensor_tensor(out, in0, scalar, in1, op0, op1), r, w)

    def cp(self, out, in_, r, w, eng="dve"):
        if eng == "act":
            self.P.op("act", lambda e: e.copy(out, in_), r, w)
        else:
            self.P.op(eng, lambda e: e.tensor_copy(out, in_), r, w)

    def memset(self, ap, val, w, eng="dve"):
        self.P.op(eng, lambda e: e.memset(ap, val), (), w)

    def stage_end(self):
        self.P.barrier()
        self.ar.base = 0
        self.ar.reset()

    def setup_consts(self):
        P, c = self.P, self.cst
        self.identf = c.f32(128, "idf")
        self.identb = c.bf16(128, "idb")
        self.onesf = c.f32(128, "onf")
        self.onesb = c.bf16(128, "onb")
        self.iotaf = c.f32(128, "iof")
        tmp = self.ar.f32(128)
        tmpi = Buf(tmp.ap.bitcast(I32), tmp.name)
        P.op("pool", lambda e: e.iota(tmpi.ap, [[1, 128]], base=0, channel_multiplier=-1), (), [tmp.name])
        self.ts(self.identf.ap, tmpi.ap, 0.0, ALU.is_equal, [tmp.name], [self.identf.name])
        self.cp(self.identb.ap, self.identf.ap, [self.identf.name], [self.identb.name])
        self.memset(self.onesf.ap, 1.0, [self.onesf.name])
        self.memset(self.onesb.ap, 1.0, [self.onesb.name])
        tmp2 = self.ar.f32(128)
        tmp2i = Buf(tmp2.ap.bitcast(I32), tmp2.name)
        P.op("pool", lambda e: e.iota(tmp2i.ap, [[1, 128]], base=0, channel_multiplier=0), (), [tmp2.name])
        self.cp(self.iotaf.ap, tmp2i.ap, [tmp2.name], [self.iotaf.name])

    def load_vecT(self, dst, dname, src_ap, n):
        raw = self.ar.f32(128)
        pb = self.pb[7]
        self.P.dma(raw.v(0, [(1, 128)], 0, n), src_ap.rearrange("(k p) -> k p", p=128), (), [raw.name])
        self.tr(pb.v(0, [(1, n)]), raw.v(0, [(1, 128)], 0, n), self.identf.v(0, [(1, n)], 0, n), [raw.name, self.identf.name], [pb.name])
        self.cp(dst, pb.v(0, [(1, n)]), [pb.name], [dname])

    def st_loadx(self):
        T, P = self.T, self.P
        x_p = self.din("x_p", [T, D])
        x_s = self.din("x_s", [1, D])
        self.xT = self.scratch("xT", [KT, 128, self.TT])
        xtv = self.xT.rearrange("k p t -> p k t")
        for ti in range(T // 128):
            raw = self.ar.f32(D)
            xt = self.ar.f32(D)
            P.dma(raw.ap, x_p[ti * 128:(ti + 1) * 128, :], (), [raw.name])
            for q in range(4):
                pb = self.pb[q]
                for j in range(4):
                    k = q * 4 + j
                    self.tr(pb.v(j * 128, [(1, 128)]), raw.v(k * 128, [(1, 128)]), self.identf.ap, [raw.name, self.identf.name], [pb.name])
                self.cp(xt.v(q * 512, [(1, 512)]), pb.ap, [pb.name], [xt.name], eng="act" if q % 2 else "dve")
            P.dma(xtv[:, :, ti * 128:(ti + 1) * 128], xt.v(0, [(128, KT), (1, 128)]), [xt.name], ["xT"], q="pool", join=True)
            if ti % 2 == 1:
                self.ar.reset()
        self.stage_end()
        col = self.ar.f32(KT)
        self.load_vecT(col.ap, col.name, x_s[0, :], KT)
        P.dma(xtv[:, :, T:T + 1], col.v(0, [(1, KT), (1, 1)]), [col.name], ["xT"], q="pool", join=True)
        self.stage_end()

    def st_mod_setup(self):
        c_p = self.din("c_p", [D])
        c_s = self.din("c_s", [D])
        self.mod_w = self.din("mod_w", [4, D, 6 * D])
        self.mod_b = self.din("mod_b", [4, 6 * D])
        self.norm1_g = self.din("norm1_g", [4, D])
        self.norm2_g = self.din("norm2_g", [4, D])
        self.sc = self.cst.f32(KT * 2, "sc")
        self.modT = self.cst.f32(96 * 2, "modT")
        self.A1 = self.cst.f32(KT * 2, "A1")
        self.A2 = self.cst.f32(KT * 2, "A2")
        craw = self.ar.f32(KT * 2)
        self.load_vecT(craw.v(0, [(2, KT)]), craw.name, c_p, KT)
        self.load_vecT(craw.v(1, [(2, KT)]), craw.name, c_s, KT)
        self.act(self.sc.ap, craw.ap, AF.Silu, [craw.name], [self.sc.name])
        self.stage_end()

    def st_mod(self, i):
        P = self.P
        pb = self.pb[0]
        wv = self.mod_w[i].rearrange("(k p) f -> p k f", p=128)
        bufs = [self.ar.f32(KT * 512, "mw%d" % j) for j in range(2)]
        for fc in range(24):
            b = bufs[fc % 2]
            P.dma(b.v(0, [(512, KT), (1, 512)]), wv[:, :, fc * 512:(fc + 1) * 512], (), [b.name], q="sp" if fc % 2 == 0 else "pool")
            for ft in range(4):
                f = fc * 4 + ft
                for k in range(KT):
                    self.mm(pb.v(f * 2, [(1, 2)]), b.v(k * 512 + ft * 128, [(1, 128)]), self.sc.v(k * 2, [(1, 2)]),
                            k == 0, k == KT - 1, [b.name, self.sc.name], [pb.name])
        self.mod_finish(i, pb)
        self.stage_end()

    def mod_finish(self, i, pb, nxt=False):
        if nxt:
            if not hasattr(self, "_alt"):
                self._alt = (self.cst.f32(96 * 2), self.cst.f32(KT * 2), self.cst.f32(KT * 2))
            m, A1, A2 = self._alt
        else:
            m, A1, A2 = self.modT, self.A1, self.A2
        mb = self.ar.f32(128)
        self.load_vecT(mb.v(0, [(1, 96)]), mb.name, self.mod_b[i], 96)
        g1 = self.ar.f32(128)
        g2 = self.ar.f32(128)
        self.load_vecT(g1.v(0, [(1, KT)]), g1.name, self.norm1_g[i], KT)
        self.load_vecT(g2.v(0, [(1, KT)]), g2.name, self.norm2_g[i], KT)
        self.tt(m.v(0, [(2, 96), (1, 2)]), pb.v(0, [(2, 96), (1, 2)]), mb.v(0, [(1, 96), (0, 2)]), ALU.add,
                [pb.name, mb.name], [m.name])
        self.stt(A1.v(0, [(2, KT), (1, 2)]), m.v(1 * 32, [(2, KT), (1, 2)]), 1.0, g1.v(0, [(1, KT), (0, 2)]), ALU.add, ALU.mult,
                 [m.name, g1.name], [A1.name])
        self.stt(A2.v(0, [(2, KT), (1, 2)]), m.v(4 * 32, [(2, KT), (1, 2)]), 1.0, g2.v(0, [(1, KT), (0, 2)]), ALU.add, ALU.mult,
                 [m.name, g2.name], [A2.name])

    def mod_swap(self):
        cur = (self.modT, self.A1, self.A2)
        self.modT, self.A1, self.A2 = self._alt
        self._alt = cur

    def modv(self, idx, col):
        return self.modT.v(idx * 32 + col, [(2, KT)])

    def st_norm(self, A, shift_idx, hT, h32T=None, gvec=None):
        P, T = self.P, self.T
        xtv = self.xT.rearrange("k p t -> p k t")
        hv = hT.rearrange("k p t -> p k t") if hT is not None else None
        h32v = h32T.rearrange("k p t -> p k t") if h32T is not None else None
        for bi, (t0, w) in enumerate(blocks_of(T)):
            col = 1 if t0 == T else 0
            x = self.ar.f32(KT * 512, "x")
            sq = self.ar.f32(KT * 512, "sq")
            rs = self.ar.f32(512, "rs")
            hb = self.ar.bf16(KT * 512, "hb")
            pb = self.pb[bi % 2]
            xv = x.v(0, [(512, KT), (1, w)])
            sqv = sq.v(0, [(512, KT), (1, w)])
            P.dma(xv, xtv[:, :, t0:t0 + w], ["xT"], [x.name])
            self.act(sqv, xv, AF.Square, [x.name], [sq.name])
            for k in range(KT):
                self.mm(pb.v(0, [(1, w)]), self.onesf.ap, sq.v(k * 512, [(1, w)]), k == 0, k == KT - 1,
                        [sq.name, self.onesf.name], [pb.name])
            self.act(rs.v(0, [(1, w)]), pb.v(0, [(1, w)]), AF.Sqrt, [pb.name], [rs.name], bias=EPS, scale=1.0 / D)
            P.op("dve", lambda e, o=rs.v(0, [(1, w)]): e.reciprocal(o, o), [rs.name], [rs.name])
            self.tt(sqv, xv, rs.v(0, [(0, KT), (1, w)]), ALU.mult, [x.name, rs.name], [sq.name])
            if A is not None:
                self.tt(sqv, sqv, A.v(col, [(2, KT), (0, w)]), ALU.mult, [sq.name, A.name], [sq.name])
                self.tt(sqv, sqv, self.modT.v(shift_idx * 32 + col, [(2, KT), (0, w)]), ALU.add, [sq.name, self.modT.name], [sq.name])
            else:
                self.tt(sqv, sqv, gvec.v(0, [(1, KT), (0, w)]), ALU.mult, [sq.name, gvec.name], [sq.name])
            if hv is not None:
                self.cp(hb.v(0, [(512, KT), (1, w)]), sqv, [sq.name], [hb.name], eng="act")
                P.dma(hv[:, :, t0:t0 + w], hb.v(0, [(512, KT), (1, w)]), [hb.name], [hT.tensor.name], q="pool", join=True)
            if h32v is not None:
                P.dma(h32v[:, :, t0:t0 + w], sqv, [sq.name], [h32T.tensor.name], q="pool", join=True)
            if bi % 2 == 1:
                self.ar.reset()
        self.stage_end()

    def fm_to_rows(self, srcT, t0, n, dst_rows, r0=0):
        P = self.P
        src = self.ar.f32(KT * 128, "f2r")
        rows = self.ar.f32(D, "rows")
        P.dma(src.v(0, [(128, KT), (1, n)]), srcT.rearrange("k p t -> p k t")[:, :, t0:t0 + n], [srcT.tensor.name], [src.name])
        for q in range(4):
            pb = self.pb[4 + q]
            for j in range(4):
                k = q * 4 + j
                self.tr(pb.v(j * 128, [(1, 128)], 0, n), src.v(k * 128, [(1, n)]), self.identf.ap, [src.name, self.identf.name], [pb.name])
            self.cp(rows.v(q * 512, [(1, 512)], 0, n), pb.v(0, [(1, 512)], 0, n), [pb.name], [rows.name], eng="act" if q % 2 else "dve")
        P.dma(dst_rows, rows.v(0, [(1, D)], r0, n - r0), [rows.name], [dst_rows.tensor.name], q="pool", join=True)

    def st_final(self):
        T = self.T
        fg = self.din("final_g", [D])
        y_p = self.dout("y_p", [T, D])
        y_s = self.dout("y_s", [1, D])
        g = self.ar.f32(KT)
        self.load_vecT(g.ap, g.name, fg, KT)
        gk = self.cst.f32(KT, "fg")
        self.cp(gk.ap, g.ap, [g.name], [gk.name])
        self.stage_end()
        yT = self.scratch("yT", [KT, 128, self.TT])
        self.st_norm(None, 0, None, yT, gvec=gk)
        for ti in range(T // 128):
            self.fm_to_rows(yT, ti * 128, 128, y_p[ti * 128:(ti + 1) * 128, :])
            if ti % 2 == 1:
                self.ar.reset()
        self.stage_end()
        self.fm_to_rows(yT, T, 1, y_s[0:1, :])
        self.stage_end()

    def dump(self, name, view, shape, r):
        o = self.dout(name, shape)
        self.P.dma(o, view, r, [name], q="sp")

    def st_pool(self, j, h32T):
        P, T, TT = self.P, self.T, self.TT
        if j == 0:
            self.state_pool = self.din("state_pool", [2, 15, D])
            self.pool_w = self.din("pool_w", [2, 4, 512, 512])
            self.pool_scale = self.din("pool_scale", [2, D])
            self.pool_p = self.dout("pool_p", [2, 15, D])
            self.pool_s = self.dout("pool_s", [2, 15, D])
        self.fm_to_rows(h32T, T - 128, 128, self.pool_p[j], r0=113)
        P.dma(self.pool_s[j, 0:14, :], self.state_pool[j, 1:15, :], (), ["pool_s"], q="pool", join=True)
        self.stage_end()
        self.fm_to_rows(h32T, T, 1, self.pool_s[j, 14:15, :])
        self.stage_end()
        ps_ = self.ar.f32(KT)
        self.load_vecT(ps_.ap, ps_.name, self.pool_scale[j], KT)
        sg = self.cst_tmp("sg", KT * 2)
        self.tt(sg.v(0, [(2, KT), (1, 2)]), self.modT.v(2 * 32, [(2, KT), (1, 2)]), ps_.v(0, [(1, KT), (0, 2)]), ALU.mult,
                [self.modT.name, ps_.name], [sg.name])
        ext = self.cst_tmp("ext", KT * 16)
        hraw = self.ar.f32(D)
        P.dma(hraw.v(0, [(1, D)], 0, 15), self.state_pool[j], (), [hraw.name])
        for k in range(KT):
            pb = self.pb[4 + k % 4]
            self.tr(pb.v(0, [(1, 15)]), hraw.v(k * 128, [(1, 128)], 0, 15), self.identf.v(0, [(1, 15)], 0, 15),
                    [hraw.name, self.identf.name], [pb.name])
            self.cp(ext.v(k * 16, [(1, 15)]), pb.v(0, [(1, 15)]), [pb.name], [ext.name])
        P.dma(ext.v(15, [(16, KT), (1, 1)]), h32T.rearrange("k p t -> p k t")[:, :, T:T + 1], [h32T.tensor.name], [ext.name])
        self.stage_end()
        for g in range(4):
            w = 2 << g
            A = self.ar.f32(4 * T)
            B = self.ar.f32(4 * T)
            C = self.ar.f32(4 * T)
            dT = self.ar.bf16(4 * TT)
            X = self.ar.f32(4 * TT)
            Wf = self.ar.f32(4 * 512)
            Wb = self.ar.bf16(4 * 512)
            red = self.ar.f32(4)
            v3 = lambda b, o=0, n=T, s=T: b.v(o, [(s, 4), (1, n)])
            P.dma(v3(A), h32T[4 * g:4 * g + 4].rearrange("k p t -> p k t")[:, :, 0:T], [h32T.tensor.name], [A.name])
            P.dma(v3(X, 0, TT, TT), self.xT[4 * g:4 * g + 4].rearrange("k p t -> p k t"), ["xT"], [X.name], q="pool")
            P.dma(Wf.v(0, [(512, 4), (1, 512)]), self.pool_w[j, g].rearrange("(c p) e -> p c e", p=128), (), [Wf.name])
            self.cp(Wb.ap, Wf.ap, [Wf.name], [Wb.name], eng="act")
            cur = A
            for si in range(g + 1):
                st = 1 << si
                nxt = B if si % 2 == 0 else C
                self.cp(v3(nxt, 0, st), v3(cur, 0, st), [cur.name], [nxt.name], eng="act")
                self.tt(v3(nxt, st, T - st), v3(cur, st, T - st), v3(cur, 0, T - st), ALU.add, [cur.name], [nxt.name])
                cur = nxt
            self.stt(v3(dT, 0, T, TT), v3(cur), 1.0 / w, v3(A), ALU.mult, ALU.subtract, [cur.name, A.name], [dT.name])
            for t in range(w - 1):
                self.stt(v3(dT, t, 1, TT), v3(cur, t, 1), 1.0 / (t + 1), v3(A, t, 1), ALU.mult, ALU.subtract, [cur.name, A.name], [dT.name])
            P.op("dve", lambda e, o=red.ap, i=ext.v(4 * g * 16 + 16 - w, [(16, 4), (1, w)]): e.tensor_reduce(o, i, AX.X, ALU.add),
                 [ext.name], [red.name])
            self.stt(v3(dT, T, 1, TT), red.v(0, [(1, 4), (1, 1)]), 1.0 / w, ext.v(4 * g * 16 + 15, [(16, 4), (1, 1)]), ALU.mult, ALU.subtract,
                     [red.name, ext.name], [dT.name])
            nb = 0
            for et in range(4):
                k = 4 * g + et
                for (t0, wd) in blocks_of(T):
                    pb = self.pb[nb % 4]
                    nb += 1
                    col = 1 if t0 == T else 0
                    for c in range(4):
                        self.mm(pb.v(0, [(1, wd)]), Wb.v(c * 512 + et * 128, [(1, 128)]), dT.v(c * TT + t0, [(1, wd)]), c == 0, c == 3,
                                [Wb.name, dT.name], [pb.name])
                    xv = X.v(et * TT + t0, [(1, wd)])
                    self.stt(xv, pb.v(0, [(1, wd)]), sg.v(k * 2 + col, [(1, 1)]), xv, ALU.mult, ALU.add, [pb.name, sg.name, X.name], [X.name])
            P.dma(self.xT[4 * g:4 * g + 4].rearrange("k p t -> p k t"), v3(X, 0, TT, TT), [X.name], ["xT"], q="pool", join=True)
            self.ar.reset()
        self.stage_end()

    def cst_tmp(self, name, n):
        if not hasattr(self, "_ct"):
            self._ct = {}
        if name not in self._ct:
            self._ct[name] = self.cst.f32(n)
        return self._ct[name]

    def st_peer_route(self, i, hT, premod=None):
        P, T, TT = self.P, self.T, self.TT
        if not hasattr(self, "peer_wq"):
            self.peer_wq = self.din("peer_w_q", [4, D, 2048])
            self.peer_keys = self.din("peer_keys", [4, 2, 128, 128])
            self.peer_u = self.din("peer_u", [4, 16384, D])
            self.peer_v = self.din("peer_v", [4, 16384, D])
            self.Gsamp = self.cst.bf16(128)
        if getattr(self, "_route_T", None) != T:
            self._route_T = T
            self.scd = self.scratch("scd%d" % T, [T // 128 + 1, 128, 16, 128])
            self.Gd = self.scratch("Gd%d" % T, [128, 128, T], BF16)
        ntile = T // 128 + 1
        hs = self.ar.bf16(KT * TT)
        P.dma(hs.v(0, [(TT, KT), (1, TT)]), hT.rearrange("k p t -> p k t"), [hT.tensor.name], [hs.name])
        keysT = self.ar.f32(256)
        kraw = self.ar.f32(256)
        P.dma(kraw.v(0, [(128, 2), (1, 128)]), self.peer_keys[i].rearrange("a m k -> m a k"), (), [kraw.name])
        for p in range(2):
            pb = self.pb[6 + p]
            self.tr(pb.v(0, [(1, 128)]), kraw.v(p * 128, [(1, 128)]), self.identf.ap, [kraw.name, self.identf.name], [pb.name])
            self.cp(keysT.v(p * 128, [(1, 128)]), pb.v(0, [(1, 128)]), [pb.name], [keysT.name])
        wb = [self.ar.bf16(KT * 128) for _ in range(2)]
        wqv = self.peer_wq[i].rearrange("(k p) f -> p k f", p=128)

        def load_wq(ft_):
            P.dma(wb[ft_ % 2].v(0, [(128, KT), (1, 128)]), wqv[:, :, ft_ * 128:(ft_ + 1) * 128], (), [wb[ft_ % 2].name], q="pool")
        load_wq(0)
        qf = [self.ar.f32(TT) for _ in range(2)]
        stg = [self.ar.f32(ntile * 128) for _ in range(2)]
        for s_ in stg:
            self.memset(s_.ap, 0.0, [s_.name], eng="pool")
        nb = 0
        for ft in range(16):
            s = ft % 2
            if ft + 1 < 16:
                load_wq(ft + 1)
            for (t0, w) in blocks_of(T):
                pb = self.pb[nb % 3]
                nb += 1
                for k in range(KT):
                    self.mm(pb.v(0, [(1, w)]), wb[s].v(k * 128, [(1, 128)]), hs.v(k * TT + t0, [(1, w)]), k == 0, k == KT - 1,
                            [wb[s].name, hs.name], [pb.name])
                self.cp(qf[s].v(t0, [(1, w)]), pb.v(0, [(1, w)]), [pb.name], [qf[s].name], eng="act")
            for tt_ in range(ntile):
                m = 128 if tt_ < ntile - 1 else 1
                pb = self.pb[3 + tt_ % 3]
                self.mm(pb.v(0, [(1, 128)], 0, m), qf[s].v(tt_ * 128, [(1, m)]), keysT.v((ft % 2) * 128, [(1, 128)]), True, True,
                        [qf[s].name, keysT.name], [pb.name])
                self.cp(stg[s].v(tt_ * 128, [(1, 128)], 0, m), pb.v(0, [(1, 128)], 0, m), [pb.name], [stg[s].name])
            P.dma(self.scd[:, :, ft, :].rearrange("n p m -> p n m"), stg[s].v(0, [(128, ntile), (1, 128)]), [stg[s].name], [self.scd.tensor.name],
                  q="pool", join=True)
        self.stage_end()
        NEG = -1.0e30
        sc2 = [self.ar.f32(2048) for _ in range(2)]
        self.ar.base = self.ar.off

        def sc_load(t_):
            if t_ < ntile:
                P.dma(sc2[t_ % 2].ap, self.scd[t_].rearrange("p f m -> p (f m)"), [self.scd.tensor.name], [sc2[t_ % 2].name])
        sc_load(0)
        if premod is not None:
            mw = [self.ar.f32(KT * 512) for _ in range(2)]
            self.ar.base = self.ar.off
            mwv = self.mod_w[premod].rearrange("(k p) f -> p k f", p=128)
            pbm = self.pb[4]

            def mod_load(fc):
                if fc < 24:
                    P.dma(mw[fc % 2].v(0, [(512, KT), (1, 512)]), mwv[:, :, fc * 512:(fc + 1) * 512], (), [mw[fc % 2].name], q="sp")

            def mod_mm(fc):
                if fc < 24:
                    b_ = mw[fc % 2]
                    for ft_ in range(4):
                        f = fc * 4 + ft_
                        for k in range(KT):
                            self.mm(pbm.v(f * 2, [(1, 2)]), b_.v(k * 512 + ft_ * 128, [(1, 128)]), self.sc.v(k * 2, [(1, 2)]),
                                    k == 0, k == KT - 1, [b_.name, self.sc.name], [pbm.name])
            mod_load(0)
        for tt_ in range(ntile):
            m = 128 if tt_ < ntile - 1 else 1
            sc = sc2[tt_ % 2]
            sc_load(tt_ + 1)
            scr = self.ar.f32(256)
            vals = self.ar.f32(256)
            idx = self.ar.u32(256)
            idxf = self.ar.f32(256)
            cand = self.ar.f32(2048)
            top = self.ar.f32(128)
            sel = self.ar.u32(128)
            selb = self.ar.u32(128)
            af = self.ar.f32(128)
            bf = self.ar.f32(128)
            gg = self.ar.f32(128)
            es = self.ar.f32(8)
            eq = self.ar.f32(2048)
            i1 = self.ar.f32(128)
            i2 = self.ar.f32(128)
            i1T = self.ar.f32(128)
            i2T = self.ar.f32(128)
            gT = self.ar.f32(128)
            oh1 = [self.ar.bf16(8 * 128) for _ in range(6)]
            oh2 = [self.ar.bf16(8 * 128) for _ in range(3)]
            Gs = self.ar.bf16(128 * 128)
            V = lambda b, o, n: b.v(o, [(1, n)])
            for ft in range(16):
                s_in = V(sc, ft * 128, 128)
                P.op("dve", lambda e, o=V(vals, ft * 16, 8), i=s_in: e.max(out=o, in_=i), [sc.name], [vals.name])
                P.op("dve", lambda e, o=V(idx, ft * 16, 8), mx=V(vals, ft * 16, 8), i=s_in: e.max_index(o, mx, i), [sc.name, vals.name], [idx.name])
                P.op("dve", lambda e, o=V(scr, 0, 128), mx=V(vals, ft * 16, 8), i=s_in: e.match_replace(o, mx, i, NEG),
                     [sc.name, vals.name], [scr.name])
                P.op("dve", lambda e, o=V(vals, ft * 16 + 8, 8), i=V(scr, 0, 128): e.max(out=o, in_=i), [scr.name], [vals.name])
                P.op("dve", lambda e, o=V(idx, ft * 16 + 8, 8), mx=V(vals, ft * 16 + 8, 8), i=V(scr, 0, 128): e.max_index(o, mx, i),
                     [scr.name, vals.name], [idx.name])
            self.cp(idxf.ap, idx.ap, [idx.name], [idxf.name])
            self.tt(cand.v(0, [(256, 8), (16, 16), (1, 16)]), vals.v(0, [(32, 8), (1, 16), (0, 16)]), vals.v(16, [(32, 8), (0, 16), (1, 16)]),
                    ALU.add, [vals.name], [cand.name])
            for h in range(8):
                c_in = V(cand, h * 256, 256)
                P.op("dve", lambda e, o=V(top, h * 16, 8), i=c_in: e.max(out=o, in_=i), [cand.name], [top.name])
                P.op("dve", lambda e, o=V(sel, h * 16, 8), mx=V(top, h * 16, 8), i=c_in: e.max_index(o, mx, i), [cand.name, top.name], [sel.name])
                P.op("dve", lambda e, o=V(scr, 0, 256), mx=V(top, h * 16, 8), i=c_in: e.match_replace(o, mx, i, NEG),
                     [cand.name, top.name], [scr.name])
                P.op("dve", lambda e, o=V(top, h * 16 + 8, 8), i=V(scr, 0, 256): e.max(out=o, in_=i), [scr.name], [top.name])
                P.op("dve", lambda e, o=V(sel, h * 16 + 8, 8), mx=V(top, h * 16 + 8, 8), i=V(scr, 0, 256): e.max_index(o, mx, i),
                     [scr.name, top.name], [sel.name])
            self.tt(gg.v(0, [(16, 8), (1, 16)]), top.v(0, [(16, 8), (1, 16)]), top.v(0, [(16, 8), (0, 16)]), ALU.subtract, [top.name], [gg.name])
            self.act(gg.ap, gg.ap, AF.Exp, [gg.name], [gg.name])
            P.op("dve", lambda e, o=es.ap, i=gg.v(0, [(16, 8), (1, 16)]): e.tensor_reduce(o, i, AX.X, ALU.add), [gg.name], [es.name])
            P.op("dve", lambda e, o=es.ap: e.reciprocal(o, o), [es.name], [es.name])
            self.tt(gg.v(0, [(16, 8), (1, 16)]), gg.v(0, [(16, 8), (1, 16)]), es.v(0, [(1, 8), (0, 16)]), ALU.mult, [gg.name, es.name], [gg.name])
            self.ts(selb.ap, sel.ap, 4, ALU.logical_shift_right, [sel.name], [selb.name])
            self.cp(af.ap, selb.ap, [selb.name], [af.name])
            self.ts(selb.ap, sel.ap, 15, ALU.bitwise_and, [sel.name, af.name], [selb.name])
            self.cp(bf.ap, selb.ap, [selb.name], [bf.name])
            for (src, off, dst) in ((af, 0, i1), (bf, 16, i2)):
                self.tt(eq.v(0, [(256, 8), (16, 16), (1, 16)]), src.v(0, [(16, 8), (1, 16), (0, 16)]), self.iotaf.v(0, [(0, 8), (0, 16), (1, 16)]),
                        ALU.is_equal, [src.name, self.iotaf.name], [eq.name])
                self.tt(eq.v(0, [(256, 8), (16, 16), (1, 16)]), eq.v(0, [(256, 8), (16, 16), (1, 16)]), idxf.v(off, [(32, 8), (0, 16), (1, 16)]),
                        ALU.mult, [eq.name, idxf.name], [eq.name])
                P.op("dve", lambda e, o=dst.ap, i=eq.v(0, [(16, 128), (1, 16)]): e.tensor_reduce(o, i, AX.X, ALU.add), [eq.name], [dst.name])
            for (src, dst, bank) in ((i1, i1T, 5), (i2, i2T, 6), (gg, gT, 7)):
                pb = self.pb[bank]
                self.tr(pb.v(0, [(1, 128)]), src.ap, self.identf.ap, [src.name, self.identf.name], [pb.name])
                self.cp(dst.ap, pb.v(0, [(1, 128)]), [pb.name], [dst.name], eng="act")
            NB = 8
            for t8 in range(0, m, NB):
                nbt = min(NB, m - t8)
                sl = (t8 // NB) % 3
                E1, O1, O2 = oh1[sl], oh1[3 + sl], oh2[sl]
                v3 = lambda b_: b_.v(0, [(128, nbt), (1, 128)])
                io = self.iotaf.v(0, [(0, nbt), (1, 128)])
                self.tt(v3(E1), io, i1T.v(t8, [(1, nbt), (0, 128)]), ALU.is_equal, [self.iotaf.name, i1T.name], [E1.name])
                self.tt(v3(O1), v3(E1), gT.v(t8, [(1, nbt), (0, 128)]), ALU.mult, [E1.name, gT.name], [O1.name])
                self.tt(v3(O2), io, i2T.v(t8, [(1, nbt), (0, 128)]), ALU.is_equal, [self.iotaf.name, i2T.name], [O2.name])
                for tl in range(nbt):
                    t = t8 + tl
                    pb = self.pb[(t // 4) % 4]
                    self.mm(pb.v(t % 4, [(4, 128)]), O2.v(tl * 128, [(1, 128)]), O1.v(tl * 128, [(1, 128)]), True, True,
                            [O1.name, O2.name], [pb.name])
                    if t % 4 == 3 or t == m - 1:
                        n4 = t % 4 + 1
                        tb = t - (t % 4)
                        self.cp(Gs.v(tb, [(128, 128), (1, n4)]), pb.v(0, [(4, 128), (1, n4)]), [pb.name], [Gs.name], eng="act")
            if m == 128:
                for cq in range(4):
                    P.dma(self.Gd[cq * 32:(cq + 1) * 32, :, tt_ * 128:(tt_ + 1) * 128].rearrange("c p t -> p c t"),
                          Gs.v(cq * 32 * 128, [(128, 32), (1, 128)]), [Gs.name], [self.Gd.tensor.name], q="pool" if cq % 2 else "sp", join=True)
            else:
                self.cp(self.Gsamp.ap, Gs.v(0, [(128, 128)]), [Gs.name], [self.Gsamp.name])
            if premod is not None:
                mod_load(tt_ + 1)
                mod_mm(tt_)
            self.ar.reset()
        if premod is not None:
            for fc in range(ntile, 24):
                mod_load(fc + 1)
                mod_mm(fc)
            self.P.barrier()
            self.mod_finish(premod, pbm, nxt=True)
        self.stage_end()

    def st_peer_main(self, i, hT):
        P, T, TT = self.P, self.T, self.TT
        NCH = self.nchunks if hasattr(self, "nchunks") else 128
        groups = [(t0, min(1024, T - t0)) for t0 in range(0, T, 1024)]
        hv = hT.rearrange("k p t -> p k t")
        for gi, (t0, n) in enumerate(groups):
            last = gi == len(groups) - 1
            ne = n + 1 if last else n
            blks = [(b0, min(512, n - b0)) for b0 in range(0, n, 512)]
            acc = self.ar.f32(KT * ne)
            hg = self.ar.bf16(KT * ne)
            ubf = [self.ar.bf16(D) for _ in range(3)]
            uT = [self.ar.bf16(D) for _ in range(2)]
            vbf = [self.ar.bf16(D) for _ in range(8)]
            WT = [self.ar.bf16(ne) for _ in range(4)]
            actb = [self.ar.bf16(ne) for _ in range(2)]
            Gc = [self.ar.bf16(n) for _ in range(2)]
            xbs = [self.ar.f32(n) for _ in range(2)]
            P.dma(hg.v(0, [(ne, KT), (1, n)]), hv[:, :, t0:t0 + n], [hT.tensor.name], [hg.name])
            if last:
                P.dma(hg.v(n, [(ne, KT), (1, 1)]), hv[:, :, T:T + 1], [hT.tensor.name], [hg.name], join=True)
            pS = [self.pb[0], self.pb[1]]
            pSs = self.pb[2]
            pT = [self.pb[3], self.pb[4]]
            pV = [self.pb[5], self.pb[6]]
            pVs = self.pb[7]
            nvc = [0]

            def loadUV(c):
                P.dma(ubf[c % 3].ap, self.peer_u[i, c * 128:(c + 1) * 128, :], (), [ubf[c % 3].name], q="pool")
                P.dma(vbf[c % 8].ap, self.peer_v[i, c * 128:(c + 1) * 128, :], (), [vbf[c % 8].name], q="pool")

            def loadG(c):
                P.dma(Gc[c % 2].ap, self.Gd[c, :, t0:t0 + n], [self.Gd.tensor.name], [Gc[c % 2].name], q="sp")

            def tr(c):
                ub = ubf[c % 3]
                for hb in range(2):
                    pt = pT[hb]
                    ptb = Buf(pt.ap.bitcast(BF16), pt.name)
                    for kk in range(8):
                        k = hb * 8 + kk
                        self.tr(ptb.v(kk * 128, [(1, 128)]), ub.v(k * 128, [(1, 128)]), self.identb.ap, [ub.name, self.identb.name], [pt.name])
                    self.cp(uT[c % 2].v(hb * 1024, [(1, 1024)]), ptb.v(0, [(1, 1024)]), [pt.name], [uT[c % 2].name], eng="act")

            def compute(c):
                ci, s2 = c % 4, c % 2
                for bi, (b0, wd) in enumerate(blks):
                    for k in range(KT):
                        self.mm(pS[bi].v(0, [(1, wd)]), uT[s2].v(k * 128, [(1, 128)]), hg.v(k * ne + b0, [(1, wd)]), k == 0, k == KT - 1,
                                [uT[s2].name, hg.name], [pS[bi].name])
                    self.act(actb[s2].v(b0, [(1, wd)]), pS[bi].v(0, [(1, wd)]), AF.Gelu, [pS[bi].name], [actb[s2].name])
                if last:
                    for k in range(KT):
                        self.mm(pSs.v(0, [(1, 1)]), uT[s2].v(k * 128, [(1, 128)]), hg.v(k * ne + n, [(1, 1)]), k == 0, k == KT - 1,
                                [uT[s2].name, hg.name], [pSs.name])
                    self.act(actb[s2].v(n, [(1, 1)]), pSs.v(0, [(1, 1)]), AF.Gelu, [pSs.name], [actb[s2].name])
                    self.tt(WT[ci].v(n, [(1, 1)]), actb[s2].v(n, [(1, 1)]), self.Gsamp.v(c, [(1, 1)]), ALU.mult,
                            [actb[s2].name, self.Gsamp.name], [WT[ci].name])
                self.tt(WT[ci].v(0, [(1, n)]), actb[s2].v(0, [(1, n)]), Gc[s2].ap, ALU.mult, [actb[s2].name, Gc[s2].name], [WT[ci].name])

            def vphase(c):
                first = c == 3
                vs = [vbf[(c - 3 + cj) % 8] for cj in range(4)]
                for dt in range(KT):
                    for (b0, wd) in blks:
                        pv = pV[nvc[0] % 2]
                        nvc[0] += 1
                        for cj in range(4):
                            self.mm(pv.v(0, [(1, wd)]), vs[cj].v(dt * 128, [(1, 128)]), WT[cj].v(b0, [(1, wd)]), cj == 0, cj == 3,
                                    [vs[cj].name, WT[cj].name], [pv.name])
                        av = acc.v(dt * ne + b0, [(1, wd)])
                        if first:
                            self.cp(av, pv.v(0, [(1, wd)]), [pv.name], [acc.name])
                        else:
                            self.tt(av, pv.v(0, [(1, wd)]), av, ALU.add, [pv.name, acc.name], [acc.name])
                if last:
                    for dt in range(KT):
                        for cj in range(4):
                            self.mm(pVs.v(dt, [(1, 1)]), vs[cj].v(dt * 128, [(1, 128)]), WT[cj].v(n, [(1, 1)]), cj == 0, cj == 3,
                                    [vs[cj].name, WT[cj].name], [pVs.name])
                    av = acc.v(n, [(ne, KT)])
                    if first:
                        self.cp(av, pVs.v(0, [(1, KT)]), [pVs.name], [acc.name])
                    else:
                        self.tt(av, pVs.v(0, [(1, KT)]), av, ALU.add, [pVs.name, acc.name], [acc.name])

            for c in range(min(3, NCH)):
                loadUV(c)
            for c in range(min(2, NCH)):
                loadG(c)
            tr(0)
            for c in range(NCH):
                if c + 3 < NCH:
                    loadUV(c + 3)
                if c + 1 < NCH:
                    tr(c + 1)
                compute(c)
                if c + 2 < NCH:
                    loadG(c + 2)
                if c % 4 == 3:
                    vphase(c)
            xtv = self.xT.rearrange("k p t -> p k t")
            for k in range(KT):
                xb = xbs[k % 2]
                P.dma(xb.v(0, [(1, n)]), xtv[:, k, t0:t0 + n], ["xT"], [xb.name])
                self.stt(xb.v(0, [(1, n)]), acc.v(k * ne, [(1, n)]), self.modT.v(5 * 32 + 2 * k, [(1, 1)]), xb.v(0, [(1, n)]), ALU.mult, ALU.add,
                         [acc.name, self.modT.name, xb.name], [xb.name])
                P.dma(xtv[:, k, t0:t0 + n], xb.v(0, [(1, n)]), [xb.name], ["xT"], q="pool", join=True)
            if last:
                xs = self.cst_tmp("xs_s", KT)
                tmpg = self.cst_tmp("xs_g", KT)
                P.dma(xs.v(0, [(1, KT), (1, 1)]), xtv[:, :, T:T + 1], ["xT"], [xs.name])
                self.tt(tmpg.v(0, [(1, KT)]), acc.v(n, [(ne, KT)]), self.modT.v(5 * 32 + 1, [(2, KT)]), ALU.mult, [acc.name, self.modT.name], [tmpg.name])
                self.tt(xs.v(0, [(1, KT)]), xs.v(0, [(1, KT)]), tmpg.v(0, [(1, KT)]), ALU.add, [tmpg.name, xs.name], [xs.name])
                P.dma(xtv[:, :, T:T + 1], xs.v(0, [(1, KT), (1, 1)]), [xs.name], ["xT"], q="pool", join=True)
            self.stage_end()


    def st_dil(self, hT):
        P, T, TT = self.P, self.T, self.TT
        DILS = (1, 4, 16)
        WIN = (128, 512, 2048)
        w_in = self.din("dil_w_in", [1, D, 9216])[0]
        w_out = self.din("dil_w_out", [1, 1024, D])[0]
        caches = [self.din("cache_dil_kv%d" % g, [1, WIN[g], 2, 8, 128])[0] for g in range(3)]
        kvp = [self.dout("kv%d_p" % g, [1, min(WIN[g], T), 2, 8, 128])[0] for g in range(3)]
        kvs = [self.dout("kv%d_s" % g, [1, WIN[g], 2, 8, 128])[0] for g in range(3)]
        winv = w_in.rearrange("(k p) f -> p k f", p=128)
        scale = 128 ** -0.5
        NEG = -1.0e30
        for g in range(3):
            Wg = WIN[g]
            nsp = 4 if Wg > 512 else 1
            for q_ in range(nsp):
                a = 1 + (Wg - 1) * q_ // nsp
                b = 1 + (Wg - 1) * (q_ + 1) // nsp
                P.dma(kvs[g][a - 1:b - 1].rearrange("w a h d -> w (a h d)"), caches[g][a:b].rearrange("w a h d -> w (a h d)"), (), ["kvs%d" % g],
                      q="pool", join=True)
        D2 = self.ar.f32(256)
        M2 = self.ar.f32(256)
        B2 = self.ar.f32(256)
        bS = self.ar.f32(1)
        bSh = self.ar.f32(1)
        ti = self.ar.f32(256)
        tii = Buf(ti.ap.bitcast(I32), ti.name)
        P.op("pool", lambda e: e.iota(tii.v(0, [(1, 128)]), [[1, 128]], base=128, channel_multiplier=-1), (), [ti.name])
        P.op("pool", lambda e: e.iota(tii.v(128, [(1, 128)]), [[1, 128]], base=0, channel_multiplier=-1), (), [ti.name])
        self.cp(D2.ap, tii.ap, [ti.name], [D2.name])
        self.ts(M2.v(0, [(1, 128)]), D2.v(0, [(1, 128)]), 128.0, ALU.is_gt, [D2.name], [M2.name], s2=NEG, op1=ALU.mult)
        self.ts(M2.v(128, [(1, 128)]), D2.v(128, [(1, 128)]), 0.0, ALU.is_lt, [D2.name], [M2.name], s2=NEG, op1=ALU.mult)
        P.op("pool", lambda e: e.iota(tii.v(0, [(1, 1)]), [[1, 1]], base=-128, channel_multiplier=1), (), [ti.name])
        self.cp(bS.ap, tii.v(0, [(1, 1)]), [ti.name], [bS.name])
        hs = self.ar.bf16(KT * TT)
        P.dma(hs.v(0, [(TT, KT), (1, TT)]), hT.rearrange("k p t -> p k t"), [hT.tensor.name], [hs.name])
        oT = self.ar.bf16(8 * TT)
        accO = self.ar.f32(T)
        accD = self.ar.f32(T)
        qT = self.ar.bf16(T)
        kT = self.ar.bf16(T)
        Vh = self.ar.bf16(16 * 128)
        wbf6 = [self.ar.bf16(D) for _ in range(6)]
        PT = [self.ar.bf16(256) for _ in range(2)]
        tmpS = [self.ar.f32(256) for _ in range(2)]
        rows = [self.ar.f32(128) for _ in range(3)]
        vcol = self.ar.f32(1)
        Kc = self.ar.f32(128)
        Vc = self.ar.f32(128)
        prod = self.ar.f32(128)
        sS = self.ar.f32(2)
        pS_ = self.ar.f32(2)
        oS = self.ar.f32(2)
        tS = self.ar.f32(2)
        def load_hg(it):
            h_, g_ = it // 3, it % 3
            for s_ in range(3):
                col = s_ * 3072 + g_ * 1024 + h_ * 128
                wd_ = wbf6[(it % 2) * 3 + s_]
                P.dma(wd_.v(0, [(128, KT), (1, 128)]), winv[:, :, col:col + 128], (), [wd_.name], q="pool")
        load_hg(0)
        for h in range(8):
            for g in range(3):
                dil = DILS[g]
                L = T // dil
                bpr = L // 128
                slope = 2.0 ** (-8.0 * (g * 8 + h + 1) / 24.0)
                it = h * 3 + g
                if it + 1 < 24:
                    load_hg(it + 1)
                wbf = wbf6[(it % 2) * 3:(it % 2) * 3 + 3]
                for (dst, wi, sc_) in ((qT, 0, scale), (kT, 1, 1.0)):
                    for bi, (t0, w) in enumerate(blocks_of(T)[:-1]):
                        pb = self.pb[bi % 2]
                        for k in range(KT):
                            self.mm(pb.v(0, [(1, w)]), wbf[wi].v(k * 128, [(1, 128)]), hs.v(k * TT + t0, [(1, w)]), k == 0, k == KT - 1,
                                    [wbf[wi].name, hs.name], [pb.name])
                        self.act(dst.v(t0 // dil, [(L, dil), (1, w // dil)]), pb.v(0, [(1, dil), (dil, w // dil)]), AF.Copy, [pb.name], [dst.name],
                                 scale=sc_)
                for jb in range(16):
                    r_ = (jb * 128) // L
                    i0 = (jb * 128) % L
                    pb = self.pb[7]
                    for k in range(KT):
                        self.mm(pb.v(0, [(1, 128)]), hs.v(k * TT + i0 * dil + r_, [(dil, 128)]), wbf[2].v(k * 128, [(1, 128)]), k == 0, k == KT - 1,
                                [wbf[2].name, hs.name], [pb.name])
                    self.cp(Vh.v(jb * 128, [(1, 128)]), pb.v(0, [(1, 128)]), [pb.name], [Vh.name])
                self.stt(B2.ap, D2.ap, -slope * dil, M2.ap, ALU.mult, ALU.add, [D2.name, M2.name], [B2.name])
                for jb in range(16):
                    hasprev = (jb % bpr) != 0
                    ks = [jb - 1, jb] if hasprev else [jb]
                    o0 = 0 if hasprev else 128
                    nk = 128 * len(ks)
                    pSb = self.pb[2]
                    s2 = jb % 2
                    for x, kb in enumerate(ks):
                        self.mm(pSb.v(o0 + x * 128, [(1, 128)]), kT.v(kb * 128, [(1, 128)]), qT.v(jb * 128, [(1, 128)]), True, True,
                                [kT.name, qT.name], [pSb.name])
                    self.tt(tmpS[s2].v(o0, [(1, nk)]), pSb.v(o0, [(1, nk)]), B2.v(o0, [(1, nk)]), ALU.add, [pSb.name, B2.name], [tmpS[s2].name])
                    self.act(PT[s2].v(o0, [(1, nk)]), tmpS[s2].v(o0, [(1, nk)]), AF.Exp, [tmpS[s2].name], [PT[s2].name])
                    pO, pD = self.pb[3 + 2 * (jb % 2)], self.pb[4 + 2 * (jb % 2)]
                    for x, kb in enumerate(ks):
                        self.mm(pO.v(0, [(1, 128)]), Vh.v(kb * 128, [(1, 128)]), PT[s2].v(o0 + x * 128, [(1, 128)]), x == 0, x == len(ks) - 1,
                                [Vh.name, PT[s2].name], [pO.name])
                    for x, kb in enumerate(ks):
                        self.mm(pD.v(0, [(1, 128)]), self.onesb.ap, PT[s2].v(o0 + x * 128, [(1, 128)]), x == 0, x == len(ks) - 1,
                                [self.onesb.name, PT[s2].name], [pD.name])
                    r_ = (jb * 128) // L
                    i0 = (jb * 128) % L
                    for (acc_, pb_) in ((accO, pO), (accD, pD)):
                        av = acc_.v(i0 * dil + r_, [(dil, 128)])
                        if g == 0:
                            self.cp(av, pb_.v(0, [(1, 128)]), [pb_.name], [acc_.name], eng="act" if acc_ is accD else "dve")
                        else:
                            self.tt(av, pb_.v(0, [(1, 128)]), av, ALU.add, [pb_.name, acc_.name], [acc_.name])
                pr = self.pb[7]
                for s_ in range(3):
                    for k in range(KT):
                        self.mm(pr.v(s_ * 128, [(1, 128)]), hs.v(k * TT + T, [(0, 128)]), wbf[s_].v(k * 128, [(1, 128)]), k == 0, k == KT - 1,
                                [wbf[s_].name, hs.name], [pr.name])
                    self.cp(rows[s_].ap, pr.v(s_ * 128, [(1, 128)]), [pr.name], [rows[s_].name])
                for k in range(KT):
                    self.mm(pr.v(384, [(1, 1)]), wbf[2].v(k * 128, [(1, 128)]), hs.v(k * TT + T, [(1, 1)]), k == 0, k == KT - 1,
                            [wbf[2].name, hs.name], [pr.name])
                self.cp(vcol.ap, pr.v(384, [(1, 1)]), [pr.name], [vcol.name])
                Wg = WIN[g]
                P.dma(kvs[g][Wg - 1:Wg, 0, h, :], rows[1].v(0, [(1, 128)], 0, 1), [rows[1].name], ["kvs%d" % g], q="pool", join=True)
                P.dma(kvs[g][Wg - 1:Wg, 1, h, :], rows[2].v(0, [(1, 128)], 0, 1), [rows[2].name], ["kvs%d" % g], q="pool", join=True)
                cv = caches[g].rearrange("(i s) a h d -> i s a h d", s=dil)
                P.dma(Kc.ap, cv[:, 0, 0, h, :], (), [Kc.name])
                P.dma(Vc.ap, cv[:, 0, 1, h, :], (), [Vc.name])
                self.tt(prod.ap, Kc.ap, rows[0].ap, ALU.mult, [Kc.name, rows[0].name], [prod.name])
                P.op("dve", lambda e, o=sS.v(0, [(1, 1)]), i=prod.ap: e.tensor_reduce(o, i, AX.X, ALU.add), [prod.name], [sS.name])
                self.tt(prod.ap, rows[1].ap, rows[0].ap, ALU.mult, [rows[1].name, rows[0].name, sS.name], [prod.name])
                P.op("dve", lambda e, o=sS.v(1, [(1, 1)]), i=prod.ap: e.tensor_reduce(o, i, AX.X, ALU.add), [prod.name], [sS.name])
                self.ts(bSh.ap, bS.ap, slope * dil, ALU.mult, [bS.name], [bSh.name])
                self.act(pS_.v(0, [(1, 1)]), sS.v(0, [(1, 1)]), AF.Exp, [sS.name, bSh.name], [pS_.name], bias=bSh.ap, scale=scale)
                self.act(pS_.v(1, [(1, 1)]), sS.v(1, [(1, 1)]), AF.Exp, [sS.name], [pS_.name], scale=scale)
                po = self.pb[2]
                self.mm(po.v(0, [(1, 1)]), Vc.ap, pS_.v(0, [(1, 1)]), True, True, [Vc.name, pS_.name], [po.name])
                self.mm(po.v(1, [(1, 1)]), self.onesf.ap, pS_.v(0, [(1, 1)]), True, True, [self.onesf.name, pS_.name], [po.name])
                self.stt(tS.v(0, [(1, 1)]), vcol.ap, pS_.v(1, [(1, 1)]), po.v(0, [(1, 1)]), ALU.mult, ALU.add, [vcol.name, pS_.name, po.name], [tS.name])
                self.tt(tS.v(1, [(1, 1)]), po.v(1, [(1, 1)]), pS_.v(1, [(1, 1)]), ALU.add, [po.name, pS_.name, tS.name], [tS.name])
                if g == 0:
                    self.cp(oS.ap, tS.ap, [tS.name], [oS.name])
                else:
                    self.tt(oS.ap, oS.ap, tS.ap, ALU.add, [tS.name, oS.name], [oS.name])
            P.op("dve", lambda e, o=accD.ap: e.reciprocal(o, o), [accD.name], [accD.name])
            self.tt(oT.v(h * TT, [(1, T)]), accO.ap, accD.ap, ALU.mult, [accO.name, accD.name], [oT.name])
            P.op("dve", lambda e, o=oS.v(1, [(1, 1)]): e.reciprocal(o, o), [oS.name], [oS.name])
            self.tt(oT.v(h * TT + T, [(1, 1)]), oS.v(0, [(1, 1)]), oS.v(1, [(1, 1)]), ALU.mult, [oS.name], [oT.name])
        xtv = self.xT.rearrange("k p t -> p k t")
        wov = w_out.rearrange("(e p) f -> p e f", p=128)
        for ft in range(KT):
            wb = wbf6[ft % 2]
            xb = [accO, accD][ft % 2]
            xs_ = tmpS[ft % 2]
            P.dma(wb.v(0, [(128, 8), (1, 128)]), wov[:, :, ft * 128:(ft + 1) * 128], (), [wb.name], q="pool")
            P.dma(xb.ap, xtv[:, ft, 0:T], ["xT"], [xb.name], q="pool")
            P.dma(xs_.v(0, [(1, 1)]), xtv[:, ft, T:T + 1], ["xT"], [xs_.name], q="pool")
            for bi, (t0, w) in enumerate(blocks_of(T)):
                pb = self.pb[bi % 2]
                col = 1 if t0 == T else 0
                for e_ in range(8):
                    self.mm(pb.v(0, [(1, w)]), wb.v(e_ * 128, [(1, 128)]), oT.v(e_ * TT + t0, [(1, w)]), e_ == 0, e_ == 7, [wb.name, oT.name], [pb.name])
                if col == 0:
                    xv = xb.v(t0, [(1, w)])
                    self.stt(xv, pb.v(0, [(1, w)]), self.modT.v(2 * 32 + 2 * ft, [(1, 1)]), xv, ALU.mult, ALU.add, [pb.name, self.modT.name, xb.name], [xb.name])
                else:
                    xv = xs_.v(0, [(1, 1)])
                    self.stt(xv, pb.v(0, [(1, 1)]), self.modT.v(2 * 32 + 2 * ft + 1, [(1, 1)]), xv, ALU.mult, ALU.add, [pb.name, self.modT.name, xs_.name], [xs_.name])
            P.dma(xtv[:, ft, 0:T], xb.ap, [xb.name], ["xT"], q="pool", join=True)
            P.dma(xtv[:, ft, T:T + 1], xs_.v(0, [(1, 1)]), [xs_.name], ["xT"], q="pool", join=True)
        self.stage_end()
        hs = self.ar.bf16(KT * TT)
        P.dma(hs.v(0, [(TT, KT), (1, TT)]), hT.rearrange("k p t -> p k t"), [hT.tensor.name], [hs.name])
        self.kvw = [self.ar.bf16(KT * 512) for _ in range(2)]
        self.kvo = [self.ar.f32(512) for _ in range(2)]
        nrot = 0
        for g in range(3):
            nl = min(WIN[g], T)
            for s_ in (1, 2):
                for half in range(2):
                    col = s_ * 3072 + g * 1024 + half * 512
                    kvw = self.kvw[nrot % 2]
                    nrot += 1
                    P.dma(kvw.v(0, [(512, KT), (1, 512)]), winv[:, :, col:col + 512], (), [kvw.name], q="pool")
                    for ti_ in range(nl // 128):
                        tok0 = T - nl + ti_ * 128
                        pb = self.pb[ti_ % 2]
                        for k in range(KT):
                            self.mm(pb.ap, hs.v(k * TT + tok0, [(1, 128)]), kvw.v(k * 512, [(1, 512)]), k == 0, k == KT - 1,
                                    [hs.name, kvw.name], [pb.name])
                        ob = self.kvo[ti_ % 2]
                        self.cp(ob.ap, pb.ap, [pb.name], [ob.name], eng="act" if ti_ % 2 else "dve")
                        P.dma(kvp[g][ti_ * 128:(ti_ + 1) * 128, s_ - 1, half * 4:(half + 1) * 4, :].rearrange("t h d -> t (h d)"), ob.ap,
                              [ob.name], ["kvp%d" % g], q="pool", join=True)
        self.stage_end()

    def st_ret(self, hT):
        P, T, TT = self.P, self.T, self.TT
        w_in = self.din("ret_w_in", [1, D, 12288])[0]
        gn_g = self.din("ret_gn_g", [1, 4096])
        w_out = self.din("ret_w_out", [1, 4096, D])[0]
        st_in = self.din("state_ret", [1, 8, 256, 512])[0]
        ret_p = self.dout("ret_p", [1, 8, 256, 512])[0]
        ret_s = self.dout("ret_s", [1, 8, 256, 512])[0]
        zT = self.scratch("zT", [32, 128, TT], BF16)
        winv = w_in.rearrange("(k p) f -> p k f", p=128)
        NCK = T // 128
        hs = self.ar.bf16(KT * TT)
        P.dma(hs.v(0, [(TT, KT), (1, TT)]), hT.rearrange("k p t -> p k t"), [hT.tensor.name], [hs.name])
        wb2 = [self.ar.bf16(KT * 512) for _ in range(2)]
        qT = self.ar.bf16(2 * T)
        kT = self.ar.bf16(2 * T)
        ktok = self.ar.bf16(NCK * 256)
        vtok = self.ar.bf16(NCK * 512)
        sg = self.ar.bf16((NCK + 1) * 512)
        S = self.ar.f32(1024)
        Sb = self.ar.bf16(1024)
        attTb = self.ar.bf16(128)
        qd = self.ar.bf16(256)
        osb = self.ar.f32(512)
        cen = self.ar.f32(512)
        zb = self.ar.bf16(512)
        zst = self.ar.bf16(512)
        dmaskT = self.ar.f32(128)
        qdecT = self.ar.f32(128)
        Dcl = self.ar.f32(128)
        msk = self.ar.f32(128)
        gng = self.ar.f32(512)
        vrow = self.ar.f32(512)
        cols = self.ar.f32(8)
        stat = self.ar.f32(8)
        ti = self.ar.f32(128)
        tii = Buf(ti.ap.bitcast(I32), ti.name)
        P.op("pool", lambda e: e.iota(tii.ap, [[1, 128]], base=0, channel_multiplier=-1), (), [ti.name])
        self.cp(Dcl.ap, tii.ap, [ti.name], [Dcl.name])
        self.ts(msk.ap, Dcl.ap, 0.0, ALU.is_ge, [Dcl.name], [msk.name])
        self.ts(Dcl.ap, Dcl.ap, 0.0, ALU.max, [Dcl.name, msk.name], [Dcl.name])
        P.op("pool", lambda e: e.iota(tii.v(0, [(1, 1)]), [[1, 1]], base=127, channel_multiplier=-1), (), [ti.name])
        self.cp(cols.v(5, [(1, 1)]), tii.v(0, [(1, 1)]), [ti.name], [cols.name])
        P.op("pool", lambda e: e.iota(tii.ap, [[1, 128]], base=1, channel_multiplier=0), (), [ti.name])
        cp1 = self.ar.f32(128)
        self.cp(cp1.ap, tii.ap, [ti.name], [cp1.name])

        wl = []
        for h_ in range(8):
            wl += [(h_ * 256, 256), (2048 + h_ * 256, 256), (4096 + h_ * 512, 512), (8192 + h_ * 512, 512)]
        wcnt = [0]

        def issue_w(n_):
            if n_ < len(wl):
                col0, ncols = wl[n_]
                P.dma(wb2[n_ % 2].v(0, [(ncols, KT), (1, ncols)]), winv[:, :, col0:col0 + ncols], (), [wb2[n_ % 2].name], q="pool")

        def load_w(col0, ncols):
            n_ = wcnt[0]
            assert wl[n_] == (col0, ncols)
            wcnt[0] += 1
            issue_w(n_ + 1)
            return wb2[n_ % 2]
        issue_w(0)

        for h in range(8):
            gam = 1.0 - 2.0 ** (-5.0 - h)
            lg = math.log(gam)
            cdec = gam ** 128
            self.act(dmaskT.ap, Dcl.ap, AF.Exp, [Dcl.name], [dmaskT.name], scale=lg)
            self.tt(dmaskT.ap, dmaskT.ap, msk.ap, ALU.mult, [dmaskT.name, msk.name], [dmaskT.name])
            self.act(qdecT.ap, cp1.ap, AF.Exp, [cp1.name], [qdecT.name], scale=lg)
            self.act(cols.v(4, [(1, 1)]), cols.v(5, [(1, 1)]), AF.Exp, [cols.name], [cols.name], scale=lg)
            self.ts(cols.v(4, [(1, 1)]), cols.v(4, [(1, 1)]), 1.0 / 16.0, ALU.mult, [cols.name], [cols.name])
            P.dma(gng.ap, gn_g[0:1, h * 512:(h + 1) * 512].to_broadcast([128, 512]), (), [gng.name])
            wb = load_w(h * 256, 256)
            nb = 0
            for dkt in range(2):
                for (t0, w) in blocks_of(T):
                    pb = self.pb[nb % 2]
                    nb += 1
                    for k in range(KT):
                        self.mm(pb.v(0, [(1, w)]), wb.v(k * 256 + dkt * 128, [(1, 128)]), hs.v(k * TT + t0, [(1, w)]), k == 0, k == KT - 1,
                                [wb.name, hs.name], [pb.name])
                    if t0 < T:
                        self.cp(qT.v(dkt * T + t0, [(1, w)]), pb.v(0, [(1, w)]), [pb.name], [qT.name], eng="act")
                    else:
                        self.cp(cols.v(dkt, [(1, 1)]), pb.v(0, [(1, 1)]), [pb.name], [cols.name])
            wb = load_w(2048 + h * 256, 256)
            for dkt in range(2):
                for (t0, w) in blocks_of(T):
                    pb = self.pb[nb % 2]
                    nb += 1
                    for k in range(KT):
                        self.mm(pb.v(0, [(1, w)]), wb.v(k * 256 + dkt * 128, [(1, 128)]), hs.v(k * TT + t0, [(1, w)]), k == 0, k == KT - 1,
                                [wb.name, hs.name], [pb.name])
                    if t0 < T:
                        self.act(kT.v(dkt * T + t0, [(1, w)]), pb.v(0, [(1, w)]), AF.Copy, [pb.name], [kT.name], scale=1.0 / 16.0)
                    else:
                        self.ts(cols.v(2 + dkt, [(1, 1)]), pb.v(0, [(1, 1)]), 1.0 / 16.0, ALU.mult, [pb.name], [cols.name])
            for c in range(NCK):
                pb = self.pb[2 + c % 2]
                for k in range(KT):
                    self.mm(pb.v(0, [(1, 256)]), hs.v(k * TT + c * 128, [(1, 128)]), wb.v(k * 256, [(1, 256)]), k == 0, k == KT - 1,
                            [wb.name, hs.name], [pb.name])
                self.ts(ktok.v(c * 256, [(1, 256)]), pb.v(0, [(1, 256)]), cols.v(4, [(1, 1)]), ALU.mult, [pb.name, cols.name], [ktok.name])
            wb = load_w(4096 + h * 512, 512)
            for c in range(NCK + 1):
                pb = self.pb[2 + c % 2]
                lhs = (lambda k: hs.v(k * TT + c * 128, [(1, 128)])) if c < NCK else (lambda k: hs.v(k * TT + T, [(0, 128)]))
                for k in range(KT):
                    self.mm(pb.ap, lhs(k), wb.v(k * 512, [(1, 512)]), k == 0, k == KT - 1, [wb.name, hs.name], [pb.name])
                if c < NCK:
                    self.cp(vtok.v(c * 512, [(1, 512)]), pb.ap, [pb.name], [vtok.name], eng="act" if c % 2 else "dve")
                else:
                    self.cp(vrow.ap, pb.ap, [pb.name], [vrow.name])
            wb = load_w(8192 + h * 512, 512)
            for c in range(NCK + 1):
                pb = self.pb[2 + c % 2]
                lhs = (lambda k: hs.v(k * TT + c * 128, [(1, 128)])) if c < NCK else (lambda k: hs.v(k * TT + T, [(0, 128)]))
                for k in range(KT):
                    self.mm(pb.ap, lhs(k), wb.v(k * 512, [(1, 512)]), k == 0, k == KT - 1, [wb.name, hs.name], [pb.name])
                self.act(sg.v(c * 512, [(1, 512)]), pb.ap, AF.Silu, [pb.name], [sg.name])
            self.memset(S.ap, 0.0, [S.name])
            self.memset(Sb.ap, 0.0, [Sb.name])

            def post(pO, m, c, tcol):
                pv = lambda b, o=0, n=512: b.v(o, [(1, n)], 0, m)
                sv = lambda j: stat.v(j, [(1, 1)], 0, m)
                self.act(pv(osb), pv(pO), AF.Identity, [pO.name], [osb.name, stat.name], accum_out=sv(0))
                self.ts(sv(1), sv(0), -1.0 / 512.0, ALU.mult, [stat.name], [stat.name])
                self.ts(pv(cen), pv(osb), sv(1), ALU.add, [osb.name, stat.name], [cen.name])
                self.act(pv(osb), pv(cen), AF.Square, [cen.name], [osb.name, stat.name], accum_out=sv(2))
                self.act(sv(3), sv(2), AF.Sqrt, [stat.name], [stat.name], bias=EPS, scale=1.0 / 512.0)
                P.op("dve", lambda e, o=sv(3): e.reciprocal(o, o), [stat.name], [stat.name])
                self.stt(pv(cen), pv(cen), sv(3), pv(gng), ALU.mult, ALU.mult, [cen.name, stat.name, gng.name], [cen.name])
                self.tt(pv(zb), pv(cen), pv(sg, c * 512), ALU.mult, [cen.name, sg.name], [zb.name])
                pt = self.pb[6]
                ptb = Buf(pt.ap.bitcast(BF16), pt.name)
                for et in range(4):
                    self.tr(ptb.v(et * 128, [(1, m)]), zb.v(et * 128, [(1, 128)], 0, m), self.identb.v(0, [(1, m)], 0, m),
                            [zb.name, self.identb.name], [pt.name])
                self.cp(zst.v(0, [(128, 4), (1, m)]), ptb.v(0, [(128, 4), (1, m)]), [pt.name], [zst.name], eng="act")
                P.dma(zT[h * 4:(h + 1) * 4].rearrange("e p t -> p e t")[:, :, tcol:tcol + m], zst.v(0, [(128, 4), (1, m)]), [zst.name], ["zT"],
                      q="pool", join=True)

            for c in range(NCK):
                pA = self.pb[0]
                for dkt in range(2):
                    self.mm(pA.v(0, [(1, 128)]), kT.v(dkt * T + c * 128, [(1, 128)]), qT.v(dkt * T + c * 128, [(1, 128)]), dkt == 0, dkt == 1,
                            [kT.name, qT.name], [pA.name])
                self.tt(attTb.ap, pA.v(0, [(1, 128)]), dmaskT.ap, ALU.mult, [pA.name, dmaskT.name], [attTb.name])
                self.tt(qd.v(0, [(128, 2), (1, 128)]), qT.v(c * 128, [(T, 2), (1, 128)]), qdecT.v(0, [(0, 2), (1, 128)]), ALU.mult,
                        [qT.name, qdecT.name], [qd.name])
                pO = self.pb[4 + c % 2]
                self.mm(pO.ap, attTb.ap, vtok.v(c * 512, [(1, 512)]), True, False, [attTb.name, vtok.name], [pO.name])
                for dkt in range(2):
                    self.mm(pO.ap, qd.v(dkt * 128, [(1, 128)]), Sb.v(dkt * 512, [(1, 512)]), False, dkt == 1, [qd.name, Sb.name], [pO.name])
                for dkt in range(2):
                    pSt = self.pb[2 + dkt]
                    self.mm(pSt.ap, ktok.v(c * 256 + dkt * 128, [(1, 128)]), vtok.v(c * 512, [(1, 512)]), True, True, [ktok.name, vtok.name], [pSt.name])
                    sv_ = S.v(dkt * 512, [(1, 512)])
                    self.stt(sv_, sv_, cdec, pSt.ap, ALU.mult, ALU.add, [S.name, pSt.name], [S.name])
                self.cp(Sb.ap, S.ap, [S.name], [Sb.name], eng="pool")
                post(pO, 128, c, c * 128)
            P.dma(ret_p[h].rearrange("(t p) e -> p t e", p=128), S.v(0, [(512, 2), (1, 512)]), [S.name], ["ret_p"], q="pool", join=True)
            P.dma(S.v(0, [(512, 2), (1, 512)]), st_in[h].rearrange("(t p) e -> p t e", p=128), (), [S.name])
            pO = self.pb[4]
            for dkt in range(2):
                self.mm(pO.v(0, [(1, 512)], 0, 1), cols.v(dkt, [(1, 1)]), S.v(dkt * 512, [(1, 512)]), dkt == 0, dkt == 1, [cols.name, S.name], [pO.name])
            pq = self.pb[5]
            for dkt in range(2):
                self.mm(pq.v(0, [(1, 1)], 0, 1), cols.v(dkt, [(1, 1)]), cols.v(2 + dkt, [(1, 1)]), dkt == 0, dkt == 1, [cols.name], [pq.name])
            self.cp(stat.v(4, [(1, 1)], 0, 1), pq.v(0, [(1, 1)], 0, 1), [pq.name], [stat.name])
            orow = self.pb[7]
            self.ts(cen.v(0, [(1, 512)], 0, 1), vrow.v(0, [(1, 512)], 0, 1), stat.v(4, [(1, 1)], 0, 1), ALU.mult, [vrow.name, stat.name], [cen.name])
            self.stt(osb.v(0, [(1, 512)], 0, 1), pO.v(0, [(1, 512)], 0, 1), gam, cen.v(0, [(1, 512)], 0, 1), ALU.mult, ALU.add,
                     [pO.name, cen.name], [osb.name])
            for dkt in range(2):
                sv_ = S.v(dkt * 512, [(1, 512)])
                self.ts(sv_, sv_, gam, ALU.mult, [S.name, pO.name], [S.name])
                self.stt(sv_, vrow.ap, cols.v(2 + dkt, [(1, 1)]), sv_, ALU.mult, ALU.add, [vrow.name, cols.name, S.name], [S.name])
            P.dma(ret_s[h].rearrange("(t p) e -> p t e", p=128), S.v(0, [(512, 2), (1, 512)]), [S.name], ["ret_s"], q="pool", join=True)
            post(osb, 1, NCK, T)
        self.stage_end()
        xtv = self.xT.rearrange("k p t -> p k t")
        wov = w_out.rearrange("(e p) f -> p e f", p=128)
        GW = 1024
        zb_ = self.ar.bf16(32 * (GW + 1))
        wbo = [self.ar.bf16(32 * 128) for _ in range(2)]
        xb = [self.ar.f32(GW + 1) for _ in range(2)]
        zs = GW + 1
        groups = [(g0, min(GW, T - g0)) for g0 in range(0, T, GW)]
        nw = 0
        for gi, (g0, gn) in enumerate(groups):
            lastg = gi == len(groups) - 1
            ncol = gn + 1 if lastg else gn
            P.dma(zb_.v(0, [(zs, 32), (1, ncol)]), zT.rearrange("e p t -> p e t")[:, :, g0:g0 + ncol], ["zT"], [zb_.name])
            blks = [(b0, min(512, gn - b0)) for b0 in range(0, gn, 512)] + ([(gn, 1)] if lastg else [])
            for ft in range(KT):
                s2 = nw % 2
                nw += 1
                P.dma(wbo[s2].v(0, [(128, 32), (1, 128)]), wov[:, :, ft * 128:(ft + 1) * 128], (), [wbo[s2].name], q="pool")
                P.dma(xb[s2].v(0, [(1, ncol)]), xtv[:, ft, g0:g0 + ncol], ["xT"], [xb[s2].name], q="sp")
                for bi, (b0, w) in enumerate(blks):
                    pb = self.pb[(s2 * 3 + bi) % 8]
                    col = 1 if (lastg and b0 == gn) else 0
                    for e_ in range(32):
                        self.mm(pb.v(0, [(1, w)]), wbo[s2].v(e_ * 128, [(1, 128)]), zb_.v(e_ * zs + b0, [(1, w)]), e_ == 0, e_ == 31,
                                [wbo[s2].name, zb_.name], [pb.name])
                    xv = xb[s2].v(b0, [(1, w)])
                    self.stt(xv, pb.v(0, [(1, w)]), self.modT.v(2 * 32 + 2 * ft + col, [(1, 1)]), xv, ALU.mult, ALU.add,
                             [pb.name, self.modT.name, xb[s2].name], [xb[s2].name])
                P.dma(xtv[:, ft, g0:g0 + ncol], xb[s2].v(0, [(1, ncol)]), [xb[s2].name], ["xT"], q="sp", join=True)
        self.stage_end()

    def st_split(self):
        P, T = self.P, self.T
        H = T // 2
        hf = self.din("hf", [1, 2])
        fl = self.cst_tmp("hf", 2)
        P.dma(fl.ap, hf.to_broadcast([128, 2]), (), [fl.name])
        x3 = self.scratch("xT3", [KT, 128, H + 1])
        xtv = self.xT.rearrange("k p t -> p k t")
        x3v = x3.rearrange("k p t -> p k t")
        for k in range(KT):
            a = self.ar.f32(H)
            b = self.ar.f32(H)
            P.dma(a.ap, xtv[:, k, 0:H], ["xT"], [a.name])
            P.dma(b.ap, xtv[:, k, H:T], ["xT"], [b.name], q="pool")
            self.ts(a.ap, a.ap, fl.v(0, [(1, 1)]), ALU.mult, [a.name, fl.name], [a.name])
            self.stt(b.ap, b.ap, fl.v(1, [(1, 1)]), a.ap, ALU.mult, ALU.add, [a.name, b.name, fl.name], [b.name])
            P.dma(x3v[:, k, 0:H], b.ap, [b.name], ["xT3"], q="pool", join=True)
            if k % 4 == 3:
                self.ar.reset()
        sc_ = self.cst_tmp("xs_s", KT)
        P.dma(sc_.v(0, [(1, KT), (1, 1)]), xtv[:, :, T:T + 1], ["xT"], [sc_.name])
        P.dma(x3v[:, :, H:H + 1], sc_.v(0, [(1, KT), (1, 1)]), [sc_.name], ["xT3"], q="pool", join=True)
        self.stage_end()
        self.T, self.TT, self.xT = H, H + 1, x3


def build(T=2048, layers=4, split=True, premod=True, kinds=None):
    k = K(T)
    k.setup_consts()
    k.stage_end()
    k.st_loadx()
    k.st_mod_setup()
    hT = k.scratch("hT", [KT, 128, k.TT], BF16)
    h32T = k.scratch("h32T", [KT, 128, k.TT])
    for i in range(layers):
        kind, j = (i % 3, i // 3) if kinds is None else (kinds[i], sum(1 for q in kinds[:i] if q == kinds[i]))
        if i == 0 or not premod:
            k.st_mod(i)
        else:
            k.mod_swap()
        if kind == 0:
            k.st_norm(k.A1, 0, None, h32T)
            k.st_pool(j, h32T)
        elif kind == 1:
            k.st_norm(k.A1, 0, hT)
            k.st_dil(hT)
        else:
            k.st_norm(k.A1, 0, hT)
            k.st_ret(hT)
        if i == layers - 1 and split:
            k.st_split()
            hT = k.scratch("hT3", [KT, 128, k.TT], BF16)
        k.st_norm(k.A2, 3, hT)
        k.st_peer_route(i, hT, premod=(i + 1) if (premod and i + 1 < layers) else None)
        k.st_peer_main(i, hT)
    k.st_final()
    nc = k.P.emit()
    return k, nc


_CACHE = {}


def kernel(x_prompt, x_sample, state_pool, cache_dil_kv0, cache_dil_kv1, cache_dil_kv2, state_ret,
           c_prompt, c_sample, norm1_g, norm2_g, mod_w, mod_b, pool_w, pool_scale,
           dil_w_in, dil_w_out, ret_w_in, ret_gn_g, ret_w_out,
           peer_w_q, peer_keys, peer_u, peer_v, final_g):
    f = lambda a: np.ascontiguousarray(np.asarray(a), dtype=np.float32)
    T = x_prompt.shape[1]
    k, nc = build(T)
    shared = dict(mod_w=f(mod_w), mod_b=f(mod_b), norm1_g=f(norm1_g), norm2_g=f(norm2_g), pool_w=f(pool_w), pool_scale=f(pool_scale),
                  dil_w_in=f(dil_w_in), dil_w_out=f(dil_w_out), ret_w_in=f(ret_w_in), ret_gn_g=f(ret_gn_g), ret_w_out=f(ret_w_out),
                  peer_w_q=f(peer_w_q), peer_keys=f(peer_keys), peer_u=f(peer_u), peer_v=f(peer_v), final_g=f(final_g))
    caches = [f(cache_dil_kv0), f(cache_dil_kv1), f(cache_dil_kv2)]
    x_prompt, x_sample, state_pool, state_ret = f(x_prompt), f(x_sample), f(state_pool), f(state_ret)
    c_prompt, c_sample = f(c_prompt), f(c_sample)
    in_maps = []
    for ci in range(8):
        b, s = ci // 2, ci
        m = dict(shared)
        m.update(x_p=x_prompt[b], x_s=x_sample[s], c_p=c_prompt[b], c_s=c_sample[s],
                 state_pool=np.ascontiguousarray(state_pool[:, s]), state_ret=np.ascontiguousarray(state_ret[:, s]))
        for g in range(3):
            m["cache_dil_kv%d" % g] = np.ascontiguousarray(caches[g][:, s])
        m["hf"] = np.array([[1.0 - ci % 2, ci % 2]], dtype=np.float32)
        in_maps.append({n: m[n] for n in k.inp})
    res = run_bass_kernel_spmd(nc, in_maps, core_ids=list(range(8)))
    R = res.results
    B = x_prompt.shape[0]
    half = T // 2

    def prompt(name):
        return np.stack([R[2 * b][name] for b in range(B)], axis=0)

    y_prompt = np.stack([np.concatenate([R[2 * b]["y_p"], R[2 * b + 1]["y_p"]], axis=0) for b in range(B)], axis=0)
    y_sample = np.stack([R[s]["y_s"] for s in range(8)], axis=0)
    pool_p = np.stack([R[2 * b]["pool_p"] for b in range(B)], axis=1)
    pool_s = np.stack([R[s]["pool_s"] for s in range(8)], axis=1)
    outs = [y_prompt, y_sample, pool_p, pool_s]
    for g in range(3):
        outs.append(np.stack([R[2 * b + (g % 2)]["kv%d_p" % g] for b in range(B)], axis=1))
        outs.append(np.stack([R[s]["kv%d_s" % g] for s in range(8)], axis=1))
    outs.append(np.stack([R[2 * b + 1]["ret_p"] for b in range(B)], axis=1))
    outs.append(np.stack([R[s]["ret_s"] for s in range(8)], axis=1))
    return tuple(np.ascontiguousarray(o, dtype=np.float32) for o in outs)
```
